# Optimizing a Trainium2 kernel written in Bass

```python
import jax
import jax.numpy as jnp
from jax import lax
import numpy as np

D_MODEL = 1024
BATCH = 8
SEQ = 4096
DEPTH = 4

GRID_W = 64
CTX_LEN = 256
N_MIXERS = 2
GDN_DK = 128
GDN_DV = 128
GDN_HEADS = D_MODEL // GDN_DK
GDN_CONV = 3
GDN_CHUNK = 64
NA_HEADS = 16
NA_DH = D_MODEL // NA_HEADS
NA_ROWS = 8
NA_COLS = 16
NA_QBLK = 16
NA_KBLK = NA_QBLK + NA_COLS
FF_DENSE = 2816
N_EXPERTS = 8
TOP_K = 2
FF_EXPERT = 3584
MOE_BLK = 512
EPS = 1e-6
NEG_INF = -1e30

kernel_name = 'hybrid_gdn_natten_moe_dit'


def _rms(x, g):
    xf = x.astype(jnp.float32)
    y = xf * lax.rsqrt(jnp.mean(xf * xf, axis=-1, keepdims=True) + EPS)
    return (y * g.astype(jnp.float32)).astype(x.dtype)


def _l2norm(x):
    xf = x.astype(jnp.float32)
    return xf * lax.rsqrt(jnp.sum(xf * xf, axis=-1, keepdims=True) + EPS)


def _glu(gu):
    g, u = jnp.split(gu, 2, axis=-1)
    return jax.nn.silu(g) * u


def _dwconv(x, w):
    return lax.conv_general_dilated(
        x, w[:, None, :].astype(x.dtype), window_strides=(1,),
        padding=[(GDN_CONV // 2, GDN_CONV // 2)],
        dimension_numbers=('NWC', 'WIO', 'NWC'), feature_group_count=x.shape[-1])


def _rev(t, d):
    return t[:, ::-1] if d == 1 else t


def _gated_delta_chunked(q, k, v, g, beta, s0):
    f32 = jnp.float32
    B, L, H, DK = k.shape
    DV = v.shape[-1]
    C = GDN_CHUNK
    n = L // C

    def chunks(t):
        return t.astype(f32).reshape(B, n, C, H, -1).transpose(1, 0, 3, 2, 4)

    q = chunks(q) * (DK ** -0.5)
    k = chunks(k)
    v = chunks(v)
    g = chunks(g[..., None])[..., 0]
    beta = chunks(beta[..., None])[..., 0]
    gc = jnp.cumsum(g, axis=-1)
    causal = jnp.tril(jnp.ones((C, C), bool))
    strict = jnp.tril(jnp.ones((C, C), bool), -1)
    diff = gc[..., :, None] - gc[..., None, :]
    decay = jnp.where(causal, jnp.exp(jnp.where(causal, diff, 0.0)), 0.0)
    kb = k * beta[..., None]
    lower = jnp.where(strict, jnp.einsum('nbhid,nbhjd->nbhij', kb, k) * decay, 0.0)
    eye = jnp.eye(C, dtype=f32)
    rhs = jnp.concatenate([v * beta[..., None], kb * jnp.exp(gc)[..., None]], axis=-1)
    uw = lax.linalg.triangular_solve(jnp.broadcast_to(eye, lower.shape) + lower, rhs, left_side=True, lower=True)
    u, w = uw[..., :DV], uw[..., DV:]
    qk = jnp.einsum('nbhid,nbhjd->nbhij', q, k) * decay

    def step(S, inp):
        q_i, k_i, u_i, w_i, qk_i, gc_i = inp
        v_new = u_i - w_i @ S
        o_i = (q_i * jnp.exp(gc_i)[..., None]) @ S + qk_i @ v_new
        g_last = gc_i[..., -1]
        S = S * jnp.exp(g_last)[..., None, None] + jnp.einsum(
            'bhck,bhcv->bhkv', k_i * jnp.exp(g_last[..., None] - gc_i)[..., None], v_new)
        return S, o_i

    S, o = lax.scan(step, s0.astype(f32), (q, k, u, w, qk, gc))
    o = o.transpose(1, 0, 3, 2, 4).reshape(B, L, H, DV)
    return o, S


def _gdn_project(h, w_in, conv_w, a_log, dt_bias):
    f32 = jnp.float32
    B, L, D = h.shape
    H = GDN_HEADS
    p = h @ w_in
    qkv = jax.nn.silu(_dwconv(p[..., :3 * D], conv_w))
    q = _l2norm(qkv[..., :D].reshape(B, L, H, GDN_DK))
    k = _l2norm(qkv[..., D:2 * D].reshape(B, L, H, GDN_DK))
    v = qkv[..., 2 * D:].reshape(B, L, H, GDN_DV)
    gate = p[..., 3 * D:4 * D].reshape(B, L, H, GDN_DV)
    ab = p[..., 4 * D:].astype(f32).reshape(B, L, 2, 2, H)
    g = -jnp.exp(a_log.astype(f32)) * jax.nn.softplus(ab[:, :, :, 0] + dt_bias.astype(f32))
    beta = jax.nn.sigmoid(ab[:, :, :, 1])
    return q, k, v, g, beta, gate


def _gdn_mixer(h_x, h_c, w_in, conv_w, a_log, dt_bias, norm_g, w_out, need_ctx):
    B, T, D = h_x.shape
    qx, kx, vx, gx, bx, gate_x = _gdn_project(h_x, w_in, conv_w, a_log, dt_bias)
    qc, kc, vc, gcx, bc, gate_c = _gdn_project(h_c, w_in, conv_w, a_log, dt_bias)
    s0 = jnp.zeros((B, GDN_HEADS, GDN_DK, GDN_DV), jnp.float32)
    outs_x, outs_c = [], []
    for d in range(2):
        oc, sc = _gated_delta_chunked(_rev(qc, d), _rev(kc, d), _rev(vc, d),
                                      _rev(gcx[:, :, d], d), _rev(bc[:, :, d], d), s0)
        ox, _ = _gated_delta_chunked(_rev(qx, d), _rev(kx, d), _rev(vx, d),
                                     _rev(gx[:, :, d], d), _rev(bx[:, :, d], d), sc)
        outs_x.append(_rev(ox, d))
        outs_c.append(_rev(oc, d))

    def finish(o, gate, h):
        y = _rms(o, norm_g) * jax.nn.silu(gate.astype(jnp.float32))
        return y.reshape(h.shape).astype(h.dtype) @ w_out

    out_x = finish(outs_x[0] + outs_x[1], gate_x, h_x)
    out_c = finish(outs_c[0] + outs_c[1], gate_c, h_c) if need_ctx else None
    return out_x, out_c


def _na_column_tables():
    n_blk = GRID_W // NA_QBLK
    kstart = np.clip(np.arange(n_blk) * NA_QBLK - NA_COLS // 2, 0, GRID_W - NA_KBLK)
    kcol = kstart[:, None] + np.arange(NA_KBLK)[None, :]
    qcol = np.arange(GRID_W).reshape(n_blk, NA_QBLK)
    wstart = np.clip(qcol - NA_COLS // 2, 0, GRID_W - NA_COLS)
    dc = kcol[:, None, :] - qcol[:, :, None]
    valid = (kcol[:, None, :] >= wstart[..., None]) & (kcol[:, None, :] < wstart[..., None] + NA_COLS)
    dc_idx = np.clip(dc + NA_COLS - 1, 0, 2 * NA_COLS - 2)
    return kcol, dc_idx, valid


def _na_mixer(h_x, h_c, w_in, q_g, k_g, rpb, w_out, need_ctx):
    B, T, D = h_x.shape
    H, DH = NA_HEADS, NA_DH
    rows = T // GRID_W
    wr = min(NA_ROWS, rows)
    scale = DH ** -0.5
    qkv = (h_x @ w_in).reshape(B, T, 3, H, DH)
    qg = (_rms(qkv[:, :, 0], q_g) * scale).reshape(B, rows, GRID_W, H, DH)
    kg = _rms(qkv[:, :, 1], k_g).reshape(B, rows, GRID_W, H, DH)
    vg = qkv[:, :, 2].reshape(B, rows, GRID_W, H, DH)
    ckv = (h_c @ w_in[:, D:]).reshape(B, -1, 2, H, DH)
    ck = _rms(ckv[:, :, 0], k_g)
    cv = ckv[:, :, 1]
    kcol, dc_idx, valid = _na_column_tables()
    n_blk = GRID_W // NA_QBLK
    nk = wr * NA_KBLK
    mask = np.broadcast_to(valid[:, :, None, :], (n_blk, NA_QBLK, wr, NA_KBLK)).reshape(n_blk, NA_QBLK, nk)

    def row_fn(r):
        rs = jnp.clip(r - NA_ROWS // 2, 0, rows - wr)
        kr = lax.dynamic_slice_in_dim(kg, rs, wr, axis=1)[:, :, kcol]
        vr = lax.dynamic_slice_in_dim(vg, rs, wr, axis=1)[:, :, kcol]
        kr = kr.transpose(0, 2, 1, 3, 4, 5).reshape(B, n_blk, nk, H, DH)
        vr = vr.transpose(0, 2, 1, 3, 4, 5).reshape(B, n_blk, nk, H, DH)
        qr = lax.dynamic_index_in_dim(qg, r, axis=1, keepdims=False).reshape(B, n_blk, NA_QBLK, H, DH)
        dr_idx = rs - r + jnp.arange(wr) + NA_ROWS - 1
        bias = rpb[:, dr_idx[None, None, :, None], dc_idx[:, :, None, :]]
        bias = bias.reshape(H, n_blk, NA_QBLK, nk).astype(jnp.float32)
        s_loc = jnp.einsum('bnqhd,bnkhd->bhnqk', qr, kr, preferred_element_type=jnp.float32) + bias
        s_loc = jnp.where(mask, s_loc, NEG_INF)
        s_ctx = jnp.einsum('bnqhd,bchd->bhnqc', qr, ck, preferred_element_type=jnp.float32)
        p = jax.nn.softmax(jnp.concatenate([s_loc, s_ctx], axis=-1), axis=-1).astype(vg.dtype)
        o = (jnp.einsum('bhnqk,bnkhd->bnqhd', p[..., :nk], vr)
             + jnp.einsum('bhnqc,bchd->bnqhd', p[..., nk:], cv))
        return o.reshape(B, GRID_W, H, DH)

    o = lax.map(row_fn, jnp.arange(rows))
    out_x = o.transpose(1, 0, 2, 3, 4).reshape(B, T, D) @ w_out
    out_c = None
    if need_ctx:
        cq = _rms((h_c @ w_in[:, :D]).reshape(B, -1, H, DH), q_g) * scale
        s = jnp.einsum('bqhd,bkhd->bhqk', cq, ck, preferred_element_type=jnp.float32)
        p = jax.nn.softmax(s, axis=-1).astype(cv.dtype)
        out_c = jnp.einsum('bhqk,bkhd->bqhd', p, cv).reshape(B, -1, D) @ w_out
    return out_x, out_c


def _moe_swiglu(h, w_router, w13, w2):
    N, D = h.shape
    logits = jnp.einsum('nd,de->ne', h, w_router, preferred_element_type=jnp.float32)
    top_logit, top_idx = lax.top_k(logits, TOP_K)
    top_w = jax.nn.softmax(top_logit, axis=-1).astype(h.dtype)
    A = N * TOP_K
    e_flat = top_idx.reshape(A)
    order = jnp.argsort(e_flat)
    e_sorted = e_flat[order]
    tok_sorted = order // TOP_K
    w_sorted = top_w.reshape(A)[order]
    counts = jnp.bincount(e_flat, length=N_EXPERTS)
    padded = (counts + MOE_BLK - 1) // MOE_BLK * MOE_BLK
    pad_end = jnp.cumsum(padded)
    pad_start = pad_end - padded
    start = jnp.cumsum(counts) - counts
    dest = pad_start[e_sorted] + jnp.arange(A) - start[e_sorted]
    n_blk = -(-A // MOE_BLK) + N_EXPERTS
    buf_tok = jnp.zeros((n_blk * MOE_BLK,), jnp.int32).at[dest].set(tok_sorted)
    blk_e = jnp.minimum(jnp.searchsorted(pad_end, jnp.arange(n_blk) * MOE_BLK, side='right'), N_EXPERTS - 1)
    xb = h[buf_tok].reshape(n_blk, MOE_BLK, D)

    def expert_block(args):
        xe, e = args
        return _glu(xe @ w13[e]) @ w2[e]

    yb = lax.map(expert_block, (xb, blk_e)).reshape(n_blk * MOE_BLK, D)
    return jnp.zeros_like(h).at[tok_sorted].add(yb[dest] * w_sorted[:, None])


def setup_inputs(seed: int = 0) -> dict:
    key = jax.random.key(seed)
    ks = jax.random.split(key, 26)
    f32 = jnp.float32
    D = D_MODEL
    n_even = (DEPTH + 1) // 2
    n_odd = DEPTH // 2

    def nrm(k, shape, fan_in, s=1.0):
        return jax.random.normal(k, shape, f32) * (s * fan_in ** -0.5)

    def gain(k, shape):
        return 1.0 + 0.05 * jax.random.normal(k, shape, f32)

    dt = jnp.exp(jax.random.uniform(ks[12], (n_even, 2, GDN_HEADS), f32, minval=-6.9078, maxval=-2.3026))
    return {
        'x': jax.random.normal(ks[0], (BATCH, SEQ, D), f32),
        'c': jax.random.normal(ks[1], (BATCH, D), f32),
        'ctx': jax.random.normal(ks[2], (BATCH, CTX_LEN, D), f32),
        'c_ctx': jax.random.normal(ks[3], (D,), f32),
        'ada_w': nrm(ks[4], (DEPTH, D, 6 * D), D, 0.5),
        'ada_b': 0.02 * jax.random.normal(ks[5], (DEPTH, 6 * D), f32),
        'norm1_g': gain(ks[6], (DEPTH, D)),
        'norm2_g': gain(ks[7], (DEPTH, D)),
        'gdn_w_in': nrm(ks[8], (n_even, D, 4 * D + 4 * GDN_HEADS), D),
        'gdn_conv_w': nrm(ks[9], (n_even, GDN_CONV, 3 * D), GDN_CONV),
        'gdn_a_log': jnp.log(jax.random.uniform(ks[10], (n_even, 2, GDN_HEADS), f32, minval=1.0, maxval=16.0)),
        'gdn_dt_bias': dt + jnp.log(-jnp.expm1(-dt)),
        'gdn_norm_g': gain(ks[11], (n_even, GDN_DV)),
        'gdn_w_out': nrm(ks[13], (n_even, D, D), D),
        'na_w_in': nrm(ks[14], (n_odd, D, 3 * D), D),
        'na_q_norm': gain(ks[15], (n_odd, NA_DH)),
        'na_k_norm': gain(ks[16], (n_odd, NA_DH)),
        'na_rpb': 0.1 * jax.random.normal(ks[17], (n_odd, NA_HEADS, 2 * NA_ROWS - 1, 2 * NA_COLS - 1), f32),
        'na_w_out': nrm(ks[18], (n_odd, D, D), D),
        'ffn_w13': nrm(ks[19], (n_even, D, 2 * FF_DENSE), D),
        'ffn_w2': nrm(ks[20], (n_even, FF_DENSE, D), FF_DENSE),
        'moe_router': nrm(ks[21], (n_odd, D, N_EXPERTS), D),
        'moe_w13': nrm(ks[22], (n_odd, N_EXPERTS, D, 2 * FF_EXPERT), D),
        'moe_w2': nrm(ks[23], (n_odd, N_EXPERTS, FF_EXPERT, D), FF_EXPERT),
    }


def reference(x, c, ctx, c_ctx, ada_w, ada_b, norm1_g, norm2_g, gdn_w_in, gdn_conv_w, gdn_a_log,
              gdn_dt_bias, gdn_norm_g, gdn_w_out, na_w_in, na_q_norm, na_k_norm, na_rpb, na_w_out,
              ffn_w13, ffn_w2, moe_router, moe_w13, moe_w2):
    B, T, D = x.shape
    cx = ctx
    for i in range(DEPTH):
        last = i == DEPTH - 1
        j = i // 2
        mx = jnp.split((jax.nn.silu(c) @ ada_w[i] + ada_b[i])[:, None, :], 6, axis=-1)
        mc = jnp.split(jax.nn.silu(c_ctx) @ ada_w[i] + ada_b[i], 6, axis=-1)
        hx = _rms(x, norm1_g[i]) * (1 + mx[1]) + mx[0]
        hc = _rms(cx, norm1_g[i]) * (1 + mc[1]) + mc[0]
        if i % N_MIXERS == 0:
            ox, oc = _gdn_mixer(hx, hc, gdn_w_in[j], gdn_conv_w[j], gdn_a_log[j], gdn_dt_bias[j],
                                gdn_norm_g[j], gdn_w_out[j], not last)
        else:
            ox, oc = _na_mixer(hx, hc, na_w_in[j], na_q_norm[j], na_k_norm[j], na_rpb[j],
                               na_w_out[j], not last)
        x = x + mx[2] * ox
        if not last:
            cx = cx + mc[2] * oc
        hx = _rms(x, norm2_g[i]) * (1 + mx[4]) + mx[3]
        if i % 2 == 0:
            x = x + mx[5] * (_glu(hx @ ffn_w13[j]) @ ffn_w2[j])
            if not last:
                hc = _rms(cx, norm2_g[i]) * (1 + mc[4]) + mc[3]
                cx = cx + mc[5] * (_glu(hc @ ffn_w13[j]) @ ffn_w2[j])
        else:
            if last:
                toks = hx.reshape(-1, D)
            else:
                hc = _rms(cx, norm2_g[i]) * (1 + mc[4]) + mc[3]
                toks = jnp.concatenate([hx.reshape(-1, D), hc.reshape(-1, D)], axis=0)
            y = _moe_swiglu(toks, moe_router[j], moe_w13[j], moe_w2[j])
            x = x + mx[5] * y[:B * T].reshape(B, T, D)
            if not last:
                cx = cx + mc[5] * y[B * T:].reshape(cx.shape)
    return x
```

```python
import numpy as np
from contextlib import ExitStack
import concourse.bass as bass
import concourse.mybir as mybir
from concourse.bass_utils import run_bass_kernel_spmd

F32 = mybir.dt.float32
BF16 = mybir.dt.bfloat16
AF = mybir.ActivationFunctionType
ALU = mybir.AluOpType

D = 1024
T = 4096
NCTX = 256
NTOK = T + NCTX
DEPTH = 4
NCORES = 8
EPS = 1e-6
FF_DENSE = 2816
FF_EXPERT = 3584
NEXP = 8


class Dep:
    __slots__ = ("w", "r", "dsem", "excl")

    def __init__(self):
        self.w = {}
        self.r = {}
        self.dsem = None
        self.excl = False


class Tl:
    def __init__(self, t, is_dram=False):
        self.t = t
        self.d = Dep()
        self.subs = {}
        self.is_dram = is_dram

    def __getitem__(self, k):
        if self.is_dram:
            return self.t.ap()[k]
        return self.t[k]

    def ap(self):
        return self.t.ap() if self.is_dram else self.t[:]

    def sub(self, key):
        if key not in self.subs:
            self.subs[key] = Dep()
        return self.subs[key]


class KB:
    def __init__(self, nc):
        self.nc = nc
        self.E = {"pe": nc.tensor, "act": nc.scalar, "dve": nc.vector, "pool": nc.gpsimd, "sp": nc.sync}
        self.es = ExitStack()
        self.sems = []
        self.csem = {}
        for e in self.E:
            self.csem[e] = self._newsem("c_" + e)
        self.cnt = {e: 0 for e in self.E}
        self.known = {e: {} for e in self.E}
        self.dfree = [self._newsem("d%d" % i) for i in range(90)]
        self.dval = {}
        self.dused = []
        self.stage_deps = []
        self.stage_es = None
        self.uid = 0
        self.ninstr = 0

    def _newsem(self, name):
        s = self.es.enter_context(self.nc.semaphore(name))
        self.sems.append(s)
        return len(self.sems) - 1

    def tile(self, shape, dtype, name=None, persistent=False):
        self.uid += 1
        name = (name or "t") + "_%d" % self.uid
        es = self.es if persistent else self.stage_es
        t = es.enter_context(self.nc.sbuf_tensor(name, list(shape), dtype))
        return Tl(t)

    def dram(self, name, shape, dtype, kind="Internal"):
        t = self.nc.dram_tensor(name, list(shape), dtype, kind=kind)
        return Tl(t, is_dram=True)

    def _wait(self, e, tok):
        if tok is None:
            return
        idx, val = tok
        if e == "pe" and idx == self.csem["pe"]:
            return
        if self.known[e].get(idx, 0) >= val:
            return
        self.E[e].wait_ge(self.sems[idx], val)
        self.known[e][idx] = val

    def _deps_wait(self, e, reads, writes, join=False):
        for d in reads:
            for tok in d.w.items():
                self._wait(e, tok)
            if d.excl:
                for tok in d.r.items():
                    if tok[0] != self.csem.get(e):
                        self._wait(e, tok)
        for d in writes:
            if not join:
                for tok in d.w.items():
                    self._wait(e, tok)
            for tok in d.r.items():
                self._wait(e, tok)

    @staticmethod
    def _norm(ds):
        out = []
        for d in ds:
            if d is None:
                continue
            out.append(d.d if isinstance(d, Tl) else d)
        return out

    def op(self, e, fn, reads=(), writes=()):
        reads = self._norm(reads)
        writes = self._norm(writes)
        self._deps_wait(e, reads, writes)
        ins = fn(self.E[e])
        self.cnt[e] += 1
        self.ninstr += 1
        ins.then_inc(self.sems[self.csem[e]], 1)
        tok = (self.csem[e], self.cnt[e])
        for d in reads:
            if d.r.get(tok[0], 0) < tok[1]:
                d.r[tok[0]] = tok[1]
        for d in writes:
            d.w = {tok[0]: tok[1]}
            d.r = {}
        return ins

    def dma(self, q, out, in_, reads=(), writes=(), join=False, sem=None, **kw):
        reads = self._norm(reads)
        writes = self._norm(writes)
        assert len(writes) == 1
        wd = writes[0]
        self._deps_wait(q, reads, writes, join=join)
        sd = wd if sem is None else self._norm([sem])[0]
        if sd.dsem is None:
            sd.dsem = self.dfree.pop()
            self.dused.append(sd)
        idx = sd.dsem
        self.dval[idx] = self.dval.get(idx, 0) + 16
        ins = self.E[q].dma_start(out=out, in_=in_, **kw)
        ins.then_inc(self.sems[idx], 16)
        self.ninstr += 1
        tok = (idx, self.dval[idx])
        for d in reads:
            if d.r.get(idx, 0) < tok[1]:
                d.r[idx] = tok[1]
        if join:
            wd.w[idx] = tok[1]
        else:
            wd.w = {idx: tok[1]}
            wd.r = {}
        return ins

    def barrier(self):
        toks = [(self.csem[e], self.cnt[e]) for e in self.E if self.cnt[e] > 0]
        toks += [(idx, v) for idx, v in self.dval.items()]
        for e in self.E:
            for tok in toks:
                self._wait(e, tok)
        for d in self.dused:
            self.dfree.append(d.dsem)
            d.dsem = None
        self.dused = []

    class _Stage:
        def __init__(self, kb):
            self.kb = kb

        def __enter__(self):
            self.prev = self.kb.stage_es
            self.kb.stage_es = ExitStack()
            self.kb.stage_es.__enter__()
            return self.kb

        def __exit__(self, *a):
            self.kb.barrier()
            self.kb.stage_es.__exit__(*a)
            self.kb.stage_es = self.prev
            return False

    def stage(self):
        return KB._Stage(self)


def token_blocks(n0, n1, blk=512):
    out = []
    t = n0
    while t < n1:
        b = min(blk, n1 - t)
        out.append((t, b))
        t += b
    return out


class Prog:
    def __init__(self, nc, layers=(0, 1, 2, 3), debug=False):
        self.nc = nc
        self.k = KB(nc)
        k = self.k
        self.layers = layers
        self.ext_shapes = {
            "x": [T, D], "ctx": [NCTX, D], "cvec": [2, D], "ada_w": [DEPTH, D, 6 * D], "ada_b": [DEPTH, 6 * D],
            "norm1_g": [DEPTH, D], "norm2_g": [DEPTH, D], "gdn_w_in": [2, D, 4 * D + 32],
            "gdn_conv_w": [2, 3, 3 * D], "gdn_a_log": [2, 2, 8], "gdn_dt_bias": [2, 2, 8],
            "gdn_norm_g": [2, 128], "gdn_w_out": [2, D, D], "na_w_in": [2, D, 3 * D], "na_q_norm": [2, 64],
            "na_k_norm": [2, 64], "na_bias": [2, 9, 128, 16 * 320], "na_w_out": [2, D, D],
            "ffn_w13": [2, D, 2 * FF_DENSE], "ffn_w2": [2, FF_DENSE, D], "moe_router": [2, D, NEXP],
            "moe_w13": [2, NEXP, D, 2 * FF_EXPERT], "moe_w2": [2, NEXP, FF_EXPERT, D],
            "consts": [128, 4 * 128 + 1024]}
        self.exts = {}
        self.y = k.dram("y", [T, D], F32, kind="ExternalOutput")
        self.XT = k.dram("XT", [D, NTOK], F32)
        self.OT = k.dram("OT", [D, NTOK], BF16)
        self.cst = k.tile([128, 4 * 128], F32, "cst", persistent=True)
        self.ones_bf = k.tile([128, 128], BF16, "ones_bf", persistent=True)
        self.ident = self.cst[:, 0:128]
        self.MODS = k.tile([128, DEPTH * 6 * 8 * 2], F32, "mods", persistent=True)
        self.GG = k.tile([128, DEPTH * 2 * 8 * 2], F32, "gg", persistent=True)
        self.eps_t = k.tile([128, 1], F32, "eps", persistent=True)
        self.ps = []
        for b in range(8):
            t = k.es.enter_context(nc.psum_tensor("ps%d" % b, [128, 512], F32))
            pt_ = Tl(t)
            pt_.d.excl = True
            self.ps.append(pt_)

    def __getattr__(self, name):
        shapes = self.__dict__.get("ext_shapes", {})
        if name in shapes:
            if name not in self.exts:
                self.exts[name] = self.k.dram(name, shapes[name], F32, kind="ExternalInput")
            return self.exts[name]
        raise AttributeError(name)

    STG = 2048

    def alloc_stg(self):
        self.stg = [self.k.tile([128, self.STG], F32, "stg%d" % i) for i in range(2)]
        self.stg_i = 0

    def cast_load(self, dst, src, pat, wdep, first=True, **dims):
        k = self.k
        st = self.stg[self.stg_i % 2]
        self.stg_i += 1
        n = 1
        for d_ in dst.shape[1:]:
            n *= d_
        assert n <= self.STG
        view = st[:, 0:n]
        if pat is not None:
            view = view.rearrange(pat, **dims)
        k.dma("sp", view, src, reads=[self.consts], writes=[st])
        deps_w = [wdep]
        k.op("pool", lambda e: e.tensor_copy(out=dst, in_=view), reads=[st], writes=deps_w) if first else \
            self._join_op("pool", lambda e: e.tensor_copy(out=dst, in_=view), [st], wdep)

    def _join_op(self, eng, fn, reads, wdep):
        k = self.k
        wd = k._norm([wdep])[0]
        reads_n = k._norm(reads)
        k._deps_wait(eng, reads_n, [wd], join=True)
        ins = fn(k.E[eng])
        k.cnt[eng] += 1
        k.ninstr += 1
        ins.then_inc(k.sems[k.csem[eng]], 1)
        tok = (k.csem[eng], k.cnt[eng])
        for d in reads_n:
            if d.r.get(tok[0], 0) < tok[1]:
                d.r[tok[0]] = tok[1]
        wd.w[tok[0]] = tok[1]

    def mod(self, l, m, c, t):
        o = ((l * 6 + m) * 8 + c) * 2 + t
        return self.MODS[:, o:o + 1]

    def gg(self, l, n, c, t):
        o = ((l * 2 + n) * 8 + c) * 2 + t
        return self.GG[:, o:o + 1]

    def stage_init(self):
        k = self.k
        nc = self.nc
        with k.stage():
            k.dma("sp", self.cst.ap(), self.consts[:, 0:512], reads=[self.consts], writes=[self.cst])
            k.op("dve", lambda e: e.tensor_copy(out=self.ones_bf.ap(), in_=self.cst[:, 384:512]),
                 reads=[self.cst], writes=[self.ones_bf])
            k.op("dve", lambda e: e.memset(self.eps_t.ap(), EPS), writes=[self.eps_t])
            craw = k.tile([128, 2, 8], F32, "craw")
            sc = k.tile([128, 8, 2], F32, "sc")
            with nc.allow_non_contiguous_dma(reason="tiny"):
                k.dma("sp", craw.ap(), self.cvec.ap().rearrange("t (c p) -> p t c", p=128),
                      reads=[self.cvec], writes=[craw])
            k.op("act", lambda e: e.activation(out=sc.ap().rearrange("p c t -> p t c"), in_=craw.ap(), func=AF.Silu),
                 reads=[craw], writes=[sc])
            ab = k.tile([128, DEPTH, 48], F32, "adab")
            g1 = k.tile([128, 2, DEPTH, 8], F32, "ng")
            with nc.allow_non_contiguous_dma(reason="tiny"):
                k.dma("sp", ab.ap(), self.ada_b.ap().rearrange("l (o p) -> p l o", p=128),
                      reads=[self.ada_b], writes=[ab])
                k.dma("sp", g1[:, 0, :, :], self.norm1_g.ap().rearrange("l (c p) -> p l c", p=128),
                      reads=[self.norm1_g], writes=[g1.sub(0)])
                k.dma("sp", g1[:, 1, :, :], self.norm2_g.ap().rearrange("l (c p) -> p l c", p=128),
                      reads=[self.norm2_g], writes=[g1.sub(1)])
            NW = 1536
            wb = [k.tile([128, 8, NW], F32, "adaw%d" % i) for i in range(2)]
            it = 0
            for l in range(DEPTH):
                pst = self.ps[l % 2]
                for q in range(6 * D // NW):
                    w = wb[it % 2]
                    it += 1
                    k.dma("sp", w.ap(),
                          self.ada_w[l].rearrange("(kk p) n -> p kk n", p=128)[:, :, q * NW:(q + 1) * NW],
                          reads=[self.ada_w], writes=[w])
                    for oc in range(NW // 128):
                        occ = q * (NW // 128) + oc
                        for kk in range(8):
                            k.op("pe", lambda e, kk=kk, oc=oc, occ=occ, w=w, pst=pst: e.matmul(
                                pst[:, occ * 2:occ * 2 + 2], lhsT=w[:, kk, oc * 128:(oc + 1) * 128],
                                rhs=sc[:, kk, :], start=(kk == 0), stop=(kk == 7)),
                                reads=[w, sc], writes=[pst])
                mv = self.MODS[:, l * 96:(l + 1) * 96].rearrange("p (o t) -> p o t", t=2)
                for t in range(2):
                    k.op("dve", lambda e, t=t, mv=mv, pst=pst, l=l: e.tensor_tensor(
                        out=mv[:, :, t], in0=pst[:, 0:96].rearrange("p (o t) -> p o t", t=2)[:, :, t],
                        in1=ab[:, l, :], op=ALU.add), reads=[pst, ab], writes=[self.MODS])
                for n in range(2):
                    m = 1 if n == 0 else 4
                    for t in range(2):
                        o = (l * 6 + m) * 16
                        src = self.MODS[:, o:o + 16].rearrange("p (c t) -> p c t", t=2)[:, :, t]
                        og = (l * 2 + n) * 16
                        dst = self.GG[:, og:og + 16].rearrange("p (c t) -> p c t", t=2)[:, :, t]
                        k.op("dve", lambda e, src=src, dst=dst, l=l, n=n: e.scalar_tensor_tensor(
                            out=dst, in0=src, scalar=1.0, in1=g1[:, n, l, :], op0=ALU.add, op1=ALU.mult),
                            reads=[self.MODS, g1.sub(0), g1.sub(1)], writes=[self.GG])

    def stage_in_transpose(self):
        k = self.k
        XTv = self.XT.ap().rearrange("(c p) t -> p c t", p=128)
        with k.stage():
            xin = [k.tile([128, D], F32, "xin%d" % i) for i in range(2)]
            xo = [k.tile([128, 8, 128], F32, "xo%d" % i) for i in range(2)]
            for ti in range(NTOK // 128):
                src = self.x[ti * 128:(ti + 1) * 128, :] if ti < T // 128 else \
                    self.ctx[(ti - T // 128) * 128:(ti - T // 128 + 1) * 128, :]
                xi = xin[ti % 2]
                o = xo[ti % 2]
                k.dma("sp", xi.ap(), src, reads=[self.x], writes=[xi])
                for half in range(2):
                    pst = self.ps[(ti % 2) * 2 + half]
                    for c4 in range(4):
                        c = half * 4 + c4
                        k.op("pe", lambda e, c=c, c4=c4, pst=pst, xi=xi: e.transpose(
                            pst[:, c4 * 128:(c4 + 1) * 128], xi[:, c * 128:(c + 1) * 128], self.ident),
                            reads=[xi, self.cst], writes=[pst])
                    eng = "act" if half == 0 else "dve"
                    if eng == "act":
                        k.op("act", lambda e, pst=pst, o=o, half=half: e.copy(
                            out=o[:, half * 4:(half + 1) * 4, :], in_=pst.ap().rearrange("p (c t) -> p c t", t=128)),
                            reads=[pst], writes=[o.sub(half)])
                    else:
                        k.op("dve", lambda e, pst=pst, o=o, half=half: e.tensor_copy(
                            out=o[:, half * 4:(half + 1) * 4, :], in_=pst.ap().rearrange("p (c t) -> p c t", t=128)),
                            reads=[pst], writes=[o.sub(half)])
                k.dma("sp", XTv[:, :, ti * 128:(ti + 1) * 128], o.ap(),
                      reads=[o.sub(0), o.sub(1)], writes=[self.XT], join=True, sem=o.sub("st"))

    def stage_out_transpose(self, debug_ctx=False):
        k = self.k
        XTv = self.XT.ap().rearrange("(c p) t -> p c t", p=128)
        if debug_ctx:
            self.yc = k.dram("yc", [NCTX, D], F32, kind="ExternalOutput")
        with k.stage():
            xin = [k.tile([128, 8, 128], F32, "oin%d" % i) for i in range(2)]
            xo = [k.tile([128, D], F32, "oo%d" % i) for i in range(2)]
            for ti in range((NTOK if debug_ctx else T) // 128):
                xi = xin[ti % 2]
                o = xo[ti % 2]
                k.dma("sp", xi.ap(), XTv[:, :, ti * 128:(ti + 1) * 128], reads=[self.XT], writes=[xi])
                for half in range(2):
                    pst = self.ps[(ti % 2) * 2 + half]
                    for c4 in range(4):
                        c = half * 4 + c4
                        k.op("pe", lambda e, c=c, c4=c4, pst=pst, xi=xi: e.transpose(
                            pst[:, c4 * 128:(c4 + 1) * 128], xi[:, c, :], self.ident),
                            reads=[xi, self.cst], writes=[pst])
                    if half == 0:
                        k.op("act", lambda e, pst=pst, o=o, half=half: e.copy(
                            out=o[:, half * 512:(half + 1) * 512], in_=pst.ap()),
                            reads=[pst], writes=[o.sub(half)])
                    else:
                        k.op("dve", lambda e, pst=pst, o=o, half=half: e.tensor_copy(
                            out=o[:, half * 512:(half + 1) * 512], in_=pst.ap()),
                            reads=[pst], writes=[o.sub(half)])
                dsto = self.y[ti * 128:(ti + 1) * 128, :] if ti < T // 128 else \
                    self.yc[(ti - T // 128) * 128:(ti - T // 128 + 1) * 128, :]
                k.dma("sp", dsto, o.ap(),
                      reads=[o.sub(0), o.sub(1)], writes=[self.y], join=True, sem=o.sub("st"))

    def norm_block(self, X, h, off, n, l, nidx, tsel, sq, rs, ps_ss, h32=None):
        k = self.k
        m_shift = 0 if nidx == 0 else 3
        k.op("act", lambda e: e.activation(out=sq[:, :, 0:n], in_=X[:, :, off:off + n], func=AF.Square),
             reads=[X], writes=[sq])
        for c in range(8):
            k.op("pe", lambda e, c=c: e.matmul(ps_ss[:, 0:n], lhsT=self.ones_bf.ap(), rhs=sq[:, c, 0:n],
                                               start=(c == 0), stop=(c == 7)),
                 reads=[sq, self.ones_bf], writes=[ps_ss])
        k.op("act", lambda e: e.activation(out=rs[:, 0:n], in_=ps_ss[:, 0:n], func=AF.Sqrt,
                                           scale=1.0 / D, bias=self.eps_t.ap()),
             reads=[ps_ss, self.eps_t], writes=[rs])
        k.op("dve", lambda e: e.reciprocal(out=rs[:, 0:n], in_=rs[:, 0:n]), reads=[rs], writes=[rs])
        for c in range(8):
            if h32 is not None:
                k.op("dve", lambda e, c=c: e.scalar_tensor_tensor(
                    out=h32[:, c, 0:n], in0=X[:, c, off:off + n], scalar=self.gg(l, nidx, c, tsel),
                    in1=rs[:, 0:n], op0=ALU.mult, op1=ALU.mult), reads=[X, rs, self.GG], writes=[h32])
                k.op("act", lambda e, c=c: e.activation(
                    out=h32[:, c, 0:n], in_=h32[:, c, 0:n], func=AF.Identity,
                    bias=self.mod(l, m_shift, c, tsel), scale=1.0), reads=[h32, self.MODS], writes=[h32])
                k.op("pool", lambda e, c=c: e.tensor_copy(out=h[:, c, off:off + n], in_=h32[:, c, 0:n]),
                     reads=[h32], writes=[h])
            else:
                k.op("dve", lambda e, c=c: e.scalar_tensor_tensor(
                    out=h[:, c, off:off + n], in0=X[:, c, off:off + n], scalar=self.gg(l, nidx, c, tsel),
                    in1=rs[:, 0:n], op0=ALU.mult, op1=ALU.mult), reads=[X, rs, self.GG], writes=[h])
                k.op("act", lambda e, c=c: e.activation(
                    out=h[:, c, off:off + n], in_=h[:, c, off:off + n], func=AF.Identity,
                    bias=self.mod(l, m_shift, c, tsel), scale=1.0), reads=[h, self.MODS], writes=[h])

    def stage_ffn(self, l, moe):
        k = self.k
        nc = self.nc
        j = l // 2
        last = (l == DEPTH - 1)
        ntok = T if last else NTOK
        FF = FF_EXPERT if moe else FF_DENSE
        nfc = FF // 128
        FG = 4
        fgroups = [(f0, min(FG, nfc - f0)) for f0 in range(0, nfc, FG)]
        nexp = NEXP if moe else 1
        halves = [(0, ntok // 2), (ntok // 2, ntok)]
        XTv = self.XT.ap().rearrange("(c p) t -> p c t", p=128)
        for (h0, h1) in halves:
            nh = h1 - h0
            with k.stage():
                X = k.tile([128, 8, nh], F32, "X")
                H = k.tile([128, 8, nh], BF16, "H")
                GW = k.tile([8, nh], F32, "GW") if moe else None
                sel = k.tile([8, NEXP * 128], F32, "sel") if moe else None
                ph1 = k.stage()
                ph1.__enter__()
                sq = k.tile([128, 8, 512], BF16, "sq")
                rs = k.tile([128, 512], F32, "rs")
                blocks = []
                for (t0, n) in token_blocks(h0, h1):
                    if t0 < T < t0 + n:
                        blocks.append((t0, T - t0))
                        blocks.append((T, t0 + n - T))
                    else:
                        blocks.append((t0, n))
                for bi, (t0, n) in enumerate(blocks):
                    k.dma("sp", X[:, :, t0 - h0:t0 - h0 + n], XTv[:, :, t0:t0 + n],
                          reads=[self.XT], writes=[X.sub(bi)])
                if moe:
                    h32 = k.tile([128, 8, 512], F32, "h32")
                    wr = k.tile([128, 8, NEXP], F32, "wr")
                    with nc.allow_non_contiguous_dma(reason="small router weight"):
                        k.dma("sp", wr.ap(), self.moe_router[j].rearrange("(kk p) e -> p kk e", p=128),
                              reads=[self.moe_router], writes=[wr])
                    k.dma("sp", sel.ap(), self.consts[0:8, 512:512 + NEXP * 128], reads=[self.consts], writes=[sel])
                    lg = k.tile([128, 8], F32, "lg")
                    r1 = k.tile([128, 8], F32, "r1")
                    r2 = k.tile([128, 8], F32, "r2")
                    m1 = k.tile([128, 1], F32, "m1")
                    m2 = k.tile([128, 1], F32, "m2")
                    gwt = k.tile([128, 8], F32, "gwt")
                for bi, (t0, n) in enumerate(blocks):
                    tsel = 0 if t0 < T else 1
                    off = t0 - h0
                    self.norm_block(_SubView(X, bi), H, off, n, l, 1, tsel, sq, rs, self.ps[0],
                                    h32=(h32 if moe else None))
                    if moe:
                        for s in range(n // 128):
                            pl = self.ps[1]
                            for kk in range(8):
                                k.op("pe", lambda e, kk=kk, s=s, pl=pl: e.matmul(
                                    pl[:, 0:8], lhsT=h32[:, kk, s * 128:(s + 1) * 128], rhs=wr[:, kk, :],
                                    start=(kk == 0), stop=(kk == 7)), reads=[h32, wr], writes=[pl])
                            k.op("dve", lambda e, pl=pl: e.tensor_copy(out=lg.ap(), in_=pl[:, 0:8]),
                                 reads=[pl], writes=[lg])
                            k.op("dve", lambda e: e.reduce_max(out=m1.ap(), in_=lg.ap(), axis=mybir.AxisListType.X),
                                 reads=[lg], writes=[m1])
                            k.op("dve", lambda e: e.tensor_scalar(out=r1.ap(), in0=lg.ap(), scalar1=m1.ap(),
                                                                  scalar2=None, op0=ALU.is_ge),
                                 reads=[lg, m1], writes=[r1])
                            k.op("dve", lambda e: e.scalar_tensor_tensor(out=r2.ap(), in0=r1.ap(), scalar=-1e30,
                                                                         in1=lg.ap(), op0=ALU.mult, op1=ALU.add),
                                 reads=[r1, lg], writes=[r2])
                            k.op("dve", lambda e: e.reduce_max(out=m2.ap(), in_=r2.ap(), axis=mybir.AxisListType.X),
                                 reads=[r2], writes=[m2])
                            k.op("dve", lambda e: e.tensor_scalar(out=r1.ap(), in0=lg.ap(), scalar1=m2.ap(),
                                                                  scalar2=None, op0=ALU.is_ge),
                                 reads=[lg, m2], writes=[r1])
                            k.op("dve", lambda e: e.tensor_scalar(out=r2.ap(), in0=lg.ap(), scalar1=m1.ap(),
                                                                  scalar2=None, op0=ALU.subtract),
                                 reads=[lg, m1], writes=[r2])
                            k.op("act", lambda e: e.activation(out=r2.ap(), in_=r2.ap(), func=AF.Exp),
                                 reads=[r2], writes=[r2])
                            k.op("dve", lambda e: e.tensor_tensor(out=gwt.ap(), in0=r1.ap(), in1=r2.ap(), op=ALU.mult),
                                 reads=[r1, r2], writes=[gwt])
                            k.op("dve", lambda e: e.reduce_sum(out=m2.ap(), in_=gwt.ap(), axis=mybir.AxisListType.X),
                                 reads=[gwt], writes=[m2])
                            k.op("dve", lambda e: e.reciprocal(out=m2.ap(), in_=m2.ap()), reads=[m2], writes=[m2])
                            k.op("dve", lambda e: e.tensor_scalar(out=gwt.ap(), in0=gwt.ap(), scalar1=m2.ap(),
                                                                  scalar2=None, op0=ALU.mult),
                                 reads=[gwt, m2], writes=[gwt])
                            pt = self.ps[2]
                            k.op("pe", lambda e, pt=pt: e.transpose(pt[0:8, 0:128], gwt.ap(), self.ident),
                                 reads=[gwt, self.cst], writes=[pt])
                            k.op("act", lambda e, pt=pt, s=s, off=off: e.copy(
                                out=GW[:, off + s * 128:off + (s + 1) * 128], in_=pt[0:8, 0:128]),
                                reads=[pt], writes=[GW])
                ph1.__exit__(None, None, None)
                ph2 = k.stage()
                ph2.__enter__()
                self.alloc_stg()
                w13b = [k.tile([128, 8, 2, FG * 128], BF16, "w13b%d" % i) for i in range(2)]
                w2b = [k.tile([128, FG, D], BF16, "w2b%d" % i) for i in range(2)]
                actb = [k.tile([128, FG, 512], BF16, "actb%d" % i) for i in range(2)]
                sg = [k.tile([128, 512], F32, "sg%d" % i) for i in range(2)]
                it = 0
                ai = 0
                pi = 0
                for ex in range(nexp):
                    if moe:
                        w13src = self.moe_w13[j, ex]
                        w2src = self.moe_w2[j, ex]
                    else:
                        w13src = self.ffn_w13[j]
                        w2src = self.ffn_w2[j]
                    for (f0, nf) in fgroups:
                        w13 = w13b[it % 2]
                        w2 = w2b[it % 2]
                        it += 1
                        w13v = w13src.rearrange("(kk p) n -> p kk n", p=128)
                        for gu in range(2):
                            for kk in range(0, 8, 4):
                                self.cast_load(w13[:, kk:kk + 4, gu, 0:nf * 128],
                                               w13v[:, kk:kk + 4, gu * FF + f0 * 128: gu * FF + (f0 + nf) * 128],
                                               "p (a b) -> p a b", w13, first=(gu == 0 and kk == 0), a=4)
                        w2v = w2src[f0 * 128:(f0 + nf) * 128, :].rearrange("(f p) n -> p f n", p=128)
                        for f2 in range(0, nf, 2):
                            self.cast_load(w2[:, f2:f2 + 2, :], w2v[:, f2:f2 + 2, :], "p (a b) -> p a b", w2,
                                           first=(f2 == 0), a=2)
                        for bi, (t0, n) in enumerate(blocks):
                            off = t0 - h0
                            act = actb[ai % 2]
                            ai += 1
                            pgw = None
                            if moe:
                                pgw = self.ps[6]
                                k.op("pe", lambda e, ex=ex, pgw=pgw: e.matmul(
                                    pgw[:, 0:n], lhsT=sel[:, ex * 128:(ex + 1) * 128], rhs=GW[:, off:off + n],
                                    start=True, stop=True), reads=[sel, GW], writes=[pgw])
                            for f in range(nf):
                                pg = self.ps[(pi % 2) * 2]
                                pu = self.ps[(pi % 2) * 2 + 1]
                                s_ = sg[pi % 2]
                                pi += 1
                                for kk in range(8):
                                    k.op("pe", lambda e, kk=kk, f=f, pg=pg, w13=w13: e.matmul(
                                        pg[:, 0:n], lhsT=w13[:, kk, 0, f * 128:(f + 1) * 128],
                                        rhs=H[:, kk, off:off + n], start=(kk == 0), stop=(kk == 7)),
                                        reads=[w13, H], writes=[pg])
                                for kk in range(8):
                                    k.op("pe", lambda e, kk=kk, f=f, pu=pu, w13=w13: e.matmul(
                                        pu[:, 0:n], lhsT=w13[:, kk, 1, f * 128:(f + 1) * 128],
                                        rhs=H[:, kk, off:off + n], start=(kk == 0), stop=(kk == 7)),
                                        reads=[w13, H], writes=[pu])
                                k.op("act", lambda e, pg=pg, s_=s_: e.activation(out=s_[:, 0:n], in_=pg[:, 0:n],
                                                                                func=AF.Silu),
                                     reads=[pg], writes=[s_])
                                if moe:
                                    k.op("dve", lambda e, pu=pu, s_=s_: e.tensor_tensor(
                                        out=s_[:, 0:n], in0=s_[:, 0:n], in1=pu[:, 0:n], op=ALU.mult),
                                        reads=[s_, pu], writes=[s_])
                                    k.op("dve", lambda e, f=f, act=act, s_=s_, pgw=pgw: e.tensor_tensor(
                                        out=act[:, f, 0:n], in0=s_[:, 0:n], in1=pgw[:, 0:n], op=ALU.mult),
                                        reads=[s_, pgw], writes=[act])
                                else:
                                    k.op("dve", lambda e, f=f, act=act, pu=pu, s_=s_: e.tensor_tensor(
                                        out=act[:, f, 0:n], in0=s_[:, 0:n], in1=pu[:, 0:n], op=ALU.mult),
                                        reads=[s_, pu], writes=[act])
                            for dc in range(8):
                                po = self.ps[4 + dc % 2]
                                for f in range(nf):
                                    k.op("pe", lambda e, f=f, dc=dc, po=po, act=act, w2=w2: e.matmul(
                                        po[:, 0:n], lhsT=w2[:, f, dc * 128:(dc + 1) * 128], rhs=act[:, f, 0:n],
                                        start=(f == 0), stop=(f == nf - 1)), reads=[w2, act], writes=[po])
                                tsel = 0 if t0 < T else 1
                                k.op("dve", lambda e, dc=dc, po=po, tsel=tsel, off=off: e.scalar_tensor_tensor(
                                    out=X[:, dc, off:off + n], in0=po[:, 0:n], scalar=self.mod(l, 5, dc, tsel),
                                    in1=X[:, dc, off:off + n], op0=ALU.mult, op1=ALU.add),
                                    reads=[po, self.MODS, X.sub(bi)], writes=[X.sub(bi)])
                ph2.__exit__(None, None, None)
                for bi, (t0, n) in enumerate(blocks):
                    k.dma("sp", XTv[:, :, t0:t0 + n], X[:, :, t0 - h0:t0 - h0 + n],
                          reads=[X.sub(bi)], writes=[self.XT], join=True, sem=X.sub(bi))


    def load_norm_all(self, l, nidx, ntok, H):
        k = self.k
        XTv = self.XT.ap().rearrange("(c p) t -> p c t", p=128)
        xb = [k.tile([128, 8, 512], F32, "xb%d" % i) for i in range(2)]
        sq = k.tile([128, 8, 512], BF16, "sq")
        rs = k.tile([128, 512], F32, "rs")
        for bi, (t0, n) in enumerate(token_blocks(0, ntok)):
            X = xb[bi % 2]
            k.dma("sp", X[:, :, 0:n], XTv[:, :, t0:t0 + n], reads=[self.XT], writes=[X])
            tsel = 0 if t0 < T else 1
            self.norm_block(_Shift(X, -t0), H, t0, n, l, nidx, tsel, sq, rs, self.ps[7])

    def stage_outproj(self, l, wsrc, OT, ntok):
        k = self.k
        XTv = self.XT.ap().rearrange("(c p) t -> p c t", p=128)
        OTv = OT.ap().rearrange("(c p) t -> p c t", p=128)
        with k.stage():
            W = k.tile([128, 8, D], BF16, "wo")
            self.alloc_stg()
            for kk in range(0, 8, 2):
                self.cast_load(W[:, kk:kk + 2, :], wsrc.rearrange("(kk p) n -> p kk n", p=128)[:, kk:kk + 2, :],
                               "p (a b) -> p a b", W, first=(kk == 0), a=2)
            xb = [k.tile([128, 8, 512], F32, "xb%d" % i) for i in range(2)]
            ob = [k.tile([128, 8, 512], BF16, "ob%d" % i) for i in range(2)]
            for bi, (t0, n) in enumerate(token_blocks(0, ntok)):
                X = xb[bi % 2]
                O = ob[bi % 2]
                tsel = 0 if t0 < T else 1
                k.dma("sp", X[:, :, 0:n], XTv[:, :, t0:t0 + n], reads=[self.XT], writes=[X])
                k.dma("sp", O[:, :, 0:n], OTv[:, :, t0:t0 + n], reads=[OT], writes=[O])
                for dc in range(8):
                    po = self.ps[dc % 4]
                    for kk in range(8):
                        k.op("pe", lambda e, kk=kk, dc=dc, po=po, O=O: e.matmul(
                            po[:, 0:n], lhsT=W[:, kk, dc * 128:(dc + 1) * 128], rhs=O[:, kk, 0:n],
                            start=(kk == 0), stop=(kk == 7)), reads=[W, O], writes=[po])
                    k.op("dve", lambda e, dc=dc, po=po, X=X, tsel=tsel: e.scalar_tensor_tensor(
                        out=X[:, dc, 0:n], in0=po[:, 0:n], scalar=self.mod(l, 2, dc, tsel),
                        in1=X[:, dc, 0:n], op0=ALU.mult, op1=ALU.add), reads=[po, self.MODS, X], writes=[X])
                k.dma("sp", XTv[:, :, t0:t0 + n], X[:, :, 0:n], reads=[X], writes=[self.XT], join=True, sem=X.sub("st"))

    def stage_na(self, l):
        k = self.k
        nc = self.nc
        j = l // 2
        last = (l == DEPTH - 1)
        OT = self.OT
        with k.stage():
            H = k.tile([128, 8, NTOK], BF16, "H")
            with k.stage():
                self.load_norm_all(l, 0, NTOK, H)
            gq = k.tile([128, 1], F32, "gq")
            gk = k.tile([128, 1], F32, "gk")
            with nc.allow_non_contiguous_dma(reason="tiny"):
                for hh in range(2):
                    k.dma("sp", gq[hh * 64:(hh + 1) * 64, :], self.na_q_norm[j].rearrange("(p o) -> p o", o=1),
                          reads=[self.na_q_norm], writes=[gq], join=(hh > 0))
                    k.dma("sp", gk[hh * 64:(hh + 1) * 64, :], self.na_k_norm[j].rearrange("(p o) -> p o", o=1),
                          reads=[self.na_k_norm], writes=[gk], join=(hh > 0))
            k.op("dve", lambda e: e.tensor_scalar(out=gq.ap(), in0=gq.ap(), scalar1=0.125, scalar2=None, op0=ALU.mult),
                 reads=[gq], writes=[gq])
            bd = k.tile([128, 128], BF16, "bd")
            k.op("dve", lambda e: e.memset(bd.ap(), 0.0), writes=[bd])
            for hh in range(2):
                k.op("dve", lambda e, hh=hh: e.memset(bd[hh * 64:(hh + 1) * 64, hh * 64:(hh + 1) * 64], 1.0 / 64),
                     writes=[bd])
            self.alloc_stg()
            wq = [k.tile([128, 8, 3, 128], BF16, "wqkv%d" % i) for i in range(2)]
            QT = k.tile([128, 2, NTOK], BF16, "QT")
            k.op("pool", lambda e: e.memset(QT.ap(), 0.0), writes=[QT])
            KT = k.tile([128, NTOK], BF16, "KT")
            V = k.tile([128, NTOK // 128, 2, 64], BF16, "V")
            OTs = k.tile([128, NTOK], BF16, "OTs")
            BI = k.tile([128, 9, 2, 320], F32, "BI")
            qf = [k.tile([128, 512], F32, "qf%d" % i) for i in range(2)]
            qs = [k.tile([128, 512], BF16, "qs%d" % i) for i in range(2)]
            qr = [k.tile([128, 512], F32, "qr%d" % i) for i in range(2)]
            ET = [k.tile([128, 512], BF16, "ET%d" % i) for i in range(2)]
            rd = [k.tile([128, 256], F32, "rd%d" % i) for i in range(2)]
            w_in = self.na_w_in[j].rearrange("(kk p) n -> p kk n", p=128)
            OTv = OT.ap().rearrange("(c p) t -> p c t", p=128)
            cnt = 0
            for hp in range(getattr(self, "na_hp_limit", 8)):
                w = wq[hp % 2]
                for part in range(3):
                    self.cast_load(w[:, :, part, :], w_in[:, :, part * D + hp * 128: part * D + (hp + 1) * 128],
                                   "p (a b) -> p a b", w, first=(part == 0), a=8)
                if getattr(self, "na_cut", 9) > 2:
                  k.dma("sp", BI.ap().rearrange("p r h q -> p r (h q)"),
                      self.na_bias[j].rearrange("r p (h q) -> p r h q", h=16)[:, :, 2 * hp:2 * hp + 2, :].rearrange("p r h q -> p r (h q)"),
                      reads=[self.na_bias], writes=[BI])
                for part, (dst, gcol) in enumerate(((QT, gq), (KT, gk))):
                    for bi, (t0, n) in enumerate(token_blocks(0, NTOK if getattr(self, "na_cut", 9) > 0 else 0)):
                        pp = self.ps[cnt % 2]
                        pm = self.ps[2 + cnt % 2]
                        f_ = qf[cnt % 2]
                        s_ = qs[cnt % 2]
                        r_ = qr[cnt % 2]
                        cnt += 1
                        for kk in range(8):
                            k.op("pe", lambda e, kk=kk, pp=pp, w=w, part=part: e.matmul(
                                pp[:, 0:n], lhsT=w[:, kk, part, :], rhs=H[:, kk, t0:t0 + n],
                                start=(kk == 0), stop=(kk == 7)), reads=[w, H], writes=[pp])
                        k.op("act", lambda e, pp=pp, s_=s_: e.activation(out=s_[:, 0:n], in_=pp[:, 0:n], func=AF.Square),
                             reads=[pp], writes=[s_])
                        k.op("dve", lambda e, pp=pp, f_=f_: e.tensor_copy(out=f_[:, 0:n], in_=pp[:, 0:n]),
                             reads=[pp], writes=[f_])
                        k.op("pe", lambda e, pm=pm, s_=s_: e.matmul(pm[:, 0:n], lhsT=bd.ap(), rhs=s_[:, 0:n],
                                                                  start=True, stop=True), reads=[bd, s_], writes=[pm])
                        k.op("act", lambda e, pm=pm, r_=r_: e.activation(out=r_[:, 0:n], in_=pm[:, 0:n], func=AF.Sqrt,
                                                                        scale=1.0, bias=self.eps_t.ap()),
                             reads=[pm, self.eps_t], writes=[r_])
                        k.op("dve", lambda e, r_=r_: e.reciprocal(out=r_[:, 0:n], in_=r_[:, 0:n]), reads=[r_], writes=[r_])
                        if part == 0 and not getattr(self, 'na_dbg_full', False):
                            for hl in range(2):
                                hs = slice(hl * 64, (hl + 1) * 64)
                                k.op("dve", lambda e, f_=f_, r_=r_, hs=hs, hl=hl: e.scalar_tensor_tensor(
                                    out=QT[hs, hl, t0:t0 + n], in0=f_[hs, 0:n], scalar=gq[hs, :], in1=r_[hs, 0:n],
                                    op0=ALU.mult, op1=ALU.mult), reads=[f_, r_, gq], writes=[QT])
                        else:
                            dstv = dst[:, 0, :] if part == 0 else dst
                            k.op("dve", lambda e, f_=f_, r_=r_, dst=dstv, gcol=gcol: e.scalar_tensor_tensor(
                                out=dst[:, t0:t0 + n], in0=f_[:, 0:n], scalar=gcol.ap(), in1=r_[:, 0:n],
                                op0=ALU.mult, op1=ALU.mult), reads=[f_, r_, gcol], writes=[dst])
                for ti in range(NTOK // 128 if getattr(self, "na_cut", 9) > 1 else 0):
                    pv = self.ps[4 + ti % 2]
                    for kk in range(8):
                        k.op("pe", lambda e, kk=kk, pv=pv, w=w, ti=ti: e.matmul(
                            pv[:, 0:128], lhsT=H[:, kk, ti * 128:(ti + 1) * 128], rhs=w[:, kk, 2, :],
                            start=(kk == 0), stop=(kk == 7)), reads=[w, H], writes=[pv])
                    eng = "act" if ti % 2 == 0 else "dve"
                    if eng == "act":
                        k.op("act", lambda e, pv=pv, ti=ti: e.copy(out=V[:, ti, :, :].rearrange("p h d -> p (h d)"),
                                                                  in_=pv[:, 0:128]), reads=[pv], writes=[V])
                    else:
                        k.op("dve", lambda e, pv=pv, ti=ti: e.tensor_copy(out=V[:, ti, :, :].rearrange("p h d -> p (h d)"),
                                                                         in_=pv[:, 0:128]), reads=[pv], writes=[V])
                if getattr(self, "na_cut", 9) <= 2:
                    continue
                groups = []
                for r in range(64):
                    rs_ = min(max(r - 4, 0), 56)
                    tb = rs_ // 2
                    nt = 4 if rs_ % 2 == 0 else 5
                    if r < 4:
                        rt = r
                    elif r > 60:
                        rt = r - 56 + 1
                    else:
                        rt = 4 + (rs_ % 2)
                    groups.append((r * 64, 64, [tb + i for i in range(nt)] + [32, 33], rt, nt))
                if not last and getattr(self, "na_cut", 9) > 3:
                    groups.append((T, 256, [32, 33], None, 0))
                for (q0, nq, tiles, rt, nt) in groups:
                    for hl in range(2):
                        pS = self.ps[cnt % 2]
                        pO = self.ps[2 + cnt % 2]
                        et = ET[cnt % 2]
                        rd_ = rd[cnt % 2]
                        cnt += 1
                        hs = slice(hl * 64, (hl + 1) * 64)
                        W_ = len(tiles) * nq
                        for si, tl in enumerate(tiles):
                            k.op("pe", lambda e, si=si, tl=tl, pS=pS, hl=hl: e.matmul(
                                pS[:, si * nq:(si + 1) * nq], lhsT=KT[:, tl * 128:(tl + 1) * 128],
                                rhs=QT[:, hl, q0:q0 + nq], start=True, stop=True), reads=[KT, QT], writes=[pS])
                        if rt is not None:
                            k.op("dve", lambda e, pS=pS, rt=rt, hl=hl, nt=nt: e.tensor_tensor(
                                out=pS[:, 0:nt * 64], in0=pS[:, 0:nt * 64], in1=BI[:, rt, hl, 0:nt * 64], op=ALU.add),
                                reads=[pS, BI], writes=[pS])
                        k.op("act", lambda e, pS=pS, et=et, W_=W_: e.activation(out=et[:, 0:W_], in_=pS[:, 0:W_], func=AF.Exp),
                             reads=[pS], writes=[et])
                        for si, tl in enumerate(tiles):
                            k.op("pe", lambda e, si=si, tl=tl, pO=pO, et=et, hs=hs, hl=hl: e.matmul(
                                pO[hs, 0:nq], lhsT=V[:, tl, hl, :], rhs=et[:, si * nq:(si + 1) * nq],
                                start=(si == 0), stop=(si == len(tiles) - 1)), reads=[V, et], writes=[pO])
                        for si, tl in enumerate(tiles):
                            k.op("pe", lambda e, si=si, pO=pO, et=et, hs=hs: e.matmul(
                                pO[hs, 256:256 + nq], lhsT=self.ones_bf[:, 0:64], rhs=et[:, si * nq:(si + 1) * nq],
                                start=(si == 0), stop=(si == len(tiles) - 1)), reads=[self.ones_bf, et], writes=[pO])
                        k.op("dve", lambda e, pO=pO, rd_=rd_, hs=hs: e.reciprocal(out=rd_[hs, 0:nq], in_=pO[hs, 256:256 + nq]),
                             reads=[pO], writes=[rd_])
                        k.op("dve", lambda e, pO=pO, rd_=rd_, hs=hs: e.tensor_tensor(
                            out=OTs[hs, q0:q0 + nq], in0=pO[hs, 0:nq], in1=rd_[hs, 0:nq], op=ALU.mult),
                            reads=[pO, rd_], writes=[OTs.sub(hl)])
                ntk = T if last else NTOK
                for c0 in range(0, ntk, 1024):
                    c1 = min(ntk, c0 + 1024)
                    k.dma("sp", OTv[:, hp, c0:c1], OTs[:, c0:c1], reads=[OTs.sub(0), OTs.sub(1)], writes=[OT],
                          join=True, sem=OTs.sub("st"))
        self.stage_outproj(l, self.na_w_out[j], OT, T if last else NTOK)


    def stage_gdn(self, l):
        j = l // 2
        if not hasattr(self, "QKVT"):
            k = self.k
            self.QKVT = k.dram("QKVT", [3 * D, NTOK], F32)
            self.GATE = k.dram("GATE", [NTOK, D], BF16)
            self.GB = k.dram("GB", [NTOK, 32], F32)
            self.OD = k.dram("OD", [2, NTOK, D], F32)
        self.gdn_proj(l, j)
        self.gdn_scan(l, j)
        self.gdn_finish(l, j)
        self.stage_outproj(l, self.gdn_w_out[j], self.OT, NTOK)

    def gdn_proj(self, l, j):
        k = self.k
        nc = self.nc
        w_in = self.gdn_w_in[j].rearrange("(kk p) n -> p kk n", p=128)
        QV = self.QKVT.ap().rearrange("(c p) t -> p c t", p=128)
        PADN = NTOK + 4
        with k.stage():
            H = k.tile([128, 8, NTOK], BF16, "H")
            with k.stage():
                self.load_norm_all(l, 0, NTOK, H)
            self.alloc_stg()
            cw = k.tile([128, 24, 3], F32, "cw")
            with nc.allow_non_contiguous_dma(reason="small conv weights"):
                for s_ in range(3):
                    k.dma("sp", cw[:, :, s_], self.gdn_conv_w[j, s_].rearrange("(c p) -> p c", p=128),
                          reads=[self.gdn_conv_w], writes=[cw], join=(s_ > 0))
            wb = [k.tile([128, 8, 128], BF16, "wfc%d" % i) for i in range(2)]
            PT = k.tile([128, PADN], F32, "PT")
            k.op("pool", lambda e: e.memset(PT.ap(), 0.0), writes=[PT])
            cv = [k.tile([128, 512], F32, "cv%d" % i) for i in range(2)]
            sv = [k.tile([128, 512], F32, "sv%d" % i) for i in range(2)]
            sqb = [k.tile([128, 512], BF16, "sqb%d" % i) for i in range(2)]
            rr = [k.tile([128, 512], F32, "rr%d" % i) for i in range(2)]
            cnt = 0
            for fc in range(24):
                w = wb[fc % 2]
                self.cast_load(w.ap(), w_in[:, :, fc * 128:(fc + 1) * 128], "p (a b) -> p a b", w, first=True, a=8)
                for (t0, n) in token_blocks(0, NTOK):
                    pp = self.ps[cnt % 2]
                    cnt += 1
                    for kk in range(8):
                        k.op("pe", lambda e, kk=kk, pp=pp, w=w: e.matmul(
                            pp[:, 0:n], lhsT=w[:, kk, :], rhs=H[:, kk, t0:t0 + n], start=(kk == 0), stop=(kk == 7)),
                            reads=[w, H], writes=[pp])
                    pc = 1 + t0 if t0 < T else 3 + t0
                    k.op("act", lambda e, pp=pp, pc=pc: e.copy(out=PT[:, pc:pc + n], in_=pp[:, 0:n]),
                         reads=[pp], writes=[PT])
                for (t0, n) in token_blocks(0, NTOK):
                    pc = t0 if t0 < T else 2 + t0
                    c_ = cv[cnt % 2]
                    s2 = sv[cnt % 2]
                    q_ = sqb[cnt % 2]
                    r_ = rr[cnt % 2]
                    pm = self.ps[2 + cnt % 2]
                    cnt += 1
                    k.op("dve", lambda e, c_=c_, pc=pc: e.tensor_scalar(
                        out=c_[:, 0:n], in0=PT[:, pc:pc + n], scalar1=cw[:, fc, 0:1], scalar2=None, op0=ALU.mult),
                        reads=[PT, cw], writes=[c_])
                    for s_ in (1, 2):
                        k.op("dve", lambda e, c_=c_, pc=pc, s_=s_: e.scalar_tensor_tensor(
                            out=c_[:, 0:n], in0=PT[:, pc + s_:pc + s_ + n], scalar=cw[:, fc, s_:s_ + 1], in1=c_[:, 0:n],
                            op0=ALU.mult, op1=ALU.add), reads=[PT, cw, c_], writes=[c_])
                    k.op("act", lambda e, c_=c_, s2=s2: e.activation(out=s2[:, 0:n], in_=c_[:, 0:n], func=AF.Silu),
                         reads=[c_], writes=[s2])
                    if fc < 16:
                        k.op("act", lambda e, s2=s2, q_=q_: e.activation(out=q_[:, 0:n], in_=s2[:, 0:n], func=AF.Square),
                             reads=[s2], writes=[q_])
                        k.op("pe", lambda e, pm=pm, q_=q_: e.matmul(pm[:, 0:n], lhsT=self.ones_bf.ap(), rhs=q_[:, 0:n],
                                                                  start=True, stop=True),
                             reads=[self.ones_bf, q_], writes=[pm])
                        k.op("act", lambda e, pm=pm, r_=r_: e.activation(out=r_[:, 0:n], in_=pm[:, 0:n], func=AF.Sqrt,
                                                                        scale=1.0, bias=self.eps_t.ap()),
                             reads=[pm, self.eps_t], writes=[r_])
                        k.op("dve", lambda e, r_=r_: e.reciprocal(out=r_[:, 0:n], in_=r_[:, 0:n]), reads=[r_], writes=[r_])
                        sc_ = (128.0 ** -0.5) if fc < 8 else 1.0
                        k.op("dve", lambda e, r_=r_, s2=s2, sc_=sc_: e.scalar_tensor_tensor(
                            out=s2[:, 0:n], in0=s2[:, 0:n], scalar=sc_, in1=r_[:, 0:n], op0=ALU.mult, op1=ALU.mult),
                            reads=[s2, r_], writes=[s2])
                    k.dma("sp", QV[:, fc, t0:t0 + n], s2[:, 0:n], reads=[s2], writes=[self.QKVT], join=True, sem=s2.sub("st"))
            WG = k.tile([128, 8, D], BF16, "WG")
            for kk in range(0, 8, 2):
                self.cast_load(WG[:, kk:kk + 2, :], w_in[:, kk:kk + 2, 3 * D:4 * D], "p (a b) -> p a b", WG,
                               first=(kk == 0), a=2)
            WAB = k.tile([128, 8, 32], BF16, "WAB")
            with nc.allow_non_contiguous_dma(reason="32-col slice"):
                self.cast_load(WAB.ap(), w_in[:, :, 4 * D:4 * D + 32], "p (a b) -> p a b", WAB, first=True, a=8)
            ALc = k.tile([128, 32], F32, "ALc")
            DTc = k.tile([128, 32], F32, "DTc")
            k.op("dve", lambda e: e.memset(ALc.ap(), 0.0), writes=[ALc])
            k.op("dve", lambda e: e.memset(DTc.ap(), 0.0), writes=[DTc])
            with nc.allow_non_contiguous_dma(reason="tiny broadcast"):
                for d_ in range(2):
                    k.dma("sp", ALc[:, d_ * 16:d_ * 16 + 8], self.gdn_a_log[j, d_].partition_broadcast(128),
                          reads=[self.gdn_a_log], writes=[ALc], join=(d_ > 0))
                    k.dma("sp", DTc[:, d_ * 16:d_ * 16 + 8], self.gdn_dt_bias[j, d_].partition_broadcast(128),
                          reads=[self.gdn_dt_bias], writes=[DTc], join=(d_ > 0))
            k.op("act", lambda e: e.activation(out=ALc.ap(), in_=ALc.ap(), func=AF.Exp), reads=[ALc], writes=[ALc])
            k.op("dve", lambda e: e.tensor_scalar(out=ALc.ap(), in0=ALc.ap(), scalar1=-1.0, scalar2=None, op0=ALU.mult),
                 reads=[ALc], writes=[ALc])
            gt = [k.tile([128, D], BF16, "gt%d" % i) for i in range(2)]
            xa = [k.tile([128, 32], F32, "xa%d" % i) for i in range(2)]
            xb_ = [k.tile([128, 32], F32, "xb_%d" % i) for i in range(2)]
            xc = [k.tile([128, 32], F32, "xc%d" % i) for i in range(2)]
            one_col = self.cst[:, 384:385]
            for ti in range(NTOK // 128):
                g_ = gt[ti % 2]
                for hf in range(2):
                    pg = self.ps[4 + hf]
                    for kk in range(8):
                        k.op("pe", lambda e, kk=kk, pg=pg, hf=hf: e.matmul(
                            pg[:, 0:512], lhsT=H[:, kk, ti * 128:(ti + 1) * 128], rhs=WG[:, kk, hf * 512:(hf + 1) * 512],
                            start=(kk == 0), stop=(kk == 7)), reads=[H, WG], writes=[pg])
                    k.op("act", lambda e, pg=pg, g_=g_, hf=hf: e.activation(out=g_[:, hf * 512:(hf + 1) * 512], in_=pg[:, 0:512],
                                                                           func=AF.Silu), reads=[pg], writes=[g_.sub(hf)])
                k.dma("sp", self.GATE[ti * 128:(ti + 1) * 128, :], g_.ap(), reads=[g_.sub(0), g_.sub(1)], writes=[self.GATE],
                      join=True, sem=g_.sub("st"))
                pa = self.ps[6 + ti % 2]
                a_ = xa[ti % 2]
                b_ = xb_[ti % 2]
                c_ = xc[ti % 2]
                for kk in range(8):
                    k.op("pe", lambda e, kk=kk, pa=pa: e.matmul(
                        pa[:, 0:32], lhsT=H[:, kk, ti * 128:(ti + 1) * 128], rhs=WAB[:, kk, :],
                        start=(kk == 0), stop=(kk == 7)), reads=[H, WAB], writes=[pa])
                k.op("dve", lambda e, pa=pa, a_=a_: e.tensor_tensor(out=a_.ap(), in0=pa[:, 0:32], in1=DTc.ap(), op=ALU.add),
                     reads=[pa, DTc], writes=[a_])
                k.op("dve", lambda e, a_=a_, b_=b_: e.scalar_tensor_tensor(out=b_.ap(), in0=a_.ap(), scalar=-1.0, in1=a_.ap(),
                                                                          op0=ALU.mult, op1=ALU.max), reads=[a_], writes=[b_])
                k.op("act", lambda e, b_=b_: e.activation(out=b_.ap(), in_=b_.ap(), func=AF.Exp, scale=-1.0),
                     reads=[b_], writes=[b_])
                k.op("act", lambda e, b_=b_, c_=c_: e.activation(out=c_.ap(), in_=b_.ap(), func=AF.Ln, bias=one_col, scale=1.0),
                     reads=[b_, self.cst], writes=[c_])
                k.op("dve", lambda e, a_=a_, c_=c_: e.scalar_tensor_tensor(out=c_.ap(), in0=a_.ap(), scalar=0.0, in1=c_.ap(),
                                                                          op0=ALU.max, op1=ALU.add), reads=[a_, c_], writes=[c_])
                k.op("dve", lambda e, c_=c_: e.tensor_tensor(out=c_.ap(), in0=c_.ap(), in1=ALc.ap(), op=ALU.mult),
                     reads=[c_, ALc], writes=[c_])
                k.op("act", lambda e, pa=pa, b_=b_: e.activation(out=b_.ap(), in_=pa[:, 0:32], func=AF.Sigmoid),
                     reads=[pa], writes=[b_])
                cvw = c_.ap().rearrange("p (d a h) -> p d a h", d=2, a=2)
                bvw = b_.ap().rearrange("p (d a h) -> p d a h", d=2, a=2)
                k.op("dve", lambda e, cvw=cvw, bvw=bvw, c_=c_, b_=b_: e.tensor_copy(out=cvw[:, :, 1, :], in_=bvw[:, :, 1, :]),
                     reads=[b_, c_], writes=[c_])
                k.dma("sp", self.GB[ti * 128:(ti + 1) * 128, :], c_.ap(), reads=[c_], writes=[self.GB], join=True,
                      sem=c_.sub("st"))

    def gdn_scan(self, l, j):
        k = self.k
        nc = self.nc
        QV = self.QKVT.ap().rearrange("(a h p) t -> a p h t", a=3, p=128)
        BIG = 30000.0
        with k.stage():
            f4 = lambda nm: k.tile([128, 8, 128], F32, nm)
            ones32 = self.cst[:, 384:512]
            tril = self.cst[:, 128:256]
            triu = self.cst[:, 256:384]
            I8 = f4("I8")
            for h in range(8):
                k.op("pool", lambda e, h=h: e.tensor_copy(out=I8[:, h, :], in_=self.ident), reads=[self.cst], writes=[I8])
            M2s, NM2T, U = [], [], []
            for d in range(2):
                a = k.tile([128, 128], F32, "M2s%d" % d)
                b = k.tile([128, 128], F32, "NM2T%d" % d)
                src_s = triu if d == 0 else tril
                k.op("dve", lambda e, a=a, src_s=src_s: e.tensor_scalar(out=a.ap(), in0=src_s, scalar1=BIG, scalar2=None,
                                                                        op0=ALU.mult), reads=[self.cst], writes=[a])
                src_t = triu if d == 0 else tril
                k.op("dve", lambda e, b=b, src_t=src_t: e.tensor_scalar(out=b.ap(), in0=src_t, scalar1=BIG, scalar2=-BIG,
                                                                        op0=ALU.mult, op1=ALU.add), reads=[self.cst], writes=[b])
                M2s.append(a)
                NM2T.append(b)
                U.append(triu if d == 0 else tril)
            inb = [[f4("in%d_%d" % (a_, i)) for a_ in range(3)] for i in range(2)]
            gbb = [k.tile([128, 32], F32, "gbb%d" % i) for i in range(2)]
            gc = k.tile([128, 8], F32, "gc")
            egl = k.tile([128, 8], F32, "egl")
            ekd = k.tile([128, 8], F32, "ekd")
            bge = k.tile([128, 8], F32, "bge")
            Gd = f4("Gd")
            Dms = f4("Dms")
            DmT = f4("DmT")
            Lb = [f4("L%d" % i) for i in range(2)]
            Mb = [f4("M%d" % i) for i in range(2)]
            R = f4("R")
            kt = f4("kt")
            vb = f4("vb")
            kbg = f4("kbg")
            kd = f4("kd")
            u = f4("u")
            wT = f4("wT")
            vn = f4("vn")
            qg = f4("qg")
            QKd = f4("QKd")
            osb = [k.tile([128, D], F32, "osb%d" % i) for i in range(2)]
            S = [f4("S%d" % d) for d in range(2)]
            self._pp = 0

            def pair():
                p = self._pp
                self._pp = (p + 1) % 4
                return (self.ps[2 * p], self.ps[2 * p + 1])

            def mm8(lhs_fn, rhs_fn, pr, start=True, stop=True, reads=()):
                for h in range(8):
                    bank = pr[h // 4]
                    k.op("pe", lambda e, h=h, bank=bank: e.matmul(
                        bank[:, (h % 4) * 128:(h % 4 + 1) * 128], lhsT=lhs_fn(h), rhs=rhs_fn(h), start=start, stop=stop),
                        reads=list(reads), writes=[bank])

            def evac2(dst, pr, eng0="act", eng1="dve"):
                for hf, eng in ((0, eng0), (1, eng1)):
                    o_ = dst[:, hf * 4:(hf + 1) * 4, :].rearrange("p h d -> p (h d)")
                    if eng == "act":
                        k.op("act", lambda e, o_=o_, hf=hf: e.copy(out=o_, in_=pr[hf].ap()), reads=[pr[hf]], writes=[dst.sub(hf)])
                    else:
                        k.op("dve", lambda e, o_=o_, hf=hf: e.tensor_copy(out=o_, in_=pr[hf].ap()), reads=[pr[hf]],
                             writes=[dst.sub(hf)])

            def both(t):
                return [t.sub(0), t.sub(1)]

            it = 0
            order = {0: [32, 33] + list(range(32)), 1: [33, 32] + list(range(31, -1, -1))}
            ODv = self.OD
            for step in range(34):
                for d in range(2):
                    c = order[d][step]
                    t0 = c * 128
                    ib = inb[it % 2]
                    gb = gbb[it % 2]
                    ob = osb[it % 2]
                    it += 1
                    qT, kT, vT = ib
                    for a_ in range(3):
                        k.dma("sp", ib[a_].ap(), QV[a_, :, :, t0:t0 + 128], reads=[self.QKVT], writes=[ib[a_]])
                    k.dma("sp", gb.ap(), self.GB[t0:t0 + 128, :], reads=[self.GB], writes=[gb])
                    if step == 0:
                        k.op("pool", lambda e, d=d: e.memset(S[d].ap(), 0.0), writes=[S[d]])
                    G = lambda h, gb=gb, d=d: gb[:, d * 16 + h:d * 16 + h + 1]
                    Bt = lambda h, gb=gb, d=d: gb[:, d * 16 + 8 + h:d * 16 + 8 + h + 1]
                    Gall = gb[:, d * 16:d * 16 + 8]
                    Ball = gb[:, d * 16 + 8:d * 16 + 16]
                    pr = pair()
                    k.op("pe", lambda e, pr=pr, d=d, Gall=Gall: e.matmul(pr[0][:, 0:8], lhsT=U[d], rhs=Gall, start=True, stop=True),
                         reads=[self.cst, gb], writes=[pr[0]])
                    k.op("pe", lambda e, pr=pr, Gall=Gall: e.matmul(pr[0][:, 8:16], lhsT=ones32, rhs=Gall, start=True, stop=True),
                         reads=[self.cst, gb], writes=[pr[0]])
                    k.op("dve", lambda e, pr=pr: e.tensor_copy(out=gc.ap(), in_=pr[0][:, 0:8]), reads=[pr[0]], writes=[gc])
                    k.op("dve", lambda e, pr=pr: e.tensor_tensor(out=ekd.ap(), in0=pr[0][:, 8:16], in1=gc.ap(), op=ALU.subtract),
                         reads=[pr[0], gc], writes=[ekd])
                    k.op("act", lambda e: e.activation(out=ekd.ap(), in_=ekd.ap(), func=AF.Exp), reads=[ekd], writes=[ekd])
                    k.op("act", lambda e, pr=pr: e.activation(out=egl.ap(), in_=pr[0][:, 8:16], func=AF.Exp), reads=[pr[0]], writes=[egl])
                    k.op("act", lambda e: e.activation(out=bge.ap(), in_=gc.ap(), func=AF.Exp), reads=[gc], writes=[bge])
                    k.op("dve", lambda e, Ball=Ball: e.tensor_tensor(out=bge.ap(), in0=bge.ap(), in1=Ball, op=ALU.mult),
                         reads=[bge, gb], writes=[bge])
                    for h in range(8):
                        k.op("pool", lambda e, h=h, d=d, G=G: e.tensor_scalar(out=Gd[:, h, :], in0=U[d], scalar1=G(h), scalar2=None,
                                                                              op0=ALU.mult), reads=[self.cst, gb], writes=[Gd.sub(h // 4)])
                    prB = pair()
                    mm8(lambda h: ones32, lambda h: Gd[:, h, :], prB, reads=[self.cst] + both(Gd))
                    for h in range(8):
                        bank = prB[h // 4]
                        sl = slice((h % 4) * 128, (h % 4 + 1) * 128)
                        k.op("dve", lambda e, h=h, bank=bank, sl=sl, d=d: e.scalar_tensor_tensor(
                            out=Dms[:, h, :], in0=bank[:, sl], scalar=gc[:, h:h + 1], in1=M2s[d].ap(),
                            op0=ALU.subtract, op1=ALU.max), reads=[bank, gc, M2s[d]], writes=[Dms])
                        k.op("dve", lambda e, h=h, bank=bank, sl=sl, d=d: e.scalar_tensor_tensor(
                            out=DmT[:, h, :], in0=bank[:, sl], scalar=gc[:, h:h + 1], in1=NM2T[d].ap(),
                            op0=ALU.subtract, op1=ALU.min), reads=[bank, gc, NM2T[d]], writes=[DmT])
                    k.op("act", lambda e: e.activation(out=Dms.ap(), in_=Dms.ap(), func=AF.Exp, scale=-1.0), reads=[Dms], writes=[Dms])
                    k.op("act", lambda e: e.activation(out=DmT.ap(), in_=DmT.ap(), func=AF.Exp), reads=[DmT], writes=[DmT])
                    for hf in range(2):
                        o_ = qg[:, hf * 4:(hf + 1) * 4, :].rearrange("p h d -> p (h d)")
                        k.op("act", lambda e, o_=o_, hf=hf, prB=prB: e.activation(out=o_, in_=prB[hf].ap(), func=AF.Exp),
                             reads=[prB[hf]], writes=[qg.sub(hf)])
                    k.op("pool", lambda e, qT=qT: e.tensor_tensor(out=qg.ap(), in0=qg.ap(), in1=qT.ap(), op=ALU.mult),
                         reads=both(qg) + [qT], writes=both(qg))
                    prK = pair()
                    mm8(lambda h: kT[:, h, :], lambda h: kT[:, h, :], prK, reads=[kT])
                    L, M = Lb[0], Mb[0]
                    for h in range(8):
                        bank = prK[h // 4]
                        sl = slice((h % 4) * 128, (h % 4 + 1) * 128)
                        k.op("dve", lambda e, h=h, bank=bank, sl=sl, Bt=Bt, L=L: e.scalar_tensor_tensor(
                            out=L[:, h, :], in0=bank[:, sl], scalar=Bt(h), in1=Dms[:, h, :], op0=ALU.mult, op1=ALU.mult),
                            reads=[bank, gb, Dms], writes=[L.sub(h // 4)])
                    prM = pair()
                    for h in range(8):
                        bank = prM[h // 4]
                        k.op("pe", lambda e, h=h, bank=bank, L=L: e.transpose(bank[:, (h % 4) * 128:(h % 4 + 1) * 128], L[:, h, :], self.ident),
                             reads=both(L) + [self.cst], writes=[bank])
                    evac2(M, prM)
                    k.op("pool", lambda e, M=M: e.scalar_tensor_tensor(out=R.ap(), in0=M.ap(), scalar=-1.0, in1=I8.ap(),
                                                                       op0=ALU.mult, op1=ALU.add) if False else
                         e.tensor_tensor(out=R.ap(), in0=I8.ap(), in1=M.ap(), op=ALU.subtract),
                         reads=both(M) + [I8], writes=both(R))
                    for lev in range(1, 7):
                        Lp, Mp = Lb[(lev - 1) % 2], Mb[(lev - 1) % 2]
                        Ln_, Mn_ = Lb[lev % 2], Mb[lev % 2]
                        prL = pair()
                        mm8(lambda h: Mp[:, h, :], lambda h: Lp[:, h, :], prL, reads=both(Mp) + both(Lp))
                        evac2(Ln_, prL)
                        if lev < 6:
                            prN = pair()
                            mm8(lambda h: Lp[:, h, :], lambda h: Mp[:, h, :], prN, reads=both(Mp) + both(Lp))
                            evac2(Mn_, prN, "dve", "act")
                        prR = pair()
                        mm8(lambda h: Ln_[:, h, :], lambda h: R[:, h, :], prR, reads=both(Ln_) + both(R))
                        for hf in range(2):
                            o_ = R[:, hf * 4:(hf + 1) * 4, :].rearrange("p h d -> p (h d)")
                            k.op("dve", lambda e, o_=o_, hf=hf, prR=prR: e.tensor_tensor(out=o_, in0=o_, in1=prR[hf].ap(), op=ALU.add),
                                 reads=[prR[hf], R.sub(hf)], writes=[R.sub(hf)])
                    prT = pair()
                    for h in range(8):
                        bank = prT[h // 4]
                        k.op("pe", lambda e, h=h, bank=bank: e.transpose(bank[:, (h % 4) * 128:(h % 4 + 1) * 128], kT[:, h, :], self.ident),
                             reads=[kT, self.cst], writes=[bank])
                    evac2(kt, prT)
                    for h in range(8):
                        k.op("pool", lambda e, h=h: e.tensor_scalar(out=kbg[:, h, :], in0=kt[:, h, :], scalar1=bge[:, h:h + 1],
                                                                    scalar2=None, op0=ALU.mult), reads=both(kt) + [bge], writes=[kbg])
                        k.op("pool", lambda e, h=h: e.tensor_scalar(out=kd[:, h, :], in0=kt[:, h, :], scalar1=ekd[:, h:h + 1],
                                                                    scalar2=None, op0=ALU.mult), reads=both(kt) + [ekd], writes=[kd])
                    prV = pair()
                    for h in range(8):
                        bank = prV[h // 4]
                        k.op("pe", lambda e, h=h, bank=bank: e.transpose(bank[:, (h % 4) * 128:(h % 4 + 1) * 128], vT[:, h, :], self.ident),
                             reads=[vT, self.cst], writes=[bank])
                    for h in range(8):
                        bank = prV[h // 4]
                        sl = slice((h % 4) * 128, (h % 4 + 1) * 128)
                        k.op("act", lambda e, h=h, bank=bank, sl=sl, Bt=Bt: e.activation(out=vb[:, h, :], in_=bank[:, sl], func=AF.Copy,
                                                                                   scale=Bt(h)), reads=[bank, gb], writes=[vb])
                    prU = pair()
                    mm8(lambda h: R[:, h, :], lambda h: vb[:, h, :], prU, reads=both(R) + [vb])
                    evac2(u, prU)
                    prW = pair()
                    mm8(lambda h: kbg[:, h, :], lambda h: R[:, h, :], prW, reads=both(R) + [kbg])
                    evac2(wT, prW, "dve", "act")
                    prQ = pair()
                    mm8(lambda h: kT[:, h, :], lambda h: qT[:, h, :], prQ, reads=[kT, qT])
                    for hf in range(2):
                        o_ = QKd[:, hf * 4:(hf + 1) * 4, :].rearrange("p h d -> p (h d)")
                        i_ = DmT[:, hf * 4:(hf + 1) * 4, :].rearrange("p h d -> p (h d)")
                        k.op("dve", lambda e, o_=o_, i_=i_, hf=hf, prQ=prQ: e.tensor_tensor(out=o_, in0=prQ[hf].ap(), in1=i_, op=ALU.mult),
                             reads=[prQ[hf], DmT], writes=[QKd.sub(hf)])
                    Sd = S[d]
                    prS = pair()
                    mm8(lambda h: wT[:, h, :], lambda h: Sd[:, h, :], prS, reads=both(wT) + [Sd])
                    for hf in range(2):
                        o_ = vn[:, hf * 4:(hf + 1) * 4, :].rearrange("p h d -> p (h d)")
                        i_ = u[:, hf * 4:(hf + 1) * 4, :].rearrange("p h d -> p (h d)")
                        k.op("dve", lambda e, o_=o_, i_=i_, hf=hf, prS=prS: e.tensor_tensor(out=o_, in0=i_, in1=prS[hf].ap(), op=ALU.subtract),
                             reads=[prS[hf], u.sub(hf)], writes=[vn.sub(hf)])
                    prO = pair()
                    for h in range(8):
                        bank = prO[h // 4]
                        sl = slice((h % 4) * 128, (h % 4 + 1) * 128)
                        k.op("pe", lambda e, h=h, bank=bank, sl=sl, Sd=Sd: e.matmul(bank[:, sl], lhsT=qg[:, h, :], rhs=Sd[:, h, :],
                                                                                 start=True, stop=False),
                             reads=both(qg) + [Sd], writes=[bank])
                        k.op("pe", lambda e, h=h, bank=bank, sl=sl: e.matmul(bank[:, sl], lhsT=QKd[:, h, :], rhs=vn[:, h, :],
                                                                           start=False, stop=True),
                             reads=both(QKd) + both(vn), writes=[bank])
                    for hf in range(2):
                        if hf == 0:
                            k.op("act", lambda e, ob=ob, prO=prO: e.copy(out=ob[:, 0:512], in_=prO[0].ap()), reads=[prO[0]], writes=[ob.sub(0)])
                        else:
                            k.op("dve", lambda e, ob=ob, prO=prO: e.tensor_copy(out=ob[:, 512:1024], in_=prO[1].ap()), reads=[prO[1]],
                                 writes=[ob.sub(1)])
                    for hf in range(2):
                        k.dma("sp", ODv[d, t0:t0 + 128, hf * 512:(hf + 1) * 512], ob[:, hf * 512:(hf + 1) * 512],
                              reads=[ob.sub(hf)], writes=[self.OD], join=True, sem=ob.sub("st%d" % hf))
                    prN2 = pair()
                    mm8(lambda h: kd[:, h, :], lambda h: vn[:, h, :], prN2, reads=[kd] + both(vn))
                    for h in range(8):
                        bank = prN2[h // 4]
                        sl = slice((h % 4) * 128, (h % 4 + 1) * 128)
                        k.op("dve", lambda e, h=h, bank=bank, sl=sl, Sd=Sd: e.scalar_tensor_tensor(
                            out=Sd[:, h, :], in0=Sd[:, h, :], scalar=egl[:, h:h + 1], in1=bank[:, sl], op0=ALU.mult, op1=ALU.add),
                            reads=[bank, egl, Sd], writes=[Sd])

    def gdn_finish(self, l, j):
        k = self.k
        nc = self.nc
        OTv = self.OT.ap().rearrange("(c p) t -> p c t", p=128)
        with k.stage():
            NG = k.tile([128, D], F32, "NG")
            with nc.allow_non_contiguous_dma(reason="tiny broadcast"):
                for h in range(8):
                    k.dma("sp", NG[:, h * 128:(h + 1) * 128], self.gdn_norm_g[j].partition_broadcast(128),
                          reads=[self.gdn_norm_g], writes=[NG], join=(h > 0))
            o0 = [k.tile([128, D], F32, "o0_%d" % i) for i in range(2)]
            o1 = [k.tile([128, D], F32, "o1_%d" % i) for i in range(2)]
            gtb = [k.tile([128, D], BF16, "gtb%d" % i) for i in range(2)]
            sq = k.tile([128, D], F32, "sq")
            ss = [k.tile([128, 8], F32, "ss%d" % i) for i in range(2)]
            yT = [k.tile([128, 8, 512], BF16, "yT%d" % i) for i in range(2)]
            ntile = NTOK // 128
            for ti in range(ntile):
                a, b, g_ = o0[ti % 2], o1[ti % 2], gtb[ti % 2]
                s_ = ss[ti % 2]
                grp, gi = ti // 4, ti % 4
                y_ = yT[grp % 2]
                for hf in range(2):
                    sl = slice(hf * 512, (hf + 1) * 512)
                    k.dma("sp", a[:, sl], self.OD[0, ti * 128:(ti + 1) * 128, sl], reads=[self.OD], writes=[a], join=(hf > 0))
                    k.dma("sp", b[:, sl], self.OD[1, ti * 128:(ti + 1) * 128, sl], reads=[self.OD], writes=[b], join=(hf > 0))
                k.dma("sp", g_.ap(), self.GATE[ti * 128:(ti + 1) * 128, :], reads=[self.GATE], writes=[g_])
                k.op("dve", lambda e, a=a, b=b: e.tensor_tensor(out=a.ap(), in0=a.ap(), in1=b.ap(), op=ALU.add), reads=[a, b], writes=[a])
                k.op("act", lambda e, a=a: e.activation(out=sq.ap(), in_=a.ap(), func=AF.Square), reads=[a], writes=[sq])
                k.op("dve", lambda e, s_=s_: e.tensor_reduce(out=s_.ap(), in_=sq.ap().rearrange("p (h d) -> p h d", h=8),
                                                             op=ALU.add, axis=mybir.AxisListType.X), reads=[sq], writes=[s_])
                k.op("act", lambda e, s_=s_: e.activation(out=s_.ap(), in_=s_.ap(), func=AF.Sqrt, scale=1.0 / 128, bias=self.eps_t.ap()),
                     reads=[s_, self.eps_t], writes=[s_])
                k.op("dve", lambda e, s_=s_: e.reciprocal(out=s_.ap(), in_=s_.ap()), reads=[s_], writes=[s_])
                for h in range(8):
                    eng = "pool" if h % 2 else "dve"
                    k.op(eng, lambda e, h=h, a=a, s_=s_: e.scalar_tensor_tensor(
                        out=a[:, h * 128:(h + 1) * 128], in0=a[:, h * 128:(h + 1) * 128], scalar=s_[:, h:h + 1],
                        in1=NG[:, h * 128:(h + 1) * 128], op0=ALU.mult, op1=ALU.mult) if eng == "dve" else
                        e.tensor_scalar(out=a[:, h * 128:(h + 1) * 128], in0=a[:, h * 128:(h + 1) * 128], scalar1=s_[:, h:h + 1],
                                        scalar2=None, op0=ALU.mult), reads=[a, s_, NG], writes=[a])
                    if eng == "pool":
                        k.op("pool", lambda e, h=h, a=a: e.tensor_tensor(out=a[:, h * 128:(h + 1) * 128], in0=a[:, h * 128:(h + 1) * 128],
                                                                        in1=NG[:, h * 128:(h + 1) * 128], op=ALU.mult),
                             reads=[a, NG], writes=[a])
                k.op("dve", lambda e, a=a, g_=g_: e.tensor_tensor(out=a.ap(), in0=a.ap(), in1=g_.ap(), op=ALU.mult), reads=[a, g_], writes=[a])
                for hf in range(2):
                    bank = self.ps[(ti % 2) * 2 + hf]
                    for c4 in range(4):
                        c = hf * 4 + c4
                        k.op("pe", lambda e, c=c, c4=c4, bank=bank, a=a: e.transpose(bank[:, c4 * 128:(c4 + 1) * 128], a[:, c * 128:(c + 1) * 128],
                                                                                    self.ident), reads=[a, self.cst], writes=[bank])
                    o_ = y_[:, hf * 4:(hf + 1) * 4, gi * 128:(gi + 1) * 128]
                    if hf == 0:
                        k.op("act", lambda e, o_=o_, bank=bank, y_=y_: e.copy(out=o_, in_=bank.ap().rearrange("p (c t) -> p c t", t=128)),
                             reads=[bank], writes=[y_])
                    else:
                        k.op("dve", lambda e, o_=o_, bank=bank, y_=y_: e.tensor_copy(out=o_, in_=bank.ap().rearrange("p (c t) -> p c t", t=128)),
                             reads=[bank], writes=[y_])
                if gi == 3 or ti == ntile - 1:
                    nn = (gi + 1) * 128
                    k.dma("sp", OTv[:, :, grp * 512:grp * 512 + nn], y_[:, :, 0:nn], reads=[y_], writes=[self.OT], join=True,
                          sem=y_.sub("st"))


class _Shift:
    def __init__(self, t, sh):
        self.t = t
        self.d = t.d
        self.sh = sh

    def __getitem__(self, kk):
        a, b, c = kk
        c = slice(c.start + self.sh, c.stop + self.sh)
        return self.t[a, b, c]


class _SubView:
    def __init__(self, t, key):
        self.t = t
        self.d = t.sub(key)

    def __getitem__(self, kk):
        return self.t[kk]


KB._norm_orig = KB._norm


def _norm2(ds):
    out = []
    for d in ds:
        if d is None:
            continue
        if isinstance(d, (Tl, _SubView, _Shift)):
            out.append(d.d)
        else:
            out.append(d)
    return out


KB._norm = staticmethod(_norm2)


def build(layers=(0, 1, 2, 3), stages=None):
    nc = bass.Bass("TRN2", target_bir_lowering=False)
    P = Prog(nc, layers)
    P.stage_init()
    P.stage_in_transpose()
    for l in layers:
        if stages is None or "mix" in stages:
            if l % 2 == 0:
                P.stage_gdn(l)
            else:
                P.stage_na(l)
        if stages is None or "ffn" in stages:
            P.stage_ffn(l, moe=(l % 2 == 1))
    P.stage_out_transpose()
    P.k.barrier()
    return nc, P


def make_consts():
    c = np.zeros((128, 512 + 1024), np.float32)
    for e in range(8):
        c[e, 512 + e * 128:512 + (e + 1) * 128] = 1.0
    c[:, 0:128] = np.eye(128, dtype=np.float32)
    c[:, 128:256] = np.tril(np.ones((128, 128), np.float32))
    c[:, 256:384] = np.triu(np.ones((128, 128), np.float32))
    c[:, 384:512] = 1.0
    return c


def make_na_bias(rpb):
    rpb = np.asarray(rpb, np.float32)
    nl = rpb.shape[0]
    out = np.full((nl, 9, 128, 16, 320), -30000.0, np.float32)
    p = np.arange(128)
    q = np.arange(64)
    kc = p % 64
    wstart = np.clip(q - 8, 0, 48)
    valid_c = (kc[:, None] >= wstart[None, :]) & (kc[:, None] < wstart[None, :] + 16)
    dc_idx = np.clip(kc[:, None] - q[None, :] + 15, 0, 30)
    for rt in range(9):
        if rt < 4:
            r, rs_ = rt, 0
        elif rt == 4:
            r, rs_ = 8, 4
        elif rt == 5:
            r, rs_ = 9, 5
        else:
            r, rs_ = 56 + rt - 1, 56
        base = rs_ - rs_ % 2
        nt = 4 if rs_ % 2 == 0 else 5
        for slot in range(nt):
            grow = base + 2 * slot + p // 64
            jw = grow - rs_
            valid = valid_c & ((jw >= 0) & (jw < 8))[:, None]
            dr_idx = np.clip(grow - r + 7, 0, 14)
            vals = rpb[:, :, dr_idx[:, None], dc_idx]
            vals = np.transpose(vals, (0, 2, 1, 3))
            blk = out[:, rt, :, :, slot * 64:(slot + 1) * 64]
            out[:, rt, :, :, slot * 64:(slot + 1) * 64] = np.where(valid[None, :, None, :], vals, blk)
    return out.reshape(nl, 9, 128, 16 * 320)


def make_in_maps(P, inp, shared):
    in_maps = []
    for b in range(NCORES):
        m = {n: v for n, v in shared.items() if n in P.exts}
        m["x"] = np.ascontiguousarray(inp["x"][b])
        m["ctx"] = np.ascontiguousarray(inp["ctx"][b])
        m["cvec"] = np.ascontiguousarray(np.stack([inp["c"][b], inp["c_ctx"]], 0))
        in_maps.append(m)
    return in_maps


def make_shared(inp):
    shared = {n: np.ascontiguousarray(inp[n], dtype=np.float32) for n in (
        "ada_w", "ada_b", "norm1_g", "norm2_g", "gdn_w_in", "gdn_conv_w", "gdn_a_log", "gdn_dt_bias",
        "gdn_norm_g", "gdn_w_out", "na_w_in", "na_q_norm", "na_k_norm", "na_w_out", "ffn_w13", "ffn_w2",
        "moe_router", "moe_w13", "moe_w2")}
    shared["consts"] = make_consts()
    shared["na_bias"] = make_na_bias(inp["na_rpb"])
    return shared


def kernel(**inp):
    nc, P = build()
    shared = make_shared(inp)
    in_maps = make_in_maps(P, inp, shared)
    res = run_bass_kernel_spmd(nc, in_maps, core_ids=list(range(NCORES)))
    return np.stack([r["y"] for r in res.results], 0)
```

```python
import numpy as np
from contextlib import ExitStack
import concourse.bass as bass
import concourse.mybir as mybir
from concourse.bass_utils import run_bass_kernel_spmd

F32 = mybir.dt.float32
BF16 = mybir.dt.bfloat16
AF = mybir.ActivationFunctionType
ALU = mybir.AluOpType

D = 1024
T = 4096
NCTX = 256
NTOK = T + NCTX
DEPTH = 4
NCORES = 8
EPS = 1e-6
FF_DENSE = 2816
FF_EXPERT = 3584
NEXP = 8


class Dep:
    __slots__ = ("w", "r", "dsem", "excl")

    def __init__(self):
        self.w = {}
        self.r = {}
        self.dsem = None
        self.excl = False


class Tl:
    def __init__(self, t, is_dram=False):
        self.t = t
        self.d = Dep()
        self.subs = {}
        self.is_dram = is_dram

    def __getitem__(self, k):
        if self.is_dram:
            return self.t.ap()[k]
        return self.t[k]

    def ap(self):
        return self.t.ap() if self.is_dram else self.t[:]

    def sub(self, key):
        if key not in self.subs:
            self.subs[key] = Dep()
        return self.subs[key]


class KB:
    def __init__(self, nc):
        self.nc = nc
        self.E = {"pe": nc.tensor, "act": nc.scalar, "dve": nc.vector, "pool": nc.gpsimd, "sp": nc.sync}
        self.es = ExitStack()
        self.sems = []
        self.csem = {}
        for e in self.E:
            self.csem[e] = self._newsem("c_" + e)
        self.cnt = {e: 0 for e in self.E}
        self.known = {e: {} for e in self.E}
        self.dfree = [self._newsem("d%d" % i) for i in range(90)]
        self.dval = {}
        self.dused = []
        self.stage_deps = []
        self.stage_es = None
        self.uid = 0
        self.ninstr = 0

    def _newsem(self, name):
        s = self.es.enter_context(self.nc.semaphore(name))
        self.sems.append(s)
        return len(self.sems) - 1

    def tile(self, shape, dtype, name=None, persistent=False):
        self.uid += 1
        name = (name or "t") + "_%d" % self.uid
        es = self.es if persistent else self.stage_es
        t = es.enter_context(self.nc.sbuf_tensor(name, list(shape), dtype))
        return Tl(t)

    def dram(self, name, shape, dtype, kind="Internal"):
        t = self.nc.dram_tensor(name, list(shape), dtype, kind=kind)
        return Tl(t, is_dram=True)

    def _wait(self, e, tok):
        if tok is None:
            return
        idx, val = tok
        if e == "pe" and idx == self.csem["pe"]:
            return
        if self.known[e].get(idx, 0) >= val:
            return
        self.E[e].wait_ge(self.sems[idx], val)
        self.known[e][idx] = val

    def _deps_wait(self, e, reads, writes, join=False):
        for d in reads:
            for tok in d.w.items():
                self._wait(e, tok)
            if d.excl:
                for tok in d.r.items():
                    if tok[0] != self.csem.get(e):
                        self._wait(e, tok)
        for d in writes:
            if not join:
                for tok in d.w.items():
                    self._wait(e, tok)
            for tok in d.r.items():
                self._wait(e, tok)

    @staticmethod
    def _norm(ds):
        out = []
        for d in ds:
            if d is None:
                continue
            out.append(d.d if isinstance(d, Tl) else d)
        return out

    def op(self, e, fn, reads=(), writes=()):
        reads = self._norm(reads)
        writes = self._norm(writes)
        self._deps_wait(e, reads, writes)
        ins = fn(self.E[e])
        self.cnt[e] += 1
        self.ninstr += 1
        ins.then_inc(self.sems[self.csem[e]], 1)
        tok = (self.csem[e], self.cnt[e])
        for d in reads:
            if d.r.get(tok[0], 0) < tok[1]:
                d.r[tok[0]] = tok[1]
        for d in writes:
            d.w = {tok[0]: tok[1]}
            d.r = {}
        return ins

    def dma(self, q, out, in_, reads=(), writes=(), join=False, sem=None, **kw):
        reads = self._norm(reads)
        writes = self._norm(writes)
        assert len(writes) == 1
        wd = writes[0]
        self._deps_wait(q, reads, writes, join=join)
        sd = wd if sem is None else self._norm([sem])[0]
        if sd.dsem is None:
            sd.dsem = self.dfree.pop()
            self.dused.append(sd)
        idx = sd.dsem
        self.dval[idx] = self.dval.get(idx, 0) + 16
        ins = self.E[q].dma_start(out=out, in_=in_, **kw)
        ins.then_inc(self.sems[idx], 16)
        self.ninstr += 1
        tok = (idx, self.dval[idx])
        for d in reads:
            if d.r.get(idx, 0) < tok[1]:
                d.r[idx] = tok[1]
        if join:
            wd.w[idx] = tok[1]
        else:
            wd.w = {idx: tok[1]}
            wd.r = {}
        return ins

    def barrier(self):
        toks = [(self.csem[e], self.cnt[e]) for e in self.E if self.cnt[e] > 0]
        toks += [(idx, v) for idx, v in self.dval.items()]
        for e in self.E:
            for tok in toks:
                self._wait(e, tok)
        for d in self.dused:
            self.dfree.append(d.dsem)
            d.dsem = None
        self.dused = []

    class _Stage:
        def __init__(self, kb):
            self.kb = kb

        def __enter__(self):
            self.prev = self.kb.stage_es
            self.kb.stage_es = ExitStack()
            self.kb.stage_es.__enter__()
            return self.kb

        def __exit__(self, *a):
            self.kb.barrier()
            self.kb.stage_es.__exit__(*a)
            self.kb.stage_es = self.prev
            return False

    def stage(self):
        return KB._Stage(self)


def token_blocks(n0, n1, blk=512):
    out = []
    t = n0
    while t < n1:
        b = min(blk, n1 - t)
        out.append((t, b))
        t += b
    return out


class Prog:
    def __init__(self, nc, layers=(0, 1, 2, 3), debug=False):
        self.nc = nc
        self.k = KB(nc)
        k = self.k
        self.layers = layers
        self.ext_shapes = {
            "x": [T, D], "ctx": [NCTX, D], "cvec": [2, D], "ada_w": [DEPTH, D, 6 * D], "ada_b": [DEPTH, 6 * D],
            "norm1_g": [DEPTH, D], "norm2_g": [DEPTH, D], "gdn_w_in": [2, D, 4 * D + 32],
            "gdn_conv_w": [2, 3, 3 * D], "gdn_a_log": [2, 2, 8], "gdn_dt_bias": [2, 2, 8],
            "gdn_norm_g": [2, 128], "gdn_w_out": [2, D, D], "na_w_in": [2, D, 3 * D], "na_q_norm": [2, 64],
            "na_k_norm": [2, 64], "na_bias": [2, 9, 128, 16 * 320], "na_w_out": [2, D, D],
            "ffn_w13": [2, D, 2 * FF_DENSE], "ffn_w2": [2, FF_DENSE, D], "moe_router": [2, D, NEXP],
            "moe_w13": [2, NEXP, D, 2 * FF_EXPERT], "moe_w2": [2, NEXP, FF_EXPERT, D],
            "consts": [128, 4 * 128 + 1024]}
        self.exts = {}
        self.y = k.dram("y", [T, D], F32, kind="ExternalOutput")
        self.XT = k.dram("XT", [D, NTOK], F32)
        self.OT = k.dram("OT", [D, NTOK], BF16)
        self.cst = k.tile([128, 4 * 128], F32, "cst", persistent=True)
        self.ones_bf = k.tile([128, 128], BF16, "ones_bf", persistent=True)
        self.ident = self.cst[:, 0:128]
        self.MODS = k.tile([128, DEPTH * 6 * 8 * 2], F32, "mods", persistent=True)
        self.GG = k.tile([128, DEPTH * 2 * 8 * 2], F32, "gg", persistent=True)
        self.eps_t = k.tile([128, 1], F32, "eps", persistent=True)
        self.ps = []
        for b in range(8):
            t = k.es.enter_context(nc.psum_tensor("ps%d" % b, [128, 512], F32))
            pt_ = Tl(t)
            pt_.d.excl = True
            self.ps.append(pt_)

    def __getattr__(self, name):
        shapes = self.__dict__.get("ext_shapes", {})
        if name in shapes:
            if name not in self.exts:
                self.exts[name] = self.k.dram(name, shapes[name], F32, kind="ExternalInput")
            return self.exts[name]
        raise AttributeError(name)

    STG = 2048

    def alloc_stg(self):
        self.stg = [self.k.tile([128, self.STG], F32, "stg%d" % i) for i in range(2)]
        self.stg_i = 0

    def cast_load(self, dst, src, pat, wdep, first=True, **dims):
        k = self.k
        st = self.stg[self.stg_i % 2]
        self.stg_i += 1
        n = 1
        for d_ in dst.shape[1:]:
            n *= d_
        assert n <= self.STG
        view = st[:, 0:n]
        if pat is not None:
            view = view.rearrange(pat, **dims)
        k.dma("sp", view, src, reads=[self.consts], writes=[st])
        deps_w = [wdep]
        k.op("pool", lambda e: e.tensor_copy(out=dst, in_=view), reads=[st], writes=deps_w) if first else \
            self._join_op("pool", lambda e: e.tensor_copy(out=dst, in_=view), [st], wdep)

    def _join_op(self, eng, fn, reads, wdep):
        k = self.k
        wd = k._norm([wdep])[0]
        reads_n = k._norm(reads)
        k._deps_wait(eng, reads_n, [wd], join=True)
        ins = fn(k.E[eng])
        k.cnt[eng] += 1
        k.ninstr += 1
        ins.then_inc(k.sems[k.csem[eng]], 1)
        tok = (k.csem[eng], k.cnt[eng])
        for d in reads_n:
            if d.r.get(tok[0], 0) < tok[1]:
                d.r[tok[0]] = tok[1]
        wd.w[tok[0]] = tok[1]

    def mod(self, l, m, c, t):
        o = ((l * 6 + m) * 8 + c) * 2 + t
        return self.MODS[:, o:o + 1]

    def gg(self, l, n, c, t):
        o = ((l * 2 + n) * 8 + c) * 2 + t
        return self.GG[:, o:o + 1]

    def stage_init(self):
        k = self.k
        nc = self.nc
        with k.stage():
            k.dma("sp", self.cst.ap(), self.consts[:, 0:512], reads=[self.consts], writes=[self.cst])
            k.op("dve", lambda e: e.tensor_copy(out=self.ones_bf.ap(), in_=self.cst[:, 384:512]),
                 reads=[self.cst], writes=[self.ones_bf])
            k.op("dve", lambda e: e.memset(self.eps_t.ap(), EPS), writes=[self.eps_t])
            craw = k.tile([128, 2, 8], F32, "craw")
            sc = k.tile([128, 8, 2], F32, "sc")
            with nc.allow_non_contiguous_dma(reason="tiny"):
                k.dma("sp", craw.ap(), self.cvec.ap().rearrange("t (c p) -> p t c", p=128),
                      reads=[self.cvec], writes=[craw])
            k.op("act", lambda e: e.activation(out=sc.ap().rearrange("p c t -> p t c"), in_=craw.ap(), func=AF.Silu),
                 reads=[craw], writes=[sc])
            ab = k.tile([128, DEPTH, 48], F32, "adab")
            g1 = k.tile([128, 2, DEPTH, 8], F32, "ng")
            with nc.allow_non_contiguous_dma(reason="tiny"):
                k.dma("sp", ab.ap(), self.ada_b.ap().rearrange("l (o p) -> p l o", p=128),
                      reads=[self.ada_b], writes=[ab])
                k.dma("sp", g1[:, 0, :, :], self.norm1_g.ap().rearrange("l (c p) -> p l c", p=128),
                      reads=[self.norm1_g], writes=[g1.sub(0)])
                k.dma("sp", g1[:, 1, :, :], self.norm2_g.ap().rearrange("l (c p) -> p l c", p=128),
                      reads=[self.norm2_g], writes=[g1.sub(1)])
            NW = 1536
            wb = [k.tile([128, 8, NW], F32, "adaw%d" % i) for i in range(2)]
            it = 0
            for l in range(DEPTH):
                pst = self.ps[l % 2]
                for q in range(6 * D // NW):
                    w = wb[it % 2]
                    it += 1
                    k.dma("sp", w.ap(),
                          self.ada_w[l].rearrange("(kk p) n -> p kk n", p=128)[:, :, q * NW:(q + 1) * NW],
                          reads=[self.ada_w], writes=[w])
                    for oc in range(NW // 128):
                        occ = q * (NW // 128) + oc
                        for kk in range(8):
                            k.op("pe", lambda e, kk=kk, oc=oc, occ=occ, w=w, pst=pst: e.matmul(
                                pst[:, occ * 2:occ * 2 + 2], lhsT=w[:, kk, oc * 128:(oc + 1) * 128],
                                rhs=sc[:, kk, :], start=(kk == 0), stop=(kk == 7)),
                                reads=[w, sc], writes=[pst])
                mv = self.MODS[:, l * 96:(l + 1) * 96].rearrange("p (o t) -> p o t", t=2)
                for t in range(2):
                    k.op("dve", lambda e, t=t, mv=mv, pst=pst, l=l: e.tensor_tensor(
                        out=mv[:, :, t], in0=pst[:, 0:96].rearrange("p (o t) -> p o t", t=2)[:, :, t],
                        in1=ab[:, l, :], op=ALU.add), reads=[pst, ab], writes=[self.MODS])
                for n in range(2):
                    m = 1 if n == 0 else 4
                    for t in range(2):
                        o = (l * 6 + m) * 16
                        src = self.MODS[:, o:o + 16].rearrange("p (c t) -> p c t", t=2)[:, :, t]
                        og = (l * 2 + n) * 16
                        dst = self.GG[:, og:og + 16].rearrange("p (c t) -> p c t", t=2)[:, :, t]
                        k.op("dve", lambda e, src=src, dst=dst, l=l, n=n: e.scalar_tensor_tensor(
                            out=dst, in0=src, scalar=1.0, in1=g1[:, n, l, :], op0=ALU.add, op1=ALU.mult),
                            reads=[self.MODS, g1.sub(0), g1.sub(1)], writes=[self.GG])

    def stage_in_transpose(self):
        k = self.k
        XTv = self.XT.ap().rearrange("(c p) t -> p c t", p=128)
        with k.stage():
            xin = [k.tile([128, D], F32, "xin%d" % i) for i in range(2)]
            xo = [k.tile([128, 8, 128], F32, "xo%d" % i) for i in range(2)]
            for ti in range(NTOK // 128):
                src = self.x[ti * 128:(ti + 1) * 128, :] if ti < T // 128 else \
                    self.ctx[(ti - T // 128) * 128:(ti - T // 128 + 1) * 128, :]
                xi = xin[ti % 2]
                o = xo[ti % 2]
                k.dma("sp", xi.ap(), src, reads=[self.x], writes=[xi])
                for half in range(2):
                    pst = self.ps[(ti % 2) * 2 + half]
                    for c4 in range(4):
                        c = half * 4 + c4
                        k.op("pe", lambda e, c=c, c4=c4, pst=pst, xi=xi: e.transpose(
                            pst[:, c4 * 128:(c4 + 1) * 128], xi[:, c * 128:(c + 1) * 128], self.ident),
                            reads=[xi, self.cst], writes=[pst])
                    eng = "act" if half == 0 else "dve"
                    if eng == "act":
                        k.op("act", lambda e, pst=pst, o=o, half=half: e.copy(
                            out=o[:, half * 4:(half + 1) * 4, :], in_=pst.ap().rearrange("p (c t) -> p c t", t=128)),
                            reads=[pst], writes=[o.sub(half)])
                    else:
                        k.op("dve", lambda e, pst=pst, o=o, half=half: e.tensor_copy(
                            out=o[:, half * 4:(half + 1) * 4, :], in_=pst.ap().rearrange("p (c t) -> p c t", t=128)),
                            reads=[pst], writes=[o.sub(half)])
                k.dma("sp", XTv[:, :, ti * 128:(ti + 1) * 128], o.ap(),
                      reads=[o.sub(0), o.sub(1)], writes=[self.XT], join=True, sem=o.sub("st"))

    def stage_out_transpose(self, debug_ctx=False):
        k = self.k
        XTv = self.XT.ap().rearrange("(c p) t -> p c t", p=128)
        if debug_ctx:
            self.yc = k.dram("yc", [NCTX, D], F32, kind="ExternalOutput")
        with k.stage():
            xin = [k.tile([128, 8, 128], F32, "oin%d" % i) for i in range(2)]
            xo = [k.tile([128, D], F32, "oo%d" % i) for i in range(2)]
            for ti in range((NTOK if debug_ctx else T) // 128):
                xi = xin[ti % 2]
                o = xo[ti % 2]
                k.dma("sp", xi.ap(), XTv[:, :, ti * 128:(ti + 1) * 128], reads=[self.XT], writes=[xi])
                for half in range(2):
                    pst = self.ps[(ti % 2) * 2 + half]
                    for c4 in range(4):
                        c = half * 4 + c4
                        k.op("pe", lambda e, c=c, c4=c4, pst=pst, xi=xi: e.transpose(
                            pst[:, c4 * 128:(c4 + 1) * 128], xi[:, c, :], self.ident),
                            reads=[xi, self.cst], writes=[pst])
                    if half == 0:
                        k.op("act", lambda e, pst=pst, o=o, half=half: e.copy(
                            out=o[:, half * 512:(half + 1) * 512], in_=pst.ap()),
                            reads=[pst], writes=[o.sub(half)])
                    else:
                        k.op("dve", lambda e, pst=pst, o=o, half=half: e.tensor_copy(
                            out=o[:, half * 512:(half + 1) * 512], in_=pst.ap()),
                            reads=[pst], writes=[o.sub(half)])
                dsto = self.y[ti * 128:(ti + 1) * 128, :] if ti < T // 128 else \
                    self.yc[(ti - T // 128) * 128:(ti - T // 128 + 1) * 128, :]
                k.dma("sp", dsto, o.ap(),
                      reads=[o.sub(0), o.sub(1)], writes=[self.y], join=True, sem=o.sub("st"))

    def norm_block(self, X, h, off, n, l, nidx, tsel, sq, rs, ps_ss, h32=None):
        k = self.k
        m_shift = 0 if nidx == 0 else 3
        k.op("act", lambda e: e.activation(out=sq[:, :, 0:n], in_=X[:, :, off:off + n], func=AF.Square),
             reads=[X], writes=[sq])
        for c in range(8):
            k.op("pe", lambda e, c=c: e.matmul(ps_ss[:, 0:n], lhsT=self.ones_bf.ap(), rhs=sq[:, c, 0:n],
                                               start=(c == 0), stop=(c == 7)),
                 reads=[sq, self.ones_bf], writes=[ps_ss])
        k.op("act", lambda e: e.activation(out=rs[:, 0:n], in_=ps_ss[:, 0:n], func=AF.Sqrt,
                                           scale=1.0 / D, bias=self.eps_t.ap()),
             reads=[ps_ss, self.eps_t], writes=[rs])
        k.op("dve", lambda e: e.reciprocal(out=rs[:, 0:n], in_=rs[:, 0:n]), reads=[rs], writes=[rs])
        for c in range(8):
            if h32 is not None:
                k.op("dve", lambda e, c=c: e.scalar_tensor_tensor(
                    out=h32[:, c, 0:n], in0=X[:, c, off:off + n], scalar=self.gg(l, nidx, c, tsel),
                    in1=rs[:, 0:n], op0=ALU.mult, op1=ALU.mult), reads=[X, rs, self.GG], writes=[h32])
                k.op("act", lambda e, c=c: e.activation(
                    out=h32[:, c, 0:n], in_=h32[:, c, 0:n], func=AF.Identity,
                    bias=self.mod(l, m_shift, c, tsel), scale=1.0), reads=[h32, self.MODS], writes=[h32])
                k.op("pool", lambda e, c=c: e.tensor_copy(out=h[:, c, off:off + n], in_=h32[:, c, 0:n]),
                     reads=[h32], writes=[h])
            else:
                k.op("dve", lambda e, c=c: e.scalar_tensor_tensor(
                    out=h[:, c, off:off + n], in0=X[:, c, off:off + n], scalar=self.gg(l, nidx, c, tsel),
                    in1=rs[:, 0:n], op0=ALU.mult, op1=ALU.mult), reads=[X, rs, self.GG], writes=[h])
                k.op("act", lambda e, c=c: e.activation(
                    out=h[:, c, off:off + n], in_=h[:, c, off:off + n], func=AF.Identity,
                    bias=self.mod(l, m_shift, c, tsel), scale=1.0), reads=[h, self.MODS], writes=[h])

    def stage_ffn(self, l, moe):
        k = self.k
        nc = self.nc
        j = l // 2
        last = (l == DEPTH - 1)
        ntok = T if last else NTOK
        FF = FF_EXPERT if moe else FF_DENSE
        nfc = FF // 128
        FG = 4
        fgroups = [(f0, min(FG, nfc - f0)) for f0 in range(0, nfc, FG)]
        nexp = NEXP if moe else 1
        halves = [(0, ntok // 2), (ntok // 2, ntok)]
        XTv = self.XT.ap().rearrange("(c p) t -> p c t", p=128)
        for (h0, h1) in halves:
            nh = h1 - h0
            with k.stage():
                X = k.tile([128, 8, nh], F32, "X")
                H = k.tile([128, 8, nh], BF16, "H")
                GW = k.tile([8, nh], F32, "GW") if moe else None
                sel = k.tile([8, NEXP * 128], F32, "sel") if moe else None
                ph1 = k.stage()
                ph1.__enter__()
                sq = k.tile([128, 8, 512], BF16, "sq")
                rs = k.tile([128, 512], F32, "rs")
                blocks = []
                for (t0, n) in token_blocks(h0, h1):
                    if t0 < T < t0 + n:
                        blocks.append((t0, T - t0))
                        blocks.append((T, t0 + n - T))
                    else:
                        blocks.append((t0, n))
                for bi, (t0, n) in enumerate(blocks):
                    k.dma("sp", X[:, :, t0 - h0:t0 - h0 + n], XTv[:, :, t0:t0 + n],
                          reads=[self.XT], writes=[X.sub(bi)])
                if moe:
                    h32 = k.tile([128, 8, 512], F32, "h32")
                    wr = k.tile([128, 8, NEXP], F32, "wr")
                    with nc.allow_non_contiguous_dma(reason="small router weight"):
                        k.dma("sp", wr.ap(), self.moe_router[j].rearrange("(kk p) e -> p kk e", p=128),
                              reads=[self.moe_router], writes=[wr])
                    k.dma("sp", sel.ap(), self.consts[0:8, 512:512 + NEXP * 128], reads=[self.consts], writes=[sel])
                    lg = k.tile([128, 8], F32, "lg")
                    r1 = k.tile([128, 8], F32, "r1")
                    r2 = k.tile([128, 8], F32, "r2")
                    m1 = k.tile([128, 1], F32, "m1")
                    m2 = k.tile([128, 1], F32, "m2")
                    gwt = k.tile([128, 8], F32, "gwt")
                for bi, (t0, n) in enumerate(blocks):
                    tsel = 0 if t0 < T else 1
                    off = t0 - h0
                    self.norm_block(_SubView(X, bi), H, off, n, l, 1, tsel, sq, rs, self.ps[0],
                                    h32=(h32 if moe else None))
                    if moe:
                        for s in range(n // 128):
                            pl = self.ps[1]
                            for kk in range(8):
                                k.op("pe", lambda e, kk=kk, s=s, pl=pl: e.matmul(
                                    pl[:, 0:8], lhsT=h32[:, kk, s * 128:(s + 1) * 128], rhs=wr[:, kk, :],
                                    start=(kk == 0), stop=(kk == 7)), reads=[h32, wr], writes=[pl])
                            k.op("dve", lambda e, pl=pl: e.tensor_copy(out=lg.ap(), in_=pl[:, 0:8]),
                                 reads=[pl], writes=[lg])
                            k.op("dve", lambda e: e.reduce_max(out=m1.ap(), in_=lg.ap(), axis=mybir.AxisListType.X),
                                 reads=[lg], writes=[m1])
                            k.op("dve", lambda e: e.tensor_scalar(out=r1.ap(), in0=lg.ap(), scalar1=m1.ap(),
                                                                  scalar2=None, op0=ALU.is_ge),
                                 reads=[lg, m1], writes=[r1])
                            k.op("dve", lambda e: e.scalar_tensor_tensor(out=r2.ap(), in0=r1.ap(), scalar=-1e30,
                                                                         in1=lg.ap(), op0=ALU.mult, op1=ALU.add),
                                 reads=[r1, lg], writes=[r2])
                            k.op("dve", lambda e: e.reduce_max(out=m2.ap(), in_=r2.ap(), axis=mybir.AxisListType.X),
                                 reads=[r2], writes=[m2])
                            k.op("dve", lambda e: e.tensor_scalar(out=r1.ap(), in0=lg.ap(), scalar1=m2.ap(),
                                                                  scalar2=None, op0=ALU.is_ge),
                                 reads=[lg, m2], writes=[r1])
                            k.op("dve", lambda e: e.tensor_scalar(out=r2.ap(), in0=lg.ap(), scalar1=m1.ap(),
                                                                  scalar2=None, op0=ALU.subtract),
                                 reads=[lg, m1], writes=[r2])
                            k.op("act", lambda e: e.activation(out=r2.ap(), in_=r2.ap(), func=AF.Exp),
                                 reads=[r2], writes=[r2])
                            k.op("dve", lambda e: e.tensor_tensor(out=gwt.ap(), in0=r1.ap(), in1=r2.ap(), op=ALU.mult),
                                 reads=[r1, r2], writes=[gwt])
                            k.op("dve", lambda e: e.reduce_sum(out=m2.ap(), in_=gwt.ap(), axis=mybir.AxisListType.X),
                                 reads=[gwt], writes=[m2])
                            k.op("dve", lambda e: e.reciprocal(out=m2.ap(), in_=m2.ap()), reads=[m2], writes=[m2])
                            k.op("dve", lambda e: e.tensor_scalar(out=gwt.ap(), in0=gwt.ap(), scalar1=m2.ap(),
                                                                  scalar2=None, op0=ALU.mult),
                                 reads=[gwt, m2], writes=[gwt])
                            pt = self.ps[2]
                            k.op("pe", lambda e, pt=pt: e.transpose(pt[0:8, 0:128], gwt.ap(), self.ident),
                                 reads=[gwt, self.cst], writes=[pt])
                            k.op("act", lambda e, pt=pt, s=s, off=off: e.copy(
                                out=GW[:, off + s * 128:off + (s + 1) * 128], in_=pt[0:8, 0:128]),
                                reads=[pt], writes=[GW])
                ph1.__exit__(None, None, None)
                ph2 = k.stage()
                ph2.__enter__()
                self.alloc_stg()
                w13b = [k.tile([128, 8, 2, FG * 128], BF16, "w13b%d" % i) for i in range(2)]
                w2b = [k.tile([128, FG, D], BF16, "w2b%d" % i) for i in range(2)]
                actb = [k.tile([128, FG, 512], BF16, "actb%d" % i) for i in range(2)]
                sg = [k.tile([128, 512], F32, "sg%d" % i) for i in range(2)]
                it = 0
                ai = 0
                pi = 0
                pend = None

                def emit_w2(bi, t0, n, off, act, w2, nf):
                    tsel = 0 if t0 < T else 1
                    for dc in range(8):
                        po = self.ps[4 + dc % 2]
                        for f in range(nf):
                            k.op("pe", lambda e, f=f, dc=dc, po=po: e.matmul(
                                po[:, 0:n], lhsT=w2[:, f, dc * 128:(dc + 1) * 128], rhs=act[:, f, 0:n],
                                start=(f == 0), stop=(f == nf - 1)), reads=[w2, act], writes=[po])
                        k.op("dve", lambda e, dc=dc, po=po: e.scalar_tensor_tensor(
                            out=X[:, dc, off:off + n], in0=po[:, 0:n], scalar=self.mod(l, 5, dc, tsel),
                            in1=X[:, dc, off:off + n], op0=ALU.mult, op1=ALU.add),
                            reads=[po, self.MODS, X.sub(bi)], writes=[X.sub(bi)])

                for ex in range(nexp):
                    if moe:
                        w13src = self.moe_w13[j, ex]
                        w2src = self.moe_w2[j, ex]
                    else:
                        w13src = self.ffn_w13[j]
                        w2src = self.ffn_w2[j]
                    for (f0, nf) in fgroups:
                        w13 = w13b[it % 2]
                        w2 = w2b[it % 2]
                        it += 1
                        w13v = w13src.rearrange("(kk p) n -> p kk n", p=128)
                        for gu in range(2):
                            for kk in range(0, 8, 4):
                                self.cast_load(w13[:, kk:kk + 4, gu, 0:nf * 128],
                                               w13v[:, kk:kk + 4, gu * FF + f0 * 128: gu * FF + (f0 + nf) * 128],
                                               "p (a b) -> p a b", w13, first=(gu == 0 and kk == 0), a=4)
                        w2v = w2src[f0 * 128:(f0 + nf) * 128, :].rearrange("(f p) n -> p f n", p=128)
                        for f2 in range(0, nf, 2):
                            self.cast_load(w2[:, f2:f2 + 2, :], w2v[:, f2:f2 + 2, :], "p (a b) -> p a b", w2,
                                           first=(f2 == 0), a=2)
                        for bi, (t0, n) in enumerate(blocks):
                            off = t0 - h0
                            act = actb[ai % 2]
                            ai += 1
                            pgw = None
                            if moe:
                                pgw = self.ps[6 + ai % 2]
                                k.op("pe", lambda e, ex=ex, pgw=pgw, n=n, off=off: e.matmul(
                                    pgw[:, 0:n], lhsT=sel[:, ex * 128:(ex + 1) * 128], rhs=GW[:, off:off + n],
                                    start=True, stop=True), reads=[sel, GW], writes=[pgw])
                            for f in range(nf):
                                pg = self.ps[(pi % 2) * 2]
                                pu = self.ps[(pi % 2) * 2 + 1]
                                s_ = sg[pi % 2]
                                pi += 1
                                for kk in range(8):
                                    k.op("pe", lambda e, kk=kk, f=f, pg=pg, w13=w13, n=n, off=off: e.matmul(
                                        pg[:, 0:n], lhsT=w13[:, kk, 0, f * 128:(f + 1) * 128],
                                        rhs=H[:, kk, off:off + n], start=(kk == 0), stop=(kk == 7)),
                                        reads=[w13, H], writes=[pg])
                                for kk in range(8):
                                    k.op("pe", lambda e, kk=kk, f=f, pu=pu, w13=w13, n=n, off=off: e.matmul(
                                        pu[:, 0:n], lhsT=w13[:, kk, 1, f * 128:(f + 1) * 128],
                                        rhs=H[:, kk, off:off + n], start=(kk == 0), stop=(kk == 7)),
                                        reads=[w13, H], writes=[pu])
                                k.op("act", lambda e, pg=pg, s_=s_, n=n: e.activation(out=s_[:, 0:n], in_=pg[:, 0:n],
                                                                                     func=AF.Silu),
                                     reads=[pg], writes=[s_])
                                if moe:
                                    k.op("dve", lambda e, pu=pu, s_=s_, n=n: e.tensor_tensor(
                                        out=s_[:, 0:n], in0=s_[:, 0:n], in1=pu[:, 0:n], op=ALU.mult),
                                        reads=[s_, pu], writes=[s_])
                                    k.op("dve", lambda e, f=f, act=act, s_=s_, pgw=pgw, n=n: e.tensor_tensor(
                                        out=act[:, f, 0:n], in0=s_[:, 0:n], in1=pgw[:, 0:n], op=ALU.mult),
                                        reads=[s_, pgw], writes=[act])
                                else:
                                    k.op("dve", lambda e, f=f, act=act, pu=pu, s_=s_, n=n: e.tensor_tensor(
                                        out=act[:, f, 0:n], in0=s_[:, 0:n], in1=pu[:, 0:n], op=ALU.mult),
                                        reads=[s_, pu], writes=[act])
                            if pend is not None:
                                emit_w2(*pend)
                            pend = (bi, t0, n, off, act, w2, nf)
                if pend is not None:
                    emit_w2(*pend)
                ph2.__exit__(None, None, None)
                for bi, (t0, n) in enumerate(blocks):
                    k.dma("sp", XTv[:, :, t0:t0 + n], X[:, :, t0 - h0:t0 - h0 + n],
                          reads=[X.sub(bi)], writes=[self.XT], join=True, sem=X.sub(bi))


    def load_norm_all(self, l, nidx, ntok, H):
        k = self.k
        XTv = self.XT.ap().rearrange("(c p) t -> p c t", p=128)
        xb = [k.tile([128, 8, 512], F32, "xb%d" % i) for i in range(2)]
        sq = k.tile([128, 8, 512], BF16, "sq")
        rs = k.tile([128, 512], F32, "rs")
        for bi, (t0, n) in enumerate(token_blocks(0, ntok)):
            X = xb[bi % 2]
            k.dma("sp", X[:, :, 0:n], XTv[:, :, t0:t0 + n], reads=[self.XT], writes=[X])
            tsel = 0 if t0 < T else 1
            self.norm_block(_Shift(X, -t0), H, t0, n, l, nidx, tsel, sq, rs, self.ps[7])

    def stage_outproj(self, l, wsrc, OT, ntok):
        k = self.k
        XTv = self.XT.ap().rearrange("(c p) t -> p c t", p=128)
        OTv = OT.ap().rearrange("(c p) t -> p c t", p=128)
        with k.stage():
            W = k.tile([128, 8, D], BF16, "wo")
            self.alloc_stg()
            for kk in range(0, 8, 2):
                self.cast_load(W[:, kk:kk + 2, :], wsrc.rearrange("(kk p) n -> p kk n", p=128)[:, kk:kk + 2, :],
                               "p (a b) -> p a b", W, first=(kk == 0), a=2)
            xb = [k.tile([128, 8, 512], F32, "xb%d" % i) for i in range(2)]
            ob = [k.tile([128, 8, 512], BF16, "ob%d" % i) for i in range(2)]
            for bi, (t0, n) in enumerate(token_blocks(0, ntok)):
                X = xb[bi % 2]
                O = ob[bi % 2]
                tsel = 0 if t0 < T else 1
                k.dma("sp", X[:, :, 0:n], XTv[:, :, t0:t0 + n], reads=[self.XT], writes=[X])
                k.dma("sp", O[:, :, 0:n], OTv[:, :, t0:t0 + n], reads=[OT], writes=[O])
                for dc in range(8):
                    po = self.ps[dc % 4]
                    for kk in range(8):
                        k.op("pe", lambda e, kk=kk, dc=dc, po=po, O=O: e.matmul(
                            po[:, 0:n], lhsT=W[:, kk, dc * 128:(dc + 1) * 128], rhs=O[:, kk, 0:n],
                            start=(kk == 0), stop=(kk == 7)), reads=[W, O], writes=[po])
                    k.op("dve", lambda e, dc=dc, po=po, X=X, tsel=tsel: e.scalar_tensor_tensor(
                        out=X[:, dc, 0:n], in0=po[:, 0:n], scalar=self.mod(l, 2, dc, tsel),
                        in1=X[:, dc, 0:n], op0=ALU.mult, op1=ALU.add), reads=[po, self.MODS, X], writes=[X])
                k.dma("sp", XTv[:, :, t0:t0 + n], X[:, :, 0:n], reads=[X], writes=[self.XT], join=True, sem=X.sub("st"))

    def stage_na(self, l):
        k = self.k
        nc = self.nc
        j = l // 2
        last = (l == DEPTH - 1)
        OT = self.OT
        with k.stage():
            H = k.tile([128, 8, NTOK], BF16, "H")
            with k.stage():
                self.load_norm_all(l, 0, NTOK, H)
            gq = k.tile([128, 1], F32, "gq")
            gk = k.tile([128, 1], F32, "gk")
            with nc.allow_non_contiguous_dma(reason="tiny"):
                for hh in range(2):
                    k.dma("sp", gq[hh * 64:(hh + 1) * 64, :], self.na_q_norm[j].rearrange("(p o) -> p o", o=1),
                          reads=[self.na_q_norm], writes=[gq], join=(hh > 0))
                    k.dma("sp", gk[hh * 64:(hh + 1) * 64, :], self.na_k_norm[j].rearrange("(p o) -> p o", o=1),
                          reads=[self.na_k_norm], writes=[gk], join=(hh > 0))
            k.op("dve", lambda e: e.tensor_scalar(out=gq.ap(), in0=gq.ap(), scalar1=0.125, scalar2=None, op0=ALU.mult),
                 reads=[gq], writes=[gq])
            bd = k.tile([128, 128], BF16, "bd")
            k.op("dve", lambda e: e.memset(bd.ap(), 0.0), writes=[bd])
            for hh in range(2):
                k.op("dve", lambda e, hh=hh: e.memset(bd[hh * 64:(hh + 1) * 64, hh * 64:(hh + 1) * 64], 1.0 / 64),
                     writes=[bd])
            self.alloc_stg()
            wq = [k.tile([128, 8, 3, 128], BF16, "wqkv%d" % i) for i in range(2)]
            QT = k.tile([128, 2, NTOK], BF16, "QT")
            k.op("pool", lambda e: e.memset(QT.ap(), 0.0), writes=[QT])
            KT = k.tile([128, NTOK], BF16, "KT")
            V = k.tile([128, NTOK // 128, 2, 64], BF16, "V")
            OTs = k.tile([128, NTOK], BF16, "OTs")
            BI = k.tile([128, 9, 2, 320], F32, "BI")
            qf = [k.tile([128, 512], F32, "qf%d" % i) for i in range(2)]
            qs = [k.tile([128, 512], BF16, "qs%d" % i) for i in range(2)]
            qr = [k.tile([128, 512], F32, "qr%d" % i) for i in range(2)]
            ET = [k.tile([128, 512], BF16, "ET%d" % i) for i in range(2)]
            rd = [k.tile([128, 256], F32, "rd%d" % i) for i in range(2)]
            w_in = self.na_w_in[j].rearrange("(kk p) n -> p kk n", p=128)
            OTv = OT.ap().rearrange("(c p) t -> p c t", p=128)
            cnt = 0
            for hp in range(getattr(self, "na_hp_limit", 8)):
                w = wq[hp % 2]
                for part in range(3):
                    self.cast_load(w[:, :, part, :], w_in[:, :, part * D + hp * 128: part * D + (hp + 1) * 128],
                                   "p (a b) -> p a b", w, first=(part == 0), a=8)
                if getattr(self, "na_cut", 9) > 2:
                  k.dma("sp", BI.ap().rearrange("p r h q -> p r (h q)"),
                      self.na_bias[j].rearrange("r p (h q) -> p r h q", h=16)[:, :, 2 * hp:2 * hp + 2, :].rearrange("p r h q -> p r (h q)"),
                      reads=[self.na_bias], writes=[BI])
                for part, (dst, gcol) in enumerate(((QT, gq), (KT, gk))):
                    for bi, (t0, n) in enumerate(token_blocks(0, NTOK if getattr(self, "na_cut", 9) > 0 else 0)):
                        pp = self.ps[cnt % 2]
                        pm = self.ps[2 + cnt % 2]
                        f_ = qf[cnt % 2]
                        s_ = qs[cnt % 2]
                        r_ = qr[cnt % 2]
                        cnt += 1
                        for kk in range(8):
                            k.op("pe", lambda e, kk=kk, pp=pp, w=w, part=part: e.matmul(
                                pp[:, 0:n], lhsT=w[:, kk, part, :], rhs=H[:, kk, t0:t0 + n],
                                start=(kk == 0), stop=(kk == 7)), reads=[w, H], writes=[pp])
                        k.op("act", lambda e, pp=pp, s_=s_: e.activation(out=s_[:, 0:n], in_=pp[:, 0:n], func=AF.Square),
                             reads=[pp], writes=[s_])
                        k.op("dve", lambda e, pp=pp, f_=f_: e.tensor_copy(out=f_[:, 0:n], in_=pp[:, 0:n]),
                             reads=[pp], writes=[f_])
                        k.op("pe", lambda e, pm=pm, s_=s_: e.matmul(pm[:, 0:n], lhsT=bd.ap(), rhs=s_[:, 0:n],
                                                                  start=True, stop=True), reads=[bd, s_], writes=[pm])
                        k.op("act", lambda e, pm=pm, r_=r_: e.activation(out=r_[:, 0:n], in_=pm[:, 0:n], func=AF.Sqrt,
                                                                        scale=1.0, bias=self.eps_t.ap()),
                             reads=[pm, self.eps_t], writes=[r_])
                        k.op("dve", lambda e, r_=r_: e.reciprocal(out=r_[:, 0:n], in_=r_[:, 0:n]), reads=[r_], writes=[r_])
                        if part == 0 and not getattr(self, 'na_dbg_full', False):
                            for hl in range(2):
                                hs = slice(hl * 64, (hl + 1) * 64)
                                k.op("dve", lambda e, f_=f_, r_=r_, hs=hs, hl=hl: e.scalar_tensor_tensor(
                                    out=QT[hs, hl, t0:t0 + n], in0=f_[hs, 0:n], scalar=gq[hs, :], in1=r_[hs, 0:n],
                                    op0=ALU.mult, op1=ALU.mult), reads=[f_, r_, gq], writes=[QT])
                        else:
                            dstv = dst[:, 0, :] if part == 0 else dst
                            k.op("dve", lambda e, f_=f_, r_=r_, dst=dstv, gcol=gcol: e.scalar_tensor_tensor(
                                out=dst[:, t0:t0 + n], in0=f_[:, 0:n], scalar=gcol.ap(), in1=r_[:, 0:n],
                                op0=ALU.mult, op1=ALU.mult), reads=[f_, r_, gcol], writes=[dst])
                for ti in range(NTOK // 128 if getattr(self, "na_cut", 9) > 1 else 0):
                    pv = self.ps[4 + ti % 2]
                    for kk in range(8):
                        k.op("pe", lambda e, kk=kk, pv=pv, w=w, ti=ti: e.matmul(
                            pv[:, 0:128], lhsT=H[:, kk, ti * 128:(ti + 1) * 128], rhs=w[:, kk, 2, :],
                            start=(kk == 0), stop=(kk == 7)), reads=[w, H], writes=[pv])
                    eng = "act" if ti % 2 == 0 else "dve"
                    if eng == "act":
                        k.op("act", lambda e, pv=pv, ti=ti: e.copy(out=V[:, ti, :, :].rearrange("p h d -> p (h d)"),
                                                                  in_=pv[:, 0:128]), reads=[pv], writes=[V])
                    else:
                        k.op("dve", lambda e, pv=pv, ti=ti: e.tensor_copy(out=V[:, ti, :, :].rearrange("p h d -> p (h d)"),
                                                                         in_=pv[:, 0:128]), reads=[pv], writes=[V])
                if getattr(self, "na_cut", 9) <= 2:
                    continue
                groups = []
                for r in range(64):
                    rs_ = min(max(r - 4, 0), 56)
                    tb = rs_ // 2
                    nt = 4 if rs_ % 2 == 0 else 5
                    if r < 4:
                        rt = r
                    elif r > 60:
                        rt = r - 56 + 1
                    else:
                        rt = 4 + (rs_ % 2)
                    groups.append((r * 64, 64, [tb + i for i in range(nt)] + [32, 33], rt, nt))
                if not last and getattr(self, "na_cut", 9) > 3:
                    groups.append((T, 256, [32, 33], None, 0))
                for (q0, nq, tiles, rt, nt) in groups:
                    for hl in range(2):
                        pS = self.ps[cnt % 2]
                        pO = self.ps[2 + cnt % 2]
                        et = ET[cnt % 2]
                        rd_ = rd[cnt % 2]
                        cnt += 1
                        hs = slice(hl * 64, (hl + 1) * 64)
                        W_ = len(tiles) * nq
                        for si, tl in enumerate(tiles):
                            k.op("pe", lambda e, si=si, tl=tl, pS=pS, hl=hl: e.matmul(
                                pS[:, si * nq:(si + 1) * nq], lhsT=KT[:, tl * 128:(tl + 1) * 128],
                                rhs=QT[:, hl, q0:q0 + nq], start=True, stop=True), reads=[KT, QT], writes=[pS])
                        if rt is not None:
                            k.op("dve", lambda e, pS=pS, rt=rt, hl=hl, nt=nt: e.tensor_tensor(
                                out=pS[:, 0:nt * 64], in0=pS[:, 0:nt * 64], in1=BI[:, rt, hl, 0:nt * 64], op=ALU.add),
                                reads=[pS, BI], writes=[pS])
                        k.op("act", lambda e, pS=pS, et=et, W_=W_: e.activation(out=et[:, 0:W_], in_=pS[:, 0:W_], func=AF.Exp),
                             reads=[pS], writes=[et])
                        for si, tl in enumerate(tiles):
                            k.op("pe", lambda e, si=si, tl=tl, pO=pO, et=et, hs=hs, hl=hl: e.matmul(
                                pO[hs, 0:nq], lhsT=V[:, tl, hl, :], rhs=et[:, si * nq:(si + 1) * nq],
                                start=(si == 0), stop=(si == len(tiles) - 1)), reads=[V, et], writes=[pO])
                        for si, tl in enumerate(tiles):
                            k.op("pe", lambda e, si=si, pO=pO, et=et, hs=hs: e.matmul(
                                pO[hs, 256:256 + nq], lhsT=self.ones_bf[:, 0:64], rhs=et[:, si * nq:(si + 1) * nq],
                                start=(si == 0), stop=(si == len(tiles) - 1)), reads=[self.ones_bf, et], writes=[pO])
                        k.op("dve", lambda e, pO=pO, rd_=rd_, hs=hs: e.reciprocal(out=rd_[hs, 0:nq], in_=pO[hs, 256:256 + nq]),
                             reads=[pO], writes=[rd_])
                        k.op("dve", lambda e, pO=pO, rd_=rd_, hs=hs: e.tensor_tensor(
                            out=OTs[hs, q0:q0 + nq], in0=pO[hs, 0:nq], in1=rd_[hs, 0:nq], op=ALU.mult),
                            reads=[pO, rd_], writes=[OTs.sub(hl)])
                ntk = T if last else NTOK
                for c0 in range(0, ntk, 1024):
                    c1 = min(ntk, c0 + 1024)
                    k.dma("sp", OTv[:, hp, c0:c1], OTs[:, c0:c1], reads=[OTs.sub(0), OTs.sub(1)], writes=[OT],
                          join=True, sem=OTs.sub("st"))
        self.stage_outproj(l, self.na_w_out[j], OT, T if last else NTOK)


    def stage_gdn(self, l):
        j = l // 2
        if not hasattr(self, "QKVT"):
            k = self.k
            self.QKVT = k.dram("QKVT", [3 * D, NTOK], F32)
            self.GATE = k.dram("GATE", [NTOK, D], BF16)
            self.GB = k.dram("GB", [NTOK, 32], F32)
            self.OD = k.dram("OD", [2, NTOK, D], F32)
        self.gdn_proj(l, j)
        self.gdn_scan(l, j)
        self.gdn_finish(l, j)
        self.stage_outproj(l, self.gdn_w_out[j], self.OT, NTOK)

    def gdn_proj(self, l, j):
        k = self.k
        nc = self.nc
        w_in = self.gdn_w_in[j].rearrange("(kk p) n -> p kk n", p=128)
        QV = self.QKVT.ap().rearrange("(c p) t -> p c t", p=128)
        PADN = NTOK + 4
        with k.stage():
            H = k.tile([128, 8, NTOK], BF16, "H")
            with k.stage():
                self.load_norm_all(l, 0, NTOK, H)
            self.alloc_stg()
            cw = k.tile([128, 24, 3], F32, "cw")
            with nc.allow_non_contiguous_dma(reason="small conv weights"):
                for s_ in range(3):
                    k.dma("sp", cw[:, :, s_], self.gdn_conv_w[j, s_].rearrange("(c p) -> p c", p=128),
                          reads=[self.gdn_conv_w], writes=[cw], join=(s_ > 0))
            wb = [k.tile([128, 8, 128], BF16, "wfc%d" % i) for i in range(2)]
            PT = k.tile([128, PADN], F32, "PT")
            k.op("pool", lambda e: e.memset(PT.ap(), 0.0), writes=[PT])
            cv = [k.tile([128, 512], F32, "cv%d" % i) for i in range(2)]
            sv = [k.tile([128, 512], F32, "sv%d" % i) for i in range(2)]
            sqb = [k.tile([128, 512], BF16, "sqb%d" % i) for i in range(2)]
            rr = [k.tile([128, 512], F32, "rr%d" % i) for i in range(2)]
            cnt = 0
            for fc in range(24):
                w = wb[fc % 2]
                self.cast_load(w.ap(), w_in[:, :, fc * 128:(fc + 1) * 128], "p (a b) -> p a b", w, first=True, a=8)
                for (t0, n) in token_blocks(0, NTOK):
                    pp = self.ps[cnt % 2]
                    cnt += 1
                    for kk in range(8):
                        k.op("pe", lambda e, kk=kk, pp=pp, w=w: e.matmul(
                            pp[:, 0:n], lhsT=w[:, kk, :], rhs=H[:, kk, t0:t0 + n], start=(kk == 0), stop=(kk == 7)),
                            reads=[w, H], writes=[pp])
                    pc = 1 + t0 if t0 < T else 3 + t0
                    k.op("act", lambda e, pp=pp, pc=pc: e.copy(out=PT[:, pc:pc + n], in_=pp[:, 0:n]),
                         reads=[pp], writes=[PT])
                for (t0, n) in token_blocks(0, NTOK):
                    pc = t0 if t0 < T else 2 + t0
                    c_ = cv[cnt % 2]
                    s2 = sv[cnt % 2]
                    q_ = sqb[cnt % 2]
                    r_ = rr[cnt % 2]
                    pm = self.ps[2 + cnt % 2]
                    cnt += 1
                    k.op("dve", lambda e, c_=c_, pc=pc: e.tensor_scalar(
                        out=c_[:, 0:n], in0=PT[:, pc:pc + n], scalar1=cw[:, fc, 0:1], scalar2=None, op0=ALU.mult),
                        reads=[PT, cw], writes=[c_])
                    for s_ in (1, 2):
                        k.op("dve", lambda e, c_=c_, pc=pc, s_=s_: e.scalar_tensor_tensor(
                            out=c_[:, 0:n], in0=PT[:, pc + s_:pc + s_ + n], scalar=cw[:, fc, s_:s_ + 1], in1=c_[:, 0:n],
                            op0=ALU.mult, op1=ALU.add), reads=[PT, cw, c_], writes=[c_])
                    k.op("act", lambda e, c_=c_, s2=s2: e.activation(out=s2[:, 0:n], in_=c_[:, 0:n], func=AF.Silu),
                         reads=[c_], writes=[s2])
                    if fc < 16:
                        k.op("act", lambda e, s2=s2, q_=q_: e.activation(out=q_[:, 0:n], in_=s2[:, 0:n], func=AF.Square),
                             reads=[s2], writes=[q_])
                        k.op("pe", lambda e, pm=pm, q_=q_: e.matmul(pm[:, 0:n], lhsT=self.ones_bf.ap(), rhs=q_[:, 0:n],
                                                                  start=True, stop=True),
                             reads=[self.ones_bf, q_], writes=[pm])
                        k.op("act", lambda e, pm=pm, r_=r_: e.activation(out=r_[:, 0:n], in_=pm[:, 0:n], func=AF.Sqrt,
                                                                        scale=1.0, bias=self.eps_t.ap()),
                             reads=[pm, self.eps_t], writes=[r_])
                        k.op("dve", lambda e, r_=r_: e.reciprocal(out=r_[:, 0:n], in_=r_[:, 0:n]), reads=[r_], writes=[r_])
                        sc_ = (128.0 ** -0.5) if fc < 8 else 1.0
                        k.op("dve", lambda e, r_=r_, s2=s2, sc_=sc_: e.scalar_tensor_tensor(
                            out=s2[:, 0:n], in0=s2[:, 0:n], scalar=sc_, in1=r_[:, 0:n], op0=ALU.mult, op1=ALU.mult),
                            reads=[s2, r_], writes=[s2])
                    k.dma("sp", QV[:, fc, t0:t0 + n], s2[:, 0:n], reads=[s2], writes=[self.QKVT], join=True, sem=s2.sub("st"))
            WG = k.tile([128, 8, D], BF16, "WG")
            for kk in range(0, 8, 2):
                self.cast_load(WG[:, kk:kk + 2, :], w_in[:, kk:kk + 2, 3 * D:4 * D], "p (a b) -> p a b", WG,
                               first=(kk == 0), a=2)
            WAB = k.tile([128, 8, 32], BF16, "WAB")
            with nc.allow_non_contiguous_dma(reason="32-col slice"):
                self.cast_load(WAB.ap(), w_in[:, :, 4 * D:4 * D + 32], "p (a b) -> p a b", WAB, first=True, a=8)
            ALc = k.tile([128, 32], F32, "ALc")
            DTc = k.tile([128, 32], F32, "DTc")
            k.op("dve", lambda e: e.memset(ALc.ap(), 0.0), writes=[ALc])
            k.op("dve", lambda e: e.memset(DTc.ap(), 0.0), writes=[DTc])
            with nc.allow_non_contiguous_dma(reason="tiny broadcast"):
                for d_ in range(2):
                    k.dma("sp", ALc[:, d_ * 16:d_ * 16 + 8], self.gdn_a_log[j, d_].partition_broadcast(128),
                          reads=[self.gdn_a_log], writes=[ALc], join=(d_ > 0))
                    k.dma("sp", DTc[:, d_ * 16:d_ * 16 + 8], self.gdn_dt_bias[j, d_].partition_broadcast(128),
                          reads=[self.gdn_dt_bias], writes=[DTc], join=(d_ > 0))
            k.op("act", lambda e: e.activation(out=ALc.ap(), in_=ALc.ap(), func=AF.Exp), reads=[ALc], writes=[ALc])
            k.op("dve", lambda e: e.tensor_scalar(out=ALc.ap(), in0=ALc.ap(), scalar1=-1.0, scalar2=None, op0=ALU.mult),
                 reads=[ALc], writes=[ALc])
            gt = [k.tile([128, D], BF16, "gt%d" % i) for i in range(2)]
            xa = [k.tile([128, 32], F32, "xa%d" % i) for i in range(2)]
            xb_ = [k.tile([128, 32], F32, "xb_%d" % i) for i in range(2)]
            xc = [k.tile([128, 32], F32, "xc%d" % i) for i in range(2)]
            one_col = self.cst[:, 384:385]
            for ti in range(NTOK // 128):
                g_ = gt[ti % 2]
                for hf in range(2):
                    pg = self.ps[4 + hf]
                    for kk in range(8):
                        k.op("pe", lambda e, kk=kk, pg=pg, hf=hf: e.matmul(
                            pg[:, 0:512], lhsT=H[:, kk, ti * 128:(ti + 1) * 128], rhs=WG[:, kk, hf * 512:(hf + 1) * 512],
                            start=(kk == 0), stop=(kk == 7)), reads=[H, WG], writes=[pg])
                    k.op("act", lambda e, pg=pg, g_=g_, hf=hf: e.activation(out=g_[:, hf * 512:(hf + 1) * 512], in_=pg[:, 0:512],
                                                                           func=AF.Silu), reads=[pg], writes=[g_.sub(hf)])
                k.dma("sp", self.GATE[ti * 128:(ti + 1) * 128, :], g_.ap(), reads=[g_.sub(0), g_.sub(1)], writes=[self.GATE],
                      join=True, sem=g_.sub("st"))
                pa = self.ps[6 + ti % 2]
                a_ = xa[ti % 2]
                b_ = xb_[ti % 2]
                c_ = xc[ti % 2]
                for kk in range(8):
                    k.op("pe", lambda e, kk=kk, pa=pa: e.matmul(
                        pa[:, 0:32], lhsT=H[:, kk, ti * 128:(ti + 1) * 128], rhs=WAB[:, kk, :],
                        start=(kk == 0), stop=(kk == 7)), reads=[H, WAB], writes=[pa])
                k.op("dve", lambda e, pa=pa, a_=a_: e.tensor_tensor(out=a_.ap(), in0=pa[:, 0:32], in1=DTc.ap(), op=ALU.add),
                     reads=[pa, DTc], writes=[a_])
                k.op("dve", lambda e, a_=a_, b_=b_: e.scalar_tensor_tensor(out=b_.ap(), in0=a_.ap(), scalar=-1.0, in1=a_.ap(),
                                                                          op0=ALU.mult, op1=ALU.max), reads=[a_], writes=[b_])
                k.op("act", lambda e, b_=b_: e.activation(out=b_.ap(), in_=b_.ap(), func=AF.Exp, scale=-1.0),
                     reads=[b_], writes=[b_])
                k.op("act", lambda e, b_=b_, c_=c_: e.activation(out=c_.ap(), in_=b_.ap(), func=AF.Ln, bias=one_col, scale=1.0),
                     reads=[b_, self.cst], writes=[c_])
                k.op("dve", lambda e, a_=a_, c_=c_: e.scalar_tensor_tensor(out=c_.ap(), in0=a_.ap(), scalar=0.0, in1=c_.ap(),
                                                                          op0=ALU.max, op1=ALU.add), reads=[a_, c_], writes=[c_])
                k.op("dve", lambda e, c_=c_: e.tensor_tensor(out=c_.ap(), in0=c_.ap(), in1=ALc.ap(), op=ALU.mult),
                     reads=[c_, ALc], writes=[c_])
                k.op("act", lambda e, pa=pa, b_=b_: e.activation(out=b_.ap(), in_=pa[:, 0:32], func=AF.Sigmoid),
                     reads=[pa], writes=[b_])
                cvw = c_.ap().rearrange("p (d a h) -> p d a h", d=2, a=2)
                bvw = b_.ap().rearrange("p (d a h) -> p d a h", d=2, a=2)
                k.op("dve", lambda e, cvw=cvw, bvw=bvw, c_=c_, b_=b_: e.tensor_copy(out=cvw[:, :, 1, :], in_=bvw[:, :, 1, :]),
                     reads=[b_, c_], writes=[c_])
                k.dma("sp", self.GB[ti * 128:(ti + 1) * 128, :], c_.ap(), reads=[c_], writes=[self.GB], join=True,
                      sem=c_.sub("st"))

    def gdn_scan(self, l, j):
        k = self.k
        nc = self.nc
        QV = self.QKVT.ap().rearrange("(a h p) t -> a p h t", a=3, p=128)
        BIG = 30000.0
        with k.stage():
            f4 = lambda nm: k.tile([128, 8, 128], F32, nm)
            ones32 = self.cst[:, 384:512]
            tril = self.cst[:, 128:256]
            triu = self.cst[:, 256:384]
            I8 = f4("I8")
            for h in range(8):
                k.op("pool", lambda e, h=h: e.tensor_copy(out=I8[:, h, :], in_=self.ident), reads=[self.cst], writes=[I8])
            M2s, NM2T, U = [], [], []
            for d in range(2):
                a = k.tile([128, 128], F32, "M2s%d" % d)
                b = k.tile([128, 128], F32, "NM2T%d" % d)
                src_s = triu if d == 0 else tril
                k.op("dve", lambda e, a=a, src_s=src_s: e.tensor_scalar(out=a.ap(), in0=src_s, scalar1=BIG, scalar2=None,
                                                                        op0=ALU.mult), reads=[self.cst], writes=[a])
                src_t = triu if d == 0 else tril
                k.op("dve", lambda e, b=b, src_t=src_t: e.tensor_scalar(out=b.ap(), in0=src_t, scalar1=BIG, scalar2=-BIG,
                                                                        op0=ALU.mult, op1=ALU.add), reads=[self.cst], writes=[b])
                M2s.append(a)
                NM2T.append(b)
                U.append(triu if d == 0 else tril)
            inb = [[f4("in%d_%d" % (a_, i)) for a_ in range(3)] for i in range(2)]
            gbb = [k.tile([128, 32], F32, "gbb%d" % i) for i in range(2)]
            WS = []
            for d_ in range(2):
                w_ = {}
                for nm in ("gc", "egl", "ekd", "bge"):
                    w_[nm] = k.tile([128, 8], F32, "%s%d" % (nm, d_))
                for nm in ("Gd", "Dms", "DmT", "L0", "L1", "M0", "M1", "R", "vb", "kbg", "kd", "u", "wT", "vn", "qg", "QKd"):
                    w_[nm] = f4("%s%d" % (nm, d_))
                WS.append(w_)
            osb = [k.tile([128, D], F32, "osb%d" % i) for i in range(2)]
            S = [f4("S%d" % d) for d in range(2)]
            bctr = {}

            def nbank(key):
                d_, hf_ = key
                base = 2 * (2 * d_ + hf_)
                n_ = bctr.get(key, 0)
                bctr[key] = n_ + 1
                return self.ps[base + n_ % 2]

            def chain(d, ib, gb, ob, t0, hf):
                qT, kT, vT = ib
                hs = list(range(hf * 4, hf * 4 + 4))
                v4 = lambda t: t[:, hf * 4:(hf + 1) * 4, :].rearrange("p h d -> p (h d)")
                G = lambda h: gb[:, d * 16 + h:d * 16 + h + 1]
                Bt = lambda h: gb[:, d * 16 + 8 + h:d * 16 + 8 + h + 1]
                csl = lambda i: slice(i * 128, (i + 1) * 128)
                Sd = S[d]
                w_ = WS[d]
                gc, egl, ekd, bge = w_["gc"], w_["egl"], w_["ekd"], w_["bge"]
                Gd, Dms, DmT, R = w_["Gd"], w_["Dms"], w_["DmT"], w_["R"]
                Lb, Mb = [w_["L0"], w_["L1"]], [w_["M0"], w_["M1"]]
                vb, kbg, kd, u, wT, vn, qg, QKd = (w_[n_] for n_ in ("vb", "kbg", "kd", "u", "wT", "vn", "qg", "QKd"))
                nb = lambda: nbank((d, hf))

                def mm4(lhs_fn, rhs_fn, bank, reads, start=True, stop=True):
                    for i, h in enumerate(hs):
                        k.op("pe", lambda e, i=i, h=h: e.matmul(bank[:, csl(i)], lhsT=lhs_fn(h), rhs=rhs_fn(h), start=start, stop=stop),
                             reads=list(reads), writes=[bank])

                def ev(dst, bank, eng):
                    if eng == "act":
                        k.op("act", lambda e: e.copy(out=v4(dst), in_=bank.ap()), reads=[bank], writes=[dst.sub(hf)])
                    else:
                        k.op("dve", lambda e: e.tensor_copy(out=v4(dst), in_=bank.ap()), reads=[bank], writes=[dst.sub(hf)])

                for h in hs:
                    k.op("act", lambda e, h=h: e.activation(out=Gd[:, h, :], in_=U[d], func=AF.Copy, scale=G(h)),
                         reads=[self.cst, gb], writes=[Gd.sub(hf)])
                yield
                bB = nb()
                mm4(lambda h: ones32, lambda h: Gd[:, h, :], bB, [self.cst, Gd.sub(hf)])
                yield
                for i, h in enumerate(hs):
                    k.op("dve", lambda e, h=h, i=i: e.scalar_tensor_tensor(
                        out=Dms[:, h, :], in0=bB[:, csl(i)], scalar=gc[:, h:h + 1], in1=M2s[d].ap(),
                        op0=ALU.subtract, op1=ALU.max), reads=[bB, gc, M2s[d]], writes=[Dms.sub(hf)])
                    k.op("dve", lambda e, h=h, i=i: e.scalar_tensor_tensor(
                        out=DmT[:, h, :], in0=bB[:, csl(i)], scalar=gc[:, h:h + 1], in1=NM2T[d].ap(),
                        op0=ALU.subtract, op1=ALU.min), reads=[bB, gc, NM2T[d]], writes=[DmT.sub(hf)])
                yield
                k.op("act", lambda e: e.activation(out=v4(qg), in_=bB.ap(), func=AF.Exp), reads=[bB], writes=[qg.sub(hf)])
                k.op("act", lambda e: e.activation(out=v4(Dms), in_=v4(Dms), func=AF.Exp, scale=-1.0), reads=[Dms.sub(hf)],
                     writes=[Dms.sub(hf)])
                k.op("act", lambda e: e.activation(out=v4(DmT), in_=v4(DmT), func=AF.Exp), reads=[DmT.sub(hf)], writes=[DmT.sub(hf)])
                bK = nb()
                mm4(lambda h: kT[:, h, :], lambda h: kT[:, h, :], bK, [kT])
                yield
                k.op("dve", lambda e: e.tensor_tensor(out=v4(qg), in0=v4(qg), in1=v4(qT), op=ALU.mult),
                     reads=[qg.sub(hf), qT], writes=[qg.sub(hf)])
                L, M = Lb[0], Mb[0]
                for i, h in enumerate(hs):
                    k.op("dve", lambda e, h=h, i=i: e.scalar_tensor_tensor(
                        out=L[:, h, :], in0=bK[:, csl(i)], scalar=Bt(h), in1=Dms[:, h, :], op0=ALU.mult, op1=ALU.mult),
                        reads=[bK, gb, Dms.sub(hf)], writes=[L.sub(hf)])
                yield
                bM = nb()
                for i, h in enumerate(hs):
                    k.op("pe", lambda e, h=h, i=i: e.transpose(bM[:, csl(i)], L[:, h, :], self.ident),
                         reads=[L.sub(hf), self.cst], writes=[bM])
                yield
                ev(M, bM, "act")
                k.op("dve", lambda e: e.tensor_tensor(out=v4(R), in0=v4(I8), in1=bM.ap(), op=ALU.subtract),
                     reads=[bM, I8], writes=[R.sub(hf)])
                yield
                for lev in range(1, 7):
                    Lp, Mp = Lb[(lev - 1) % 2], Mb[(lev - 1) % 2]
                    Ln_, Mn_ = Lb[lev % 2], Mb[lev % 2]
                    bL = nb()
                    mm4(lambda h: Mp[:, h, :], lambda h: Lp[:, h, :], bL, [Mp.sub(hf), Lp.sub(hf)])
                    if lev < 6:
                        bN = nb()
                        mm4(lambda h: Lp[:, h, :], lambda h: Mp[:, h, :], bN, [Mp.sub(hf), Lp.sub(hf)])
                    yield
                    ev(Ln_, bL, "act")
                    if lev < 6:
                        ev(Mn_, bN, "dve")
                    yield
                    bR = nb()
                    mm4(lambda h: Ln_[:, h, :], lambda h: R[:, h, :], bR, [Ln_.sub(hf), R.sub(hf)])
                    yield
                    k.op("dve", lambda e, bR=bR: e.tensor_tensor(out=v4(R), in0=v4(R), in1=bR.ap(), op=ALU.add),
                         reads=[bR, R.sub(hf)], writes=[R.sub(hf)])
                    yield
                bT = nb()
                for i, h in enumerate(hs):
                    k.op("pe", lambda e, h=h, i=i: e.transpose(bT[:, csl(i)], kT[:, h, :], self.ident), reads=[kT, self.cst], writes=[bT])
                bV = nb()
                for i, h in enumerate(hs):
                    k.op("pe", lambda e, h=h, i=i: e.transpose(bV[:, csl(i)], vT[:, h, :], self.ident), reads=[vT, self.cst], writes=[bV])
                yield
                for i, h in enumerate(hs):
                    k.op("act", lambda e, h=h, i=i: e.activation(out=kbg[:, h, :], in_=bT[:, csl(i)], func=AF.Copy,
                                                                scale=bge[:, h:h + 1]), reads=[bT, bge], writes=[kbg.sub(hf)])
                    k.op("dve", lambda e, h=h, i=i: e.tensor_scalar(out=kd[:, h, :], in0=bT[:, csl(i)], scalar1=ekd[:, h:h + 1],
                                                                  scalar2=None, op0=ALU.mult), reads=[bT, ekd], writes=[kd.sub(hf)])
                yield
                for i, h in enumerate(hs):
                    k.op("act", lambda e, h=h, i=i: e.activation(out=vb[:, h, :], in_=bV[:, csl(i)], func=AF.Copy, scale=Bt(h)),
                         reads=[bV, gb], writes=[vb.sub(hf)])
                yield
                bU = nb()
                mm4(lambda h: R[:, h, :], lambda h: vb[:, h, :], bU, [R.sub(hf), vb.sub(hf)])
                bW = nb()
                mm4(lambda h: kbg[:, h, :], lambda h: R[:, h, :], bW, [R.sub(hf), kbg.sub(hf)])
                yield
                ev(u, bU, "act")
                ev(wT, bW, "dve")
                bQ = nb()
                mm4(lambda h: kT[:, h, :], lambda h: qT[:, h, :], bQ, [kT, qT])
                yield
                k.op("dve", lambda e: e.tensor_tensor(out=v4(QKd), in0=bQ.ap(), in1=v4(DmT), op=ALU.mult),
                     reads=[bQ, DmT.sub(hf)], writes=[QKd.sub(hf)])
                bS = nb()
                mm4(lambda h: wT[:, h, :], lambda h: Sd[:, h, :], bS, [wT.sub(hf), Sd.sub(hf)])
                yield
                k.op("dve", lambda e: e.tensor_tensor(out=v4(vn), in0=v4(u), in1=bS.ap(), op=ALU.subtract),
                     reads=[bS, u.sub(hf)], writes=[vn.sub(hf)])
                yield
                bO = nb()
                for i, h in enumerate(hs):
                    k.op("pe", lambda e, h=h, i=i: e.matmul(bO[:, csl(i)], lhsT=qg[:, h, :], rhs=Sd[:, h, :], start=True, stop=False),
                         reads=[qg.sub(hf), Sd.sub(hf)], writes=[bO])
                    k.op("pe", lambda e, h=h, i=i: e.matmul(bO[:, csl(i)], lhsT=QKd[:, h, :], rhs=vn[:, h, :], start=False, stop=True),
                         reads=[QKd.sub(hf), vn.sub(hf)], writes=[bO])
                bN2 = nb()
                mm4(lambda h: kd[:, h, :], lambda h: vn[:, h, :], bN2, [kd.sub(hf), vn.sub(hf)])
                yield
                k.op("act", lambda e: e.copy(out=ob[:, hf * 512:(hf + 1) * 512], in_=bO.ap()), reads=[bO], writes=[ob.sub(hf)])
                k.dma("sp", self.OD[d, t0:t0 + 128, hf * 512:(hf + 1) * 512], ob[:, hf * 512:(hf + 1) * 512],
                      reads=[ob.sub(hf)], writes=[self.OD], join=True, sem=ob.sub("st%d" % hf))
                for i, h in enumerate(hs):
                    k.op("dve", lambda e, h=h, i=i: e.scalar_tensor_tensor(
                        out=Sd[:, h, :], in0=Sd[:, h, :], scalar=egl[:, h:h + 1], in1=bN2[:, csl(i)], op0=ALU.mult, op1=ALU.add),
                        reads=[bN2, egl, Sd.sub(hf)], writes=[Sd.sub(hf)])
                yield

            it = 0
            order = {0: [32, 33] + list(range(32)), 1: [33, 32] + list(range(31, -1, -1))}
            for step in range(34):
                gens = []
                for d in range(2):
                    c = order[d][step]
                    t0 = c * 128
                    ib = inb[d]
                    gb = gbb[d]
                    ob = osb[d]
                    w_ = WS[d]
                    gc, egl, ekd, bge = w_["gc"], w_["egl"], w_["ekd"], w_["bge"]
                    for a_ in range(3):
                        k.dma("sp", ib[a_].ap(), QV[a_, :, :, t0:t0 + 128], reads=[self.QKVT], writes=[ib[a_]])
                    k.dma("sp", gb.ap(), self.GB[t0:t0 + 128, :], reads=[self.GB], writes=[gb])
                    if step == 0:
                        for hf in range(2):
                            k.op("pool", lambda e, d=d, hf=hf: e.memset(S[d][:, hf * 4:(hf + 1) * 4, :], 0.0), writes=[S[d].sub(hf)])
                    Gall = gb[:, d * 16:d * 16 + 8]
                    Ball = gb[:, d * 16 + 8:d * 16 + 16]
                    p0 = nbank((d, 0))
                    k.op("pe", lambda e, p0=p0, d=d, Gall=Gall: e.matmul(p0[:, 0:8], lhsT=U[d], rhs=Gall, start=True, stop=True),
                         reads=[self.cst, gb], writes=[p0])
                    k.op("pe", lambda e, p0=p0, Gall=Gall: e.matmul(p0[:, 8:16], lhsT=ones32, rhs=Gall, start=True, stop=True),
                         reads=[self.cst, gb], writes=[p0])
                    k.op("dve", lambda e, p0=p0, gc=gc: e.tensor_copy(out=gc.ap(), in_=p0[:, 0:8]), reads=[p0], writes=[gc])
                    k.op("dve", lambda e, p0=p0, gc=gc, ekd=ekd: e.tensor_tensor(out=ekd.ap(), in0=p0[:, 8:16], in1=gc.ap(), op=ALU.subtract),
                         reads=[p0, gc], writes=[ekd])
                    k.op("act", lambda e, p0=p0, egl=egl: e.activation(out=egl.ap(), in_=p0[:, 8:16], func=AF.Exp), reads=[p0], writes=[egl])
                    k.op("act", lambda e, ekd=ekd: e.activation(out=ekd.ap(), in_=ekd.ap(), func=AF.Exp), reads=[ekd], writes=[ekd])
                    k.op("act", lambda e, gc=gc, bge=bge: e.activation(out=bge.ap(), in_=gc.ap(), func=AF.Exp), reads=[gc], writes=[bge])
                    k.op("dve", lambda e, Ball=Ball, bge=bge: e.tensor_tensor(out=bge.ap(), in0=bge.ap(), in1=Ball, op=ALU.mult),
                         reads=[bge, gb], writes=[bge])
                    gens += [chain(d, ib, gb, ob, t0, 0), chain(d, ib, gb, ob, t0, 1)]
                while gens:
                    for g_ in list(gens):
                        try:
                            next(g_)
                        except StopIteration:
                            gens.remove(g_)

    def gdn_finish(self, l, j):
        k = self.k
        nc = self.nc
        OTv = self.OT.ap().rearrange("(c p) t -> p c t", p=128)
        with k.stage():
            NG = k.tile([128, D], F32, "NG")
            with nc.allow_non_contiguous_dma(reason="tiny broadcast"):
                for h in range(8):
                    k.dma("sp", NG[:, h * 128:(h + 1) * 128], self.gdn_norm_g[j].partition_broadcast(128),
                          reads=[self.gdn_norm_g], writes=[NG], join=(h > 0))
            o0 = [k.tile([128, D], F32, "o0_%d" % i) for i in range(2)]
            o1 = [k.tile([128, D], F32, "o1_%d" % i) for i in range(2)]
            gtb = [k.tile([128, D], BF16, "gtb%d" % i) for i in range(2)]
            sq = k.tile([128, D], F32, "sq")
            ss = [k.tile([128, 8], F32, "ss%d" % i) for i in range(2)]
            yT = [k.tile([128, 8, 512], BF16, "yT%d" % i) for i in range(2)]
            ntile = NTOK // 128
            for ti in range(ntile):
                a, b, g_ = o0[ti % 2], o1[ti % 2], gtb[ti % 2]
                s_ = ss[ti % 2]
                grp, gi = ti // 4, ti % 4
                y_ = yT[grp % 2]
                for hf in range(2):
                    sl = slice(hf * 512, (hf + 1) * 512)
                    k.dma("sp", a[:, sl], self.OD[0, ti * 128:(ti + 1) * 128, sl], reads=[self.OD], writes=[a], join=(hf > 0))
                    k.dma("sp", b[:, sl], self.OD[1, ti * 128:(ti + 1) * 128, sl], reads=[self.OD], writes=[b], join=(hf > 0))
                k.dma("sp", g_.ap(), self.GATE[ti * 128:(ti + 1) * 128, :], reads=[self.GATE], writes=[g_])
                k.op("dve", lambda e, a=a, b=b: e.tensor_tensor(out=a.ap(), in0=a.ap(), in1=b.ap(), op=ALU.add), reads=[a, b], writes=[a])
                k.op("act", lambda e, a=a: e.activation(out=sq.ap(), in_=a.ap(), func=AF.Square), reads=[a], writes=[sq])
                k.op("dve", lambda e, s_=s_: e.tensor_reduce(out=s_.ap(), in_=sq.ap().rearrange("p (h d) -> p h d", h=8),
                                                             op=ALU.add, axis=mybir.AxisListType.X), reads=[sq], writes=[s_])
                k.op("act", lambda e, s_=s_: e.activation(out=s_.ap(), in_=s_.ap(), func=AF.Sqrt, scale=1.0 / 128, bias=self.eps_t.ap()),
                     reads=[s_, self.eps_t], writes=[s_])
                k.op("dve", lambda e, s_=s_: e.reciprocal(out=s_.ap(), in_=s_.ap()), reads=[s_], writes=[s_])
                for h in range(8):
                    k.op("dve", lambda e, h=h, a=a, s_=s_: e.scalar_tensor_tensor(
                        out=a[:, h * 128:(h + 1) * 128], in0=a[:, h * 128:(h + 1) * 128], scalar=s_[:, h:h + 1],
                        in1=NG[:, h * 128:(h + 1) * 128], op0=ALU.mult, op1=ALU.mult), reads=[a, s_, NG], writes=[a])
                k.op("dve", lambda e, a=a, g_=g_: e.tensor_tensor(out=a.ap(), in0=a.ap(), in1=g_.ap(), op=ALU.mult), reads=[a, g_], writes=[a])
                for hf in range(2):
                    bank = self.ps[(ti % 2) * 2 + hf]
                    for c4 in range(4):
                        c = hf * 4 + c4
                        k.op("pe", lambda e, c=c, c4=c4, bank=bank, a=a: e.transpose(bank[:, c4 * 128:(c4 + 1) * 128], a[:, c * 128:(c + 1) * 128],
                                                                                    self.ident), reads=[a, self.cst], writes=[bank])
                    o_ = y_[:, hf * 4:(hf + 1) * 4, gi * 128:(gi + 1) * 128]
                    if hf == 0:
                        k.op("act", lambda e, o_=o_, bank=bank, y_=y_: e.copy(out=o_, in_=bank.ap().rearrange("p (c t) -> p c t", t=128)),
                             reads=[bank], writes=[y_])
                    else:
                        k.op("dve", lambda e, o_=o_, bank=bank, y_=y_: e.tensor_copy(out=o_, in_=bank.ap().rearrange("p (c t) -> p c t", t=128)),
                             reads=[bank], writes=[y_])
                if gi == 3 or ti == ntile - 1:
                    nn = (gi + 1) * 128
                    k.dma("sp", OTv[:, :, grp * 512:grp * 512 + nn], y_[:, :, 0:nn], reads=[y_], writes=[self.OT], join=True,
                          sem=y_.sub("st"))


class _Shift:
    def __init__(self, t, sh):
        self.t = t
        self.d = t.d
        self.sh = sh

    def __getitem__(self, kk):
        a, b, c = kk
        c = slice(c.start + self.sh, c.stop + self.sh)
        return self.t[a, b, c]


class _SubView:
    def __init__(self, t, key):
        self.t = t
        self.d = t.sub(key)

    def __getitem__(self, kk):
        return self.t[kk]


KB._norm_orig = KB._norm


def _norm2(ds):
    out = []
    for d in ds:
        if d is None:
            continue
        if isinstance(d, (Tl, _SubView, _Shift)):
            out.append(d.d)
        else:
            out.append(d)
    return out


KB._norm = staticmethod(_norm2)


def build(layers=(0, 1, 2, 3), stages=None):
    nc = bass.Bass("TRN2", target_bir_lowering=False)
    P = Prog(nc, layers)
    P.stage_init()
    P.stage_in_transpose()
    for l in layers:
        if stages is None or "mix" in stages:
            if l % 2 == 0:
                P.stage_gdn(l)
            else:
                P.stage_na(l)
        if stages is None or "ffn" in stages:
            P.stage_ffn(l, moe=(l % 2 == 1))
    P.stage_out_transpose()
    P.k.barrier()
    return nc, P


def make_consts():
    c = np.zeros((128, 512 + 1024), np.float32)
    for e in range(8):
        c[e, 512 + e * 128:512 + (e + 1) * 128] = 1.0
    c[:, 0:128] = np.eye(128, dtype=np.float32)
    c[:, 128:256] = np.tril(np.ones((128, 128), np.float32))
    c[:, 256:384] = np.triu(np.ones((128, 128), np.float32))
    c[:, 384:512] = 1.0
    return c


def make_na_bias(rpb):
    rpb = np.asarray(rpb, np.float32)
    nl = rpb.shape[0]
    out = np.full((nl, 9, 128, 16, 320), -30000.0, np.float32)
    p = np.arange(128)
    q = np.arange(64)
    kc = p % 64
    wstart = np.clip(q - 8, 0, 48)
    valid_c = (kc[:, None] >= wstart[None, :]) & (kc[:, None] < wstart[None, :] + 16)
    dc_idx = np.clip(kc[:, None] - q[None, :] + 15, 0, 30)
    for rt in range(9):
        if rt < 4:
            r, rs_ = rt, 0
        elif rt == 4:
            r, rs_ = 8, 4
        elif rt == 5:
            r, rs_ = 9, 5
        else:
            r, rs_ = 56 + rt - 1, 56
        base = rs_ - rs_ % 2
        nt = 4 if rs_ % 2 == 0 else 5
        for slot in range(nt):
            grow = base + 2 * slot + p // 64
            jw = grow - rs_
            valid = valid_c & ((jw >= 0) & (jw < 8))[:, None]
            dr_idx = np.clip(grow - r + 7, 0, 14)
            vals = rpb[:, :, dr_idx[:, None], dc_idx]
            vals = np.transpose(vals, (0, 2, 1, 3))
            blk = out[:, rt, :, :, slot * 64:(slot + 1) * 64]
            out[:, rt, :, :, slot * 64:(slot + 1) * 64] = np.where(valid[None, :, None, :], vals, blk)
    return out.reshape(nl, 9, 128, 16 * 320)


def make_in_maps(P, inp, shared):
    in_maps = []
    for b in range(NCORES):
        m = {n: v for n, v in shared.items() if n in P.exts}
        m["x"] = np.ascontiguousarray(inp["x"][b])
        m["ctx"] = np.ascontiguousarray(inp["ctx"][b])
        m["cvec"] = np.ascontiguousarray(np.stack([inp["c"][b], inp["c_ctx"]], 0))
        in_maps.append(m)
    return in_maps


def make_shared(inp):
    shared = {n: np.ascontiguousarray(inp[n], dtype=np.float32) for n in (
        "ada_w", "ada_b", "norm1_g", "norm2_g", "gdn_w_in", "gdn_conv_w", "gdn_a_log", "gdn_dt_bias",
        "gdn_norm_g", "gdn_w_out", "na_w_in", "na_q_norm", "na_k_norm", "na_w_out", "ffn_w13", "ffn_w2",
        "moe_router", "moe_w13", "moe_w2")}
    shared["consts"] = make_consts()
    shared["na_bias"] = make_na_bias(inp["na_rpb"])
    return shared


def kernel(**inp):
    nc, P = build()
    shared = make_shared(inp)
    in_maps = make_in_maps(P, inp, shared)
    res = run_bass_kernel_spmd(nc, in_maps, core_ids=list(range(NCORES)))
    return np.stack([r["y"] for r in res.results], 0)
```

```python
import numpy as np
from contextlib import ExitStack
import concourse.bass as bass
import concourse.mybir as mybir
from concourse.bass_utils import run_bass_kernel_spmd

F32 = mybir.dt.float32
BF16 = mybir.dt.bfloat16
AF = mybir.ActivationFunctionType
ALU = mybir.AluOpType

D = 1024
T = 4096
NCTX = 256
NTOK = T + NCTX
DEPTH = 4
NCORES = 8
EPS = 1e-6
FF_DENSE = 2816
FF_EXPERT = 3584
NEXP = 8


class Dep:
    __slots__ = ("w", "r", "dsem", "excl")

    def __init__(self):
        self.w = {}
        self.r = {}
        self.dsem = None
        self.excl = False


class Tl:
    def __init__(self, t, is_dram=False):
        self.t = t
        self.d = Dep()
        self.subs = {}
        self.is_dram = is_dram

    def __getitem__(self, k):
        if self.is_dram:
            return self.t.ap()[k]
        return self.t[k]

    def ap(self):
        return self.t.ap() if self.is_dram else self.t[:]

    def sub(self, key):
        if key not in self.subs:
            self.subs[key] = Dep()
        return self.subs[key]


class KB:
    def __init__(self, nc):
        self.nc = nc
        self.E = {"pe": nc.tensor, "act": nc.scalar, "dve": nc.vector, "pool": nc.gpsimd, "sp": nc.sync}
        self.es = ExitStack()
        self.sems = []
        self.csem = {}
        for e in self.E:
            self.csem[e] = self._newsem("c_" + e)
        self.cnt = {e: 0 for e in self.E}
        self.known = {e: {} for e in self.E}
        self.dfree = [self._newsem("d%d" % i) for i in range(90)]
        self.dval = {}
        self.dused = []
        self.stage_deps = []
        self.stage_es = None
        self.uid = 0
        self.ninstr = 0

    def _newsem(self, name):
        s = self.es.enter_context(self.nc.semaphore(name))
        self.sems.append(s)
        return len(self.sems) - 1

    def tile(self, shape, dtype, name=None, persistent=False):
        self.uid += 1
        name = (name or "t") + "_%d" % self.uid
        es = self.es if persistent else self.stage_es
        t = es.enter_context(self.nc.sbuf_tensor(name, list(shape), dtype))
        return Tl(t)

    def dram(self, name, shape, dtype, kind="Internal"):
        t = self.nc.dram_tensor(name, list(shape), dtype, kind=kind)
        return Tl(t, is_dram=True)

    def _wait(self, e, tok):
        if tok is None:
            return
        idx, val = tok
        if e == "pe" and idx == self.csem["pe"]:
            return
        if self.known[e].get(idx, 0) >= val:
            return
        self.E[e].wait_ge(self.sems[idx], val)
        self.known[e][idx] = val

    def _deps_wait(self, e, reads, writes, join=False):
        for d in reads:
            for tok in d.w.items():
                self._wait(e, tok)
            if d.excl:
                for tok in d.r.items():
                    if tok[0] != self.csem.get(e):
                        self._wait(e, tok)
        for d in writes:
            if not join:
                for tok in d.w.items():
                    self._wait(e, tok)
            for tok in d.r.items():
                self._wait(e, tok)

    @staticmethod
    def _norm(ds):
        out = []
        for d in ds:
            if d is None:
                continue
            out.append(d.d if isinstance(d, Tl) else d)
        return out

    def op(self, e, fn, reads=(), writes=()):
        reads = self._norm(reads)
        writes = self._norm(writes)
        self._deps_wait(e, reads, writes)
        ins = fn(self.E[e])
        self.cnt[e] += 1
        self.ninstr += 1
        ins.then_inc(self.sems[self.csem[e]], 1)
        tok = (self.csem[e], self.cnt[e])
        for d in reads:
            if d.r.get(tok[0], 0) < tok[1]:
                d.r[tok[0]] = tok[1]
        for d in writes:
            d.w = {tok[0]: tok[1]}
            d.r = {}
        return ins

    def dma(self, q, out, in_, reads=(), writes=(), join=False, sem=None, **kw):
        reads = self._norm(reads)
        writes = self._norm(writes)
        assert len(writes) == 1
        wd = writes[0]
        self._deps_wait(q, reads, writes, join=join)
        sd = wd if sem is None else self._norm([sem])[0]
        if sd.dsem is None:
            sd.dsem = self.dfree.pop()
            self.dused.append(sd)
        idx = sd.dsem
        self.dval[idx] = self.dval.get(idx, 0) + 16
        ins = self.E[q].dma_start(out=out, in_=in_, **kw)
        ins.then_inc(self.sems[idx], 16)
        self.ninstr += 1
        tok = (idx, self.dval[idx])
        for d in reads:
            if d.r.get(idx, 0) < tok[1]:
                d.r[idx] = tok[1]
        if join:
            wd.w[idx] = tok[1]
        else:
            wd.w = {idx: tok[1]}
            wd.r = {}
        return ins

    def barrier(self):
        toks = [(self.csem[e], self.cnt[e]) for e in self.E if self.cnt[e] > 0]
        toks += [(idx, v) for idx, v in self.dval.items()]
        for e in self.E:
            for tok in toks:
                self._wait(e, tok)
        for d in self.dused:
            self.dfree.append(d.dsem)
            d.dsem = None
        self.dused = []

    class _Stage:
        def __init__(self, kb):
            self.kb = kb

        def __enter__(self):
            self.prev = self.kb.stage_es
            self.kb.stage_es = ExitStack()
            self.kb.stage_es.__enter__()
            return self.kb

        def __exit__(self, *a):
            self.kb.barrier()
            self.kb.stage_es.__exit__(*a)
            self.kb.stage_es = self.prev
            return False

    def stage(self):
        return KB._Stage(self)


def token_blocks(n0, n1, blk=512):
    out = []
    t = n0
    while t < n1:
        b = min(blk, n1 - t)
        out.append((t, b))
        t += b
    return out


class Prog:
    def __init__(self, nc, layers=(0, 1, 2, 3), debug=False):
        self.nc = nc
        self.k = KB(nc)
        k = self.k
        self.layers = layers
        self.ext_shapes = {
            "x": [T, D], "ctx": [NCTX, D], "cvec": [2, D], "ada_w": [DEPTH, D, 6 * D], "ada_b": [DEPTH, 6 * D],
            "norm1_g": [DEPTH, D], "norm2_g": [DEPTH, D], "gdn_w_in": [2, D, 4 * D + 32],
            "gdn_conv_w": [2, 3, 3 * D], "gdn_a_log": [2, 2, 8], "gdn_dt_bias": [2, 2, 8],
            "gdn_norm_g": [2, 128], "gdn_w_out": [2, D, D], "na_w_in": [2, D, 3 * D], "na_q_norm": [2, 64],
            "na_k_norm": [2, 64], "na_bias": [2, 9, 128, 16 * 320], "na_w_out": [2, D, D],
            "ffn_w13": [2, D, 2 * FF_DENSE], "ffn_w2": [2, FF_DENSE, D], "moe_router": [2, D, NEXP],
            "moe_w13": [2, NEXP, D, 2 * FF_EXPERT], "moe_w2": [2, NEXP, FF_EXPERT, D],
            "consts": [128, 4 * 128 + 1024]}
        self.exts = {}
        self.y = k.dram("y", [T, D], F32, kind="ExternalOutput")
        self.XT = k.dram("XT", [D, NTOK], F32)
        self.OT = k.dram("OT", [D, NTOK], BF16)
        self.cst = k.tile([128, 4 * 128], F32, "cst", persistent=True)
        self.ones_bf = k.tile([128, 128], BF16, "ones_bf", persistent=True)
        self.ident = self.cst[:, 0:128]
        self.MODS = k.tile([128, DEPTH * 6 * 8 * 2], F32, "mods", persistent=True)
        self.GG = k.tile([128, DEPTH * 2 * 8 * 2], F32, "gg", persistent=True)
        self.eps_t = k.tile([128, 1], F32, "eps", persistent=True)
        self.ps = []
        for b in range(8):
            t = k.es.enter_context(nc.psum_tensor("ps%d" % b, [128, 512], F32))
            pt_ = Tl(t)
            pt_.d.excl = True
            self.ps.append(pt_)

    def __getattr__(self, name):
        shapes = self.__dict__.get("ext_shapes", {})
        if name in shapes:
            if name not in self.exts:
                self.exts[name] = self.k.dram(name, shapes[name], F32, kind="ExternalInput")
            return self.exts[name]
        raise AttributeError(name)

    STG = 2048

    def alloc_stg(self):
        self.stg = [self.k.tile([128, self.STG], F32, "stg%d" % i) for i in range(2)]
        self.stg_i = 0

    def cast_load(self, dst, src, pat, wdep, first=True, **dims):
        k = self.k
        st = self.stg[self.stg_i % 2]
        self.stg_i += 1
        n = 1
        for d_ in dst.shape[1:]:
            n *= d_
        assert n <= self.STG
        view = st[:, 0:n]
        if pat is not None:
            view = view.rearrange(pat, **dims)
        k.dma("sp", view, src, reads=[self.consts], writes=[st])
        deps_w = [wdep]
        k.op("pool", lambda e: e.tensor_copy(out=dst, in_=view), reads=[st], writes=deps_w) if first else \
            self._join_op("pool", lambda e: e.tensor_copy(out=dst, in_=view), [st], wdep)

    def _join_op(self, eng, fn, reads, wdep):
        k = self.k
        wd = k._norm([wdep])[0]
        reads_n = k._norm(reads)
        k._deps_wait(eng, reads_n, [wd], join=True)
        ins = fn(k.E[eng])
        k.cnt[eng] += 1
        k.ninstr += 1
        ins.then_inc(k.sems[k.csem[eng]], 1)
        tok = (k.csem[eng], k.cnt[eng])
        for d in reads_n:
            if d.r.get(tok[0], 0) < tok[1]:
                d.r[tok[0]] = tok[1]
        wd.w[tok[0]] = tok[1]

    def mod(self, l, m, c, t):
        o = ((l * 6 + m) * 8 + c) * 2 + t
        return self.MODS[:, o:o + 1]

    def gg(self, l, n, c, t):
        o = ((l * 2 + n) * 8 + c) * 2 + t
        return self.GG[:, o:o + 1]

    def stage_init(self):
        k = self.k
        nc = self.nc
        with k.stage():
            k.dma("sp", self.cst.ap(), self.consts[:, 0:512], reads=[self.consts], writes=[self.cst])
            k.op("dve", lambda e: e.tensor_copy(out=self.ones_bf.ap(), in_=self.cst[:, 384:512]),
                 reads=[self.cst], writes=[self.ones_bf])
            k.op("dve", lambda e: e.memset(self.eps_t.ap(), EPS), writes=[self.eps_t])
            craw = k.tile([128, 2, 8], F32, "craw")
            sc = k.tile([128, 8, 2], F32, "sc")
            with nc.allow_non_contiguous_dma(reason="tiny"):
                k.dma("sp", craw.ap(), self.cvec.ap().rearrange("t (c p) -> p t c", p=128),
                      reads=[self.cvec], writes=[craw])
            k.op("act", lambda e: e.activation(out=sc.ap().rearrange("p c t -> p t c"), in_=craw.ap(), func=AF.Silu),
                 reads=[craw], writes=[sc])
            ab = k.tile([128, DEPTH, 48], F32, "adab")
            g1 = k.tile([128, 2, DEPTH, 8], F32, "ng")
            with nc.allow_non_contiguous_dma(reason="tiny"):
                k.dma("sp", ab.ap(), self.ada_b.ap().rearrange("l (o p) -> p l o", p=128),
                      reads=[self.ada_b], writes=[ab])
                k.dma("sp", g1[:, 0, :, :], self.norm1_g.ap().rearrange("l (c p) -> p l c", p=128),
                      reads=[self.norm1_g], writes=[g1.sub(0)])
                k.dma("sp", g1[:, 1, :, :], self.norm2_g.ap().rearrange("l (c p) -> p l c", p=128),
                      reads=[self.norm2_g], writes=[g1.sub(1)])
            NW = 1536
            wb = [k.tile([128, 8, NW], F32, "adaw%d" % i) for i in range(2)]
            it = 0
            for l in range(DEPTH):
                pst = self.ps[l % 2]
                for q in range(6 * D // NW):
                    w = wb[it % 2]
                    it += 1
                    k.dma("sp", w.ap(),
                          self.ada_w[l].rearrange("(kk p) n -> p kk n", p=128)[:, :, q * NW:(q + 1) * NW],
                          reads=[self.ada_w], writes=[w])
                    for oc in range(NW // 128):
                        occ = q * (NW // 128) + oc
                        for kk in range(8):
                            k.op("pe", lambda e, kk=kk, oc=oc, occ=occ, w=w, pst=pst: e.matmul(
                                pst[:, occ * 2:occ * 2 + 2], lhsT=w[:, kk, oc * 128:(oc + 1) * 128],
                                rhs=sc[:, kk, :], start=(kk == 0), stop=(kk == 7)),
                                reads=[w, sc], writes=[pst])
                mv = self.MODS[:, l * 96:(l + 1) * 96].rearrange("p (o t) -> p o t", t=2)
                for t in range(2):
                    k.op("dve", lambda e, t=t, mv=mv, pst=pst, l=l: e.tensor_tensor(
                        out=mv[:, :, t], in0=pst[:, 0:96].rearrange("p (o t) -> p o t", t=2)[:, :, t],
                        in1=ab[:, l, :], op=ALU.add), reads=[pst, ab], writes=[self.MODS])
                for n in range(2):
                    m = 1 if n == 0 else 4
                    for t in range(2):
                        o = (l * 6 + m) * 16
                        src = self.MODS[:, o:o + 16].rearrange("p (c t) -> p c t", t=2)[:, :, t]
                        og = (l * 2 + n) * 16
                        dst = self.GG[:, og:og + 16].rearrange("p (c t) -> p c t", t=2)[:, :, t]
                        k.op("dve", lambda e, src=src, dst=dst, l=l, n=n: e.scalar_tensor_tensor(
                            out=dst, in0=src, scalar=1.0, in1=g1[:, n, l, :], op0=ALU.add, op1=ALU.mult),
                            reads=[self.MODS, g1.sub(0), g1.sub(1)], writes=[self.GG])

    def stage_in_transpose(self):
        k = self.k
        XTv = self.XT.ap().rearrange("(c p) t -> p c t", p=128)
        with k.stage():
            xin = [k.tile([128, D], F32, "xin%d" % i) for i in range(2)]
            xo = [k.tile([128, 8, 128], F32, "xo%d" % i) for i in range(2)]
            for ti in range(NTOK // 128):
                src = self.x[ti * 128:(ti + 1) * 128, :] if ti < T // 128 else \
                    self.ctx[(ti - T // 128) * 128:(ti - T // 128 + 1) * 128, :]
                xi = xin[ti % 2]
                o = xo[ti % 2]
                k.dma("sp", xi.ap(), src, reads=[self.x], writes=[xi])
                for half in range(2):
                    pst = self.ps[(ti % 2) * 2 + half]
                    for c4 in range(4):
                        c = half * 4 + c4
                        k.op("pe", lambda e, c=c, c4=c4, pst=pst, xi=xi: e.transpose(
                            pst[:, c4 * 128:(c4 + 1) * 128], xi[:, c * 128:(c + 1) * 128], self.ident),
                            reads=[xi, self.cst], writes=[pst])
                    eng = "act" if half == 0 else "dve"
                    if eng == "act":
                        k.op("act", lambda e, pst=pst, o=o, half=half: e.copy(
                            out=o[:, half * 4:(half + 1) * 4, :], in_=pst.ap().rearrange("p (c t) -> p c t", t=128)),
                            reads=[pst], writes=[o.sub(half)])
                    else:
                        k.op("dve", lambda e, pst=pst, o=o, half=half: e.tensor_copy(
                            out=o[:, half * 4:(half + 1) * 4, :], in_=pst.ap().rearrange("p (c t) -> p c t", t=128)),
                            reads=[pst], writes=[o.sub(half)])
                k.dma("sp", XTv[:, :, ti * 128:(ti + 1) * 128], o.ap(),
                      reads=[o.sub(0), o.sub(1)], writes=[self.XT], join=True, sem=o.sub("st"))

    def stage_out_transpose(self, debug_ctx=False):
        k = self.k
        XTv = self.XT.ap().rearrange("(c p) t -> p c t", p=128)
        if debug_ctx:
            self.yc = k.dram("yc", [NCTX, D], F32, kind="ExternalOutput")
        with k.stage():
            xin = [k.tile([128, 8, 128], F32, "oin%d" % i) for i in range(2)]
            xo = [k.tile([128, D], F32, "oo%d" % i) for i in range(2)]
            for ti in range((NTOK if debug_ctx else T) // 128):
                xi = xin[ti % 2]
                o = xo[ti % 2]
                k.dma("sp", xi.ap(), XTv[:, :, ti * 128:(ti + 1) * 128], reads=[self.XT], writes=[xi])
                for half in range(2):
                    pst = self.ps[(ti % 2) * 2 + half]
                    for c4 in range(4):
                        c = half * 4 + c4
                        k.op("pe", lambda e, c=c, c4=c4, pst=pst, xi=xi: e.transpose(
                            pst[:, c4 * 128:(c4 + 1) * 128], xi[:, c, :], self.ident),
                            reads=[xi, self.cst], writes=[pst])
                    if half == 0:
                        k.op("act", lambda e, pst=pst, o=o, half=half: e.copy(
                            out=o[:, half * 512:(half + 1) * 512], in_=pst.ap()),
                            reads=[pst], writes=[o.sub(half)])
                    else:
                        k.op("dve", lambda e, pst=pst, o=o, half=half: e.tensor_copy(
                            out=o[:, half * 512:(half + 1) * 512], in_=pst.ap()),
                            reads=[pst], writes=[o.sub(half)])
                dsto = self.y[ti * 128:(ti + 1) * 128, :] if ti < T // 128 else \
                    self.yc[(ti - T // 128) * 128:(ti - T // 128 + 1) * 128, :]
                k.dma("sp", dsto, o.ap(),
                      reads=[o.sub(0), o.sub(1)], writes=[self.y], join=True, sem=o.sub("st"))

    def norm_block(self, X, h, off, n, l, nidx, tsel, sq, rs, ps_ss, h32=None):
        k = self.k
        m_shift = 0 if nidx == 0 else 3
        k.op("act", lambda e: e.activation(out=sq[:, :, 0:n], in_=X[:, :, off:off + n], func=AF.Square),
             reads=[X], writes=[sq])
        for c in range(8):
            k.op("pe", lambda e, c=c: e.matmul(ps_ss[:, 0:n], lhsT=self.ones_bf.ap(), rhs=sq[:, c, 0:n],
                                               start=(c == 0), stop=(c == 7)),
                 reads=[sq, self.ones_bf], writes=[ps_ss])
        k.op("act", lambda e: e.activation(out=rs[:, 0:n], in_=ps_ss[:, 0:n], func=AF.Sqrt,
                                           scale=1.0 / D, bias=self.eps_t.ap()),
             reads=[ps_ss, self.eps_t], writes=[rs])
        k.op("dve", lambda e: e.reciprocal(out=rs[:, 0:n], in_=rs[:, 0:n]), reads=[rs], writes=[rs])
        for c in range(8):
            if h32 is not None:
                k.op("dve", lambda e, c=c: e.scalar_tensor_tensor(
                    out=h32[:, c, 0:n], in0=X[:, c, off:off + n], scalar=self.gg(l, nidx, c, tsel),
                    in1=rs[:, 0:n], op0=ALU.mult, op1=ALU.mult), reads=[X, rs, self.GG], writes=[h32])
                k.op("act", lambda e, c=c: e.activation(
                    out=h32[:, c, 0:n], in_=h32[:, c, 0:n], func=AF.Identity,
                    bias=self.mod(l, m_shift, c, tsel), scale=1.0), reads=[h32, self.MODS], writes=[h32])
                k.op("pool", lambda e, c=c: e.tensor_copy(out=h[:, c, off:off + n], in_=h32[:, c, 0:n]),
                     reads=[h32], writes=[h])
            else:
                k.op("dve", lambda e, c=c: e.scalar_tensor_tensor(
                    out=h[:, c, off:off + n], in0=X[:, c, off:off + n], scalar=self.gg(l, nidx, c, tsel),
                    in1=rs[:, 0:n], op0=ALU.mult, op1=ALU.mult), reads=[X, rs, self.GG], writes=[h])
                k.op("act", lambda e, c=c: e.activation(
                    out=h[:, c, off:off + n], in_=h[:, c, off:off + n], func=AF.Identity,
                    bias=self.mod(l, m_shift, c, tsel), scale=1.0), reads=[h, self.MODS], writes=[h])

    def stage_ffn(self, l, moe):
        k = self.k
        nc = self.nc
        j = l // 2
        last = (l == DEPTH - 1)
        ntok = T if last else NTOK
        FF = FF_EXPERT if moe else FF_DENSE
        nfc = FF // 128
        FG = 4
        fgroups = [(f0, min(FG, nfc - f0)) for f0 in range(0, nfc, FG)]
        nexp = NEXP if moe else 1
        halves = [(0, ntok // 2), (ntok // 2, ntok)]
        XTv = self.XT.ap().rearrange("(c p) t -> p c t", p=128)
        for (h0, h1) in halves:
            nh = h1 - h0
            with k.stage():
                X = k.tile([128, 8, nh], F32, "X")
                H = k.tile([128, 8, nh], BF16, "H")
                GW = k.tile([8, nh], F32, "GW") if moe else None
                sel = k.tile([8, NEXP * 128], F32, "sel") if moe else None
                ph1 = k.stage()
                ph1.__enter__()
                sq = k.tile([128, 8, 512], BF16, "sq")
                rs = k.tile([128, 512], F32, "rs")
                blocks = []
                for (t0, n) in token_blocks(h0, h1):
                    if t0 < T < t0 + n:
                        blocks.append((t0, T - t0))
                        blocks.append((T, t0 + n - T))
                    else:
                        blocks.append((t0, n))
                for bi, (t0, n) in enumerate(blocks):
                    k.dma("sp", X[:, :, t0 - h0:t0 - h0 + n], XTv[:, :, t0:t0 + n],
                          reads=[self.XT], writes=[X.sub(bi)])
                if moe:
                    h32 = k.tile([128, 8, 512], F32, "h32")
                    wr = k.tile([128, 8, NEXP], F32, "wr")
                    with nc.allow_non_contiguous_dma(reason="small router weight"):
                        k.dma("sp", wr.ap(), self.moe_router[j].rearrange("(kk p) e -> p kk e", p=128),
                              reads=[self.moe_router], writes=[wr])
                    k.dma("sp", sel.ap(), self.consts[0:8, 512:512 + NEXP * 128], reads=[self.consts], writes=[sel])
                    lg = k.tile([128, 8], F32, "lg")
                    r1 = k.tile([128, 8], F32, "r1")
                    r2 = k.tile([128, 8], F32, "r2")
                    m1 = k.tile([128, 1], F32, "m1")
                    m2 = k.tile([128, 1], F32, "m2")
                    gwt = k.tile([128, 8], F32, "gwt")
                for bi, (t0, n) in enumerate(blocks):
                    tsel = 0 if t0 < T else 1
                    off = t0 - h0
                    self.norm_block(_SubView(X, bi), H, off, n, l, 1, tsel, sq, rs, self.ps[0],
                                    h32=(h32 if moe else None))
                    if moe:
                        for s in range(n // 128):
                            pl = self.ps[1]
                            for kk in range(8):
                                k.op("pe", lambda e, kk=kk, s=s, pl=pl: e.matmul(
                                    pl[:, 0:8], lhsT=h32[:, kk, s * 128:(s + 1) * 128], rhs=wr[:, kk, :],
                                    start=(kk == 0), stop=(kk == 7)), reads=[h32, wr], writes=[pl])
                            k.op("dve", lambda e, pl=pl: e.tensor_copy(out=lg.ap(), in_=pl[:, 0:8]),
                                 reads=[pl], writes=[lg])
                            k.op("dve", lambda e: e.reduce_max(out=m1.ap(), in_=lg.ap(), axis=mybir.AxisListType.X),
                                 reads=[lg], writes=[m1])
                            k.op("dve", lambda e: e.tensor_scalar(out=r1.ap(), in0=lg.ap(), scalar1=m1.ap(),
                                                                  scalar2=None, op0=ALU.is_ge),
                                 reads=[lg, m1], writes=[r1])
                            k.op("dve", lambda e: e.scalar_tensor_tensor(out=r2.ap(), in0=r1.ap(), scalar=-1e30,
                                                                         in1=lg.ap(), op0=ALU.mult, op1=ALU.add),
                                 reads=[r1, lg], writes=[r2])
                            k.op("dve", lambda e: e.reduce_max(out=m2.ap(), in_=r2.ap(), axis=mybir.AxisListType.X),
                                 reads=[r2], writes=[m2])
                            k.op("dve", lambda e: e.tensor_scalar(out=r1.ap(), in0=lg.ap(), scalar1=m2.ap(),
                                                                  scalar2=None, op0=ALU.is_ge),
                                 reads=[lg, m2], writes=[r1])
                            k.op("dve", lambda e: e.tensor_scalar(out=r2.ap(), in0=lg.ap(), scalar1=m1.ap(),
                                                                  scalar2=None, op0=ALU.subtract),
                                 reads=[lg, m1], writes=[r2])
                            k.op("act", lambda e: e.activation(out=r2.ap(), in_=r2.ap(), func=AF.Exp),
                                 reads=[r2], writes=[r2])
                            k.op("dve", lambda e: e.tensor_tensor(out=gwt.ap(), in0=r1.ap(), in1=r2.ap(), op=ALU.mult),
                                 reads=[r1, r2], writes=[gwt])
                            k.op("dve", lambda e: e.reduce_sum(out=m2.ap(), in_=gwt.ap(), axis=mybir.AxisListType.X),
                                 reads=[gwt], writes=[m2])
                            k.op("dve", lambda e: e.reciprocal(out=m2.ap(), in_=m2.ap()), reads=[m2], writes=[m2])
                            k.op("dve", lambda e: e.tensor_scalar(out=gwt.ap(), in0=gwt.ap(), scalar1=m2.ap(),
                                                                  scalar2=None, op0=ALU.mult),
                                 reads=[gwt, m2], writes=[gwt])
                            pt = self.ps[2]
                            k.op("pe", lambda e, pt=pt: e.transpose(pt[0:8, 0:128], gwt.ap(), self.ident),
                                 reads=[gwt, self.cst], writes=[pt])
                            k.op("act", lambda e, pt=pt, s=s, off=off: e.copy(
                                out=GW[:, off + s * 128:off + (s + 1) * 128], in_=pt[0:8, 0:128]),
                                reads=[pt], writes=[GW])
                ph1.__exit__(None, None, None)
                ph2 = k.stage()
                ph2.__enter__()
                self.alloc_stg()
                w13b = [k.tile([128, 8, 2, FG * 128], BF16, "w13b%d" % i) for i in range(2)]
                w2b = [k.tile([128, FG, D], BF16, "w2b%d" % i) for i in range(2)]
                actb = [k.tile([128, FG, 512], BF16, "actb%d" % i) for i in range(2)]
                sg = [k.tile([128, 512], F32, "sg%d" % i) for i in range(2)]
                it = 0
                ai = 0
                pi = 0
                pend = None

                def emit_w2(bi, t0, n, off, act, w2, nf):
                    tsel = 0 if t0 < T else 1
                    for dc in range(8):
                        po = self.ps[4 + dc % 2]
                        for f in range(nf):
                            k.op("pe", lambda e, f=f, dc=dc, po=po: e.matmul(
                                po[:, 0:n], lhsT=w2[:, f, dc * 128:(dc + 1) * 128], rhs=act[:, f, 0:n],
                                start=(f == 0), stop=(f == nf - 1)), reads=[w2, act], writes=[po])
                        k.op("dve", lambda e, dc=dc, po=po: e.scalar_tensor_tensor(
                            out=X[:, dc, off:off + n], in0=po[:, 0:n], scalar=self.mod(l, 5, dc, tsel),
                            in1=X[:, dc, off:off + n], op0=ALU.mult, op1=ALU.add),
                            reads=[po, self.MODS, X.sub(bi)], writes=[X.sub(bi)])

                for ex in range(nexp):
                    if moe:
                        w13src = self.moe_w13[j, ex]
                        w2src = self.moe_w2[j, ex]
                    else:
                        w13src = self.ffn_w13[j]
                        w2src = self.ffn_w2[j]
                    for (f0, nf) in fgroups:
                        w13 = w13b[it % 2]
                        w2 = w2b[it % 2]
                        it += 1
                        w13v = w13src.rearrange("(kk p) n -> p kk n", p=128)
                        for gu in range(2):
                            for kk in range(0, 8, 4):
                                self.cast_load(w13[:, kk:kk + 4, gu, 0:nf * 128],
                                               w13v[:, kk:kk + 4, gu * FF + f0 * 128: gu * FF + (f0 + nf) * 128],
                                               "p (a b) -> p a b", w13, first=(gu == 0 and kk == 0), a=4)
                        w2v = w2src[f0 * 128:(f0 + nf) * 128, :].rearrange("(f p) n -> p f n", p=128)
                        for f2 in range(0, nf, 2):
                            self.cast_load(w2[:, f2:f2 + 2, :], w2v[:, f2:f2 + 2, :], "p (a b) -> p a b", w2,
                                           first=(f2 == 0), a=2)
                        for bi, (t0, n) in enumerate(blocks):
                            off = t0 - h0
                            act = actb[ai % 2]
                            ai += 1
                            pgw = None
                            if moe:
                                pgw = self.ps[6 + ai % 2]
                                k.op("pe", lambda e, ex=ex, pgw=pgw, n=n, off=off: e.matmul(
                                    pgw[:, 0:n], lhsT=sel[:, ex * 128:(ex + 1) * 128], rhs=GW[:, off:off + n],
                                    start=True, stop=True), reads=[sel, GW], writes=[pgw])
                            for f in range(nf):
                                pg = self.ps[(pi % 2) * 2]
                                pu = self.ps[(pi % 2) * 2 + 1]
                                s_ = sg[pi % 2]
                                pi += 1
                                for kk in range(8):
                                    k.op("pe", lambda e, kk=kk, f=f, pg=pg, w13=w13, n=n, off=off: e.matmul(
                                        pg[:, 0:n], lhsT=w13[:, kk, 0, f * 128:(f + 1) * 128],
                                        rhs=H[:, kk, off:off + n], start=(kk == 0), stop=(kk == 7)),
                                        reads=[w13, H], writes=[pg])
                                for kk in range(8):
                                    k.op("pe", lambda e, kk=kk, f=f, pu=pu, w13=w13, n=n, off=off: e.matmul(
                                        pu[:, 0:n], lhsT=w13[:, kk, 1, f * 128:(f + 1) * 128],
                                        rhs=H[:, kk, off:off + n], start=(kk == 0), stop=(kk == 7)),
                                        reads=[w13, H], writes=[pu])
                                k.op("act", lambda e, pg=pg, s_=s_, n=n: e.activation(out=s_[:, 0:n], in_=pg[:, 0:n],
                                                                                     func=AF.Silu),
                                     reads=[pg], writes=[s_])
                                if moe:
                                    k.op("dve", lambda e, pu=pu, s_=s_, n=n: e.tensor_tensor(
                                        out=s_[:, 0:n], in0=s_[:, 0:n], in1=pu[:, 0:n], op=ALU.mult),
                                        reads=[s_, pu], writes=[s_])
                                    k.op("dve", lambda e, f=f, act=act, s_=s_, pgw=pgw, n=n: e.tensor_tensor(
                                        out=act[:, f, 0:n], in0=s_[:, 0:n], in1=pgw[:, 0:n], op=ALU.mult),
                                        reads=[s_, pgw], writes=[act])
                                else:
                                    k.op("dve", lambda e, f=f, act=act, pu=pu, s_=s_, n=n: e.tensor_tensor(
                                        out=act[:, f, 0:n], in0=s_[:, 0:n], in1=pu[:, 0:n], op=ALU.mult),
                                        reads=[s_, pu], writes=[act])
                            if pend is not None:
                                emit_w2(*pend)
                            pend = (bi, t0, n, off, act, w2, nf)
                if pend is not None:
                    emit_w2(*pend)
                ph2.__exit__(None, None, None)
                for bi, (t0, n) in enumerate(blocks):
                    k.dma("sp", XTv[:, :, t0:t0 + n], X[:, :, t0 - h0:t0 - h0 + n],
                          reads=[X.sub(bi)], writes=[self.XT], join=True, sem=X.sub(bi))


    def load_norm_all(self, l, nidx, ntok, H):
        k = self.k
        XTv = self.XT.ap().rearrange("(c p) t -> p c t", p=128)
        xb = [k.tile([128, 8, 512], F32, "xb%d" % i) for i in range(2)]
        sq = k.tile([128, 8, 512], BF16, "sq")
        rs = k.tile([128, 512], F32, "rs")
        for bi, (t0, n) in enumerate(token_blocks(0, ntok)):
            X = xb[bi % 2]
            k.dma("sp", X[:, :, 0:n], XTv[:, :, t0:t0 + n], reads=[self.XT], writes=[X])
            tsel = 0 if t0 < T else 1
            self.norm_block(_Shift(X, -t0), H, t0, n, l, nidx, tsel, sq, rs, self.ps[7])

    def stage_outproj(self, l, wsrc, OT, ntok):
        k = self.k
        XTv = self.XT.ap().rearrange("(c p) t -> p c t", p=128)
        OTv = OT.ap().rearrange("(c p) t -> p c t", p=128)
        with k.stage():
            W = k.tile([128, 8, D], BF16, "wo")
            self.alloc_stg()
            for kk in range(0, 8, 2):
                self.cast_load(W[:, kk:kk + 2, :], wsrc.rearrange("(kk p) n -> p kk n", p=128)[:, kk:kk + 2, :],
                               "p (a b) -> p a b", W, first=(kk == 0), a=2)
            xb = [k.tile([128, 8, 512], F32, "xb%d" % i) for i in range(2)]
            ob = [k.tile([128, 8, 512], BF16, "ob%d" % i) for i in range(2)]
            for bi, (t0, n) in enumerate(token_blocks(0, ntok)):
                X = xb[bi % 2]
                O = ob[bi % 2]
                tsel = 0 if t0 < T else 1
                k.dma("sp", X[:, :, 0:n], XTv[:, :, t0:t0 + n], reads=[self.XT], writes=[X])
                k.dma("sp", O[:, :, 0:n], OTv[:, :, t0:t0 + n], reads=[OT], writes=[O])
                for dc in range(8):
                    po = self.ps[dc % 4]
                    for kk in range(8):
                        k.op("pe", lambda e, kk=kk, dc=dc, po=po, O=O: e.matmul(
                            po[:, 0:n], lhsT=W[:, kk, dc * 128:(dc + 1) * 128], rhs=O[:, kk, 0:n],
                            start=(kk == 0), stop=(kk == 7)), reads=[W, O], writes=[po])
                    k.op("dve", lambda e, dc=dc, po=po, X=X, tsel=tsel: e.scalar_tensor_tensor(
                        out=X[:, dc, 0:n], in0=po[:, 0:n], scalar=self.mod(l, 2, dc, tsel),
                        in1=X[:, dc, 0:n], op0=ALU.mult, op1=ALU.add), reads=[po, self.MODS, X], writes=[X])
                k.dma("sp", XTv[:, :, t0:t0 + n], X[:, :, 0:n], reads=[X], writes=[self.XT], join=True, sem=X.sub("st"))

    def stage_na(self, l):
        k = self.k
        nc = self.nc
        j = l // 2
        last = (l == DEPTH - 1)
        OT = self.OT
        with k.stage():
            H = k.tile([128, 8, NTOK], BF16, "H")
            with k.stage():
                self.load_norm_all(l, 0, NTOK, H)
            gq = k.tile([128, 1], F32, "gq")
            gk = k.tile([128, 1], F32, "gk")
            with nc.allow_non_contiguous_dma(reason="tiny"):
                for hh in range(2):
                    k.dma("sp", gq[hh * 64:(hh + 1) * 64, :], self.na_q_norm[j].rearrange("(p o) -> p o", o=1),
                          reads=[self.na_q_norm], writes=[gq], join=(hh > 0))
                    k.dma("sp", gk[hh * 64:(hh + 1) * 64, :], self.na_k_norm[j].rearrange("(p o) -> p o", o=1),
                          reads=[self.na_k_norm], writes=[gk], join=(hh > 0))
            k.op("dve", lambda e: e.tensor_scalar(out=gq.ap(), in0=gq.ap(), scalar1=0.125, scalar2=None, op0=ALU.mult),
                 reads=[gq], writes=[gq])
            bd = k.tile([128, 128], BF16, "bd")
            k.op("dve", lambda e: e.memset(bd.ap(), 0.0), writes=[bd])
            for hh in range(2):
                k.op("dve", lambda e, hh=hh: e.memset(bd[hh * 64:(hh + 1) * 64, hh * 64:(hh + 1) * 64], 1.0 / 64),
                     writes=[bd])
            self.alloc_stg()
            wq = [k.tile([128, 8, 3, 128], BF16, "wqkv%d" % i) for i in range(2)]
            QT = k.tile([128, 2, NTOK], BF16, "QT")
            k.op("pool", lambda e: e.memset(QT.ap(), 0.0), writes=[QT])
            KT = k.tile([128, NTOK], BF16, "KT")
            V = k.tile([128, NTOK // 128, 2, 64], BF16, "V")
            OTs = k.tile([128, NTOK], BF16, "OTs")
            BI = k.tile([128, 9, 2, 320], F32, "BI")
            qf = [k.tile([128, 512], F32, "qf%d" % i) for i in range(2)]
            qs = [k.tile([128, 512], BF16, "qs%d" % i) for i in range(2)]
            qr = [k.tile([128, 512], F32, "qr%d" % i) for i in range(2)]
            ET = [k.tile([128, 512], BF16, "ET%d" % i) for i in range(4)]
            rd = [k.tile([128, 256], F32, "rd%d" % i) for i in range(4)]
            w_in = self.na_w_in[j].rearrange("(kk p) n -> p kk n", p=128)
            OTv = OT.ap().rearrange("(c p) t -> p c t", p=128)
            cnt = 0
            for hp in range(getattr(self, "na_hp_limit", 8)):
                w = wq[hp % 2]
                for part in range(3):
                    self.cast_load(w[:, :, part, :], w_in[:, :, part * D + hp * 128: part * D + (hp + 1) * 128],
                                   "p (a b) -> p a b", w, first=(part == 0), a=8)
                if getattr(self, "na_cut", 9) > 2:
                  k.dma("sp", BI.ap().rearrange("p r h q -> p r (h q)"),
                      self.na_bias[j].rearrange("r p (h q) -> p r h q", h=16)[:, :, 2 * hp:2 * hp + 2, :].rearrange("p r h q -> p r (h q)"),
                      reads=[self.na_bias], writes=[BI])
                for part, (dst, gcol) in enumerate(((QT, gq), (KT, gk))):
                    for bi, (t0, n) in enumerate(token_blocks(0, NTOK if getattr(self, "na_cut", 9) > 0 else 0)):
                        pp = self.ps[cnt % 2]
                        pm = self.ps[2 + cnt % 2]
                        f_ = qf[cnt % 2]
                        s_ = qs[cnt % 2]
                        r_ = qr[cnt % 2]
                        cnt += 1
                        for kk in range(8):
                            k.op("pe", lambda e, kk=kk, pp=pp, w=w, part=part: e.matmul(
                                pp[:, 0:n], lhsT=w[:, kk, part, :], rhs=H[:, kk, t0:t0 + n],
                                start=(kk == 0), stop=(kk == 7)), reads=[w, H], writes=[pp])
                        k.op("act", lambda e, pp=pp, s_=s_: e.activation(out=s_[:, 0:n], in_=pp[:, 0:n], func=AF.Square),
                             reads=[pp], writes=[s_])
                        k.op("dve", lambda e, pp=pp, f_=f_: e.tensor_copy(out=f_[:, 0:n], in_=pp[:, 0:n]),
                             reads=[pp], writes=[f_])
                        k.op("pe", lambda e, pm=pm, s_=s_: e.matmul(pm[:, 0:n], lhsT=bd.ap(), rhs=s_[:, 0:n],
                                                                  start=True, stop=True), reads=[bd, s_], writes=[pm])
                        k.op("act", lambda e, pm=pm, r_=r_: e.activation(out=r_[:, 0:n], in_=pm[:, 0:n], func=AF.Sqrt,
                                                                        scale=1.0, bias=self.eps_t.ap()),
                             reads=[pm, self.eps_t], writes=[r_])
                        k.op("dve", lambda e, r_=r_: e.reciprocal(out=r_[:, 0:n], in_=r_[:, 0:n]), reads=[r_], writes=[r_])
                        if part == 0 and not getattr(self, 'na_dbg_full', False):
                            for hl in range(2):
                                hs = slice(hl * 64, (hl + 1) * 64)
                                k.op("dve", lambda e, f_=f_, r_=r_, hs=hs, hl=hl: e.scalar_tensor_tensor(
                                    out=QT[hs, hl, t0:t0 + n], in0=f_[hs, 0:n], scalar=gq[hs, :], in1=r_[hs, 0:n],
                                    op0=ALU.mult, op1=ALU.mult), reads=[f_, r_, gq], writes=[QT])
                        else:
                            dstv = dst[:, 0, :] if part == 0 else dst
                            k.op("dve", lambda e, f_=f_, r_=r_, dst=dstv, gcol=gcol: e.scalar_tensor_tensor(
                                out=dst[:, t0:t0 + n], in0=f_[:, 0:n], scalar=gcol.ap(), in1=r_[:, 0:n],
                                op0=ALU.mult, op1=ALU.mult), reads=[f_, r_, gcol], writes=[dst])
                for ti in range(NTOK // 128 if getattr(self, "na_cut", 9) > 1 else 0):
                    pv = self.ps[4 + ti % 2]
                    for kk in range(8):
                        k.op("pe", lambda e, kk=kk, pv=pv, w=w, ti=ti: e.matmul(
                            pv[:, 0:128], lhsT=H[:, kk, ti * 128:(ti + 1) * 128], rhs=w[:, kk, 2, :],
                            start=(kk == 0), stop=(kk == 7)), reads=[w, H], writes=[pv])
                    eng = "act" if ti % 2 == 0 else "dve"
                    if eng == "act":
                        k.op("act", lambda e, pv=pv, ti=ti: e.copy(out=V[:, ti, :, :].rearrange("p h d -> p (h d)"),
                                                                  in_=pv[:, 0:128]), reads=[pv], writes=[V])
                    else:
                        k.op("dve", lambda e, pv=pv, ti=ti: e.tensor_copy(out=V[:, ti, :, :].rearrange("p h d -> p (h d)"),
                                                                         in_=pv[:, 0:128]), reads=[pv], writes=[V])
                if getattr(self, "na_cut", 9) <= 2:
                    continue
                groups = []
                for r in range(64):
                    rs_ = min(max(r - 4, 0), 56)
                    tb = rs_ // 2
                    nt = 4 if rs_ % 2 == 0 else 5
                    if r < 4:
                        rt = r
                    elif r > 60:
                        rt = r - 56 + 1
                    else:
                        rt = 4 + (rs_ % 2)
                    groups.append((r * 64, 64, [tb + i for i in range(nt)] + [32, 33], rt, nt))
                if not last and getattr(self, "na_cut", 9) > 3:
                    groups.append((T, 256, [32, 33], None, 0))
                def att_chain(q0, nq, tiles, rt, nt, hl, ci):
                    pS = self.ps[ci % 4]
                    pO = self.ps[4 + ci % 4]
                    et = ET[ci % 4]
                    rd_ = rd[ci % 4]
                    hs = slice(hl * 64, (hl + 1) * 64)
                    W_ = len(tiles) * nq
                    for si, tl in enumerate(tiles):
                        k.op("pe", lambda e, si=si, tl=tl: e.matmul(
                            pS[:, si * nq:(si + 1) * nq], lhsT=KT[:, tl * 128:(tl + 1) * 128],
                            rhs=QT[:, hl, q0:q0 + nq], start=True, stop=True), reads=[KT, QT], writes=[pS])
                    if rt is not None:
                        k.op("dve", lambda e: e.tensor_tensor(
                            out=pS[:, 0:nt * 64], in0=pS[:, 0:nt * 64], in1=BI[:, rt, hl, 0:nt * 64], op=ALU.add),
                            reads=[pS, BI], writes=[pS])
                    k.op("act", lambda e: e.activation(out=et[:, 0:W_], in_=pS[:, 0:W_], func=AF.Exp),
                         reads=[pS], writes=[et])
                    yield
                    for si, tl in enumerate(tiles):
                        k.op("pe", lambda e, si=si, tl=tl: e.matmul(
                            pO[hs, 0:nq], lhsT=V[:, tl, hl, :], rhs=et[:, si * nq:(si + 1) * nq],
                            start=(si == 0), stop=(si == len(tiles) - 1)), reads=[V, et], writes=[pO])
                    for si, tl in enumerate(tiles):
                        k.op("pe", lambda e, si=si: e.matmul(
                            pO[hs, 256:256 + nq], lhsT=self.ones_bf[:, 0:64], rhs=et[:, si * nq:(si + 1) * nq],
                            start=(si == 0), stop=(si == len(tiles) - 1)), reads=[self.ones_bf, et], writes=[pO])
                    yield
                    k.op("dve", lambda e: e.reciprocal(out=rd_[hs, 0:nq], in_=pO[hs, 256:256 + nq]),
                         reads=[pO], writes=[rd_])
                    k.op("dve", lambda e: e.tensor_tensor(
                        out=OTs[hs, q0:q0 + nq], in0=pO[hs, 0:nq], in1=rd_[hs, 0:nq], op=ALU.mult),
                        reads=[pO, rd_], writes=[OTs.sub(hl)])
                    yield

                todo = [(q0, nq, tiles, rt, nt, hl) for (q0, nq, tiles, rt, nt) in groups for hl in range(2)]
                active = []
                ci = 0
                while todo or active:
                    while todo and len(active) < 3:
                        active.append(att_chain(*todo.pop(0), ci))
                        ci += 1
                    for g_ in list(active):
                        try:
                            next(g_)
                        except StopIteration:
                            active.remove(g_)
                ntk = T if last else NTOK
                for c0 in range(0, ntk, 1024):
                    c1 = min(ntk, c0 + 1024)
                    k.dma("sp", OTv[:, hp, c0:c1], OTs[:, c0:c1], reads=[OTs.sub(0), OTs.sub(1)], writes=[OT],
                          join=True, sem=OTs.sub("st"))
        self.stage_outproj(l, self.na_w_out[j], OT, T if last else NTOK)


    def stage_gdn(self, l):
        j = l // 2
        if not hasattr(self, "QKVT"):
            k = self.k
            self.QKVT = k.dram("QKVT", [3 * D, NTOK], F32)
            self.GATE = k.dram("GATE", [NTOK, D], BF16)
            self.GB = k.dram("GB", [NTOK, 32], F32)
            self.OD = k.dram("OD", [2, NTOK, D], F32)
        self.gdn_proj(l, j)
        self.gdn_scan(l, j)
        self.gdn_finish(l, j)
        self.stage_outproj(l, self.gdn_w_out[j], self.OT, NTOK)

    def gdn_proj(self, l, j):
        k = self.k
        nc = self.nc
        w_in = self.gdn_w_in[j].rearrange("(kk p) n -> p kk n", p=128)
        QV = self.QKVT.ap().rearrange("(c p) t -> p c t", p=128)
        PADN = NTOK + 4
        with k.stage():
            H = k.tile([128, 8, NTOK], BF16, "H")
            with k.stage():
                self.load_norm_all(l, 0, NTOK, H)
            self.alloc_stg()
            cw = k.tile([128, 24, 3], F32, "cw")
            with nc.allow_non_contiguous_dma(reason="small conv weights"):
                for s_ in range(3):
                    k.dma("sp", cw[:, :, s_], self.gdn_conv_w[j, s_].rearrange("(c p) -> p c", p=128),
                          reads=[self.gdn_conv_w], writes=[cw], join=(s_ > 0))
            wb = [k.tile([128, 8, 128], BF16, "wfc%d" % i) for i in range(2)]
            PT = k.tile([128, PADN], F32, "PT")
            k.op("pool", lambda e: e.memset(PT.ap(), 0.0), writes=[PT])
            cv = [k.tile([128, 512], F32, "cv%d" % i) for i in range(2)]
            sv = [k.tile([128, 512], F32, "sv%d" % i) for i in range(2)]
            sqb = [k.tile([128, 512], BF16, "sqb%d" % i) for i in range(2)]
            rr = [k.tile([128, 512], F32, "rr%d" % i) for i in range(2)]
            cnt = 0
            for fc in range(24):
                w = wb[fc % 2]
                self.cast_load(w.ap(), w_in[:, :, fc * 128:(fc + 1) * 128], "p (a b) -> p a b", w, first=True, a=8)
                for (t0, n) in token_blocks(0, NTOK):
                    pp = self.ps[cnt % 2]
                    cnt += 1
                    for kk in range(8):
                        k.op("pe", lambda e, kk=kk, pp=pp, w=w: e.matmul(
                            pp[:, 0:n], lhsT=w[:, kk, :], rhs=H[:, kk, t0:t0 + n], start=(kk == 0), stop=(kk == 7)),
                            reads=[w, H], writes=[pp])
                    pc = 1 + t0 if t0 < T else 3 + t0
                    k.op("act", lambda e, pp=pp, pc=pc: e.copy(out=PT[:, pc:pc + n], in_=pp[:, 0:n]),
                         reads=[pp], writes=[PT])
                for (t0, n) in token_blocks(0, NTOK):
                    pc = t0 if t0 < T else 2 + t0
                    c_ = cv[cnt % 2]
                    s2 = sv[cnt % 2]
                    q_ = sqb[cnt % 2]
                    r_ = rr[cnt % 2]
                    pm = self.ps[2 + cnt % 2]
                    cnt += 1
                    k.op("dve", lambda e, c_=c_, pc=pc: e.tensor_scalar(
                        out=c_[:, 0:n], in0=PT[:, pc:pc + n], scalar1=cw[:, fc, 0:1], scalar2=None, op0=ALU.mult),
                        reads=[PT, cw], writes=[c_])
                    for s_ in (1, 2):
                        k.op("dve", lambda e, c_=c_, pc=pc, s_=s_: e.scalar_tensor_tensor(
                            out=c_[:, 0:n], in0=PT[:, pc + s_:pc + s_ + n], scalar=cw[:, fc, s_:s_ + 1], in1=c_[:, 0:n],
                            op0=ALU.mult, op1=ALU.add), reads=[PT, cw, c_], writes=[c_])
                    k.op("act", lambda e, c_=c_, s2=s2: e.activation(out=s2[:, 0:n], in_=c_[:, 0:n], func=AF.Silu),
                         reads=[c_], writes=[s2])
                    if fc < 16:
                        k.op("act", lambda e, s2=s2, q_=q_: e.activation(out=q_[:, 0:n], in_=s2[:, 0:n], func=AF.Square),
                             reads=[s2], writes=[q_])
                        k.op("pe", lambda e, pm=pm, q_=q_: e.matmul(pm[:, 0:n], lhsT=self.ones_bf.ap(), rhs=q_[:, 0:n],
                                                                  start=True, stop=True),
                             reads=[self.ones_bf, q_], writes=[pm])
                        k.op("act", lambda e, pm=pm, r_=r_: e.activation(out=r_[:, 0:n], in_=pm[:, 0:n], func=AF.Sqrt,
                                                                        scale=1.0, bias=self.eps_t.ap()),
                             reads=[pm, self.eps_t], writes=[r_])
                        k.op("dve", lambda e, r_=r_: e.reciprocal(out=r_[:, 0:n], in_=r_[:, 0:n]), reads=[r_], writes=[r_])
                        sc_ = (128.0 ** -0.5) if fc < 8 else 1.0
                        k.op("dve", lambda e, r_=r_, s2=s2, sc_=sc_: e.scalar_tensor_tensor(
                            out=s2[:, 0:n], in0=s2[:, 0:n], scalar=sc_, in1=r_[:, 0:n], op0=ALU.mult, op1=ALU.mult),
                            reads=[s2, r_], writes=[s2])
                    k.dma("sp", QV[:, fc, t0:t0 + n], s2[:, 0:n], reads=[s2], writes=[self.QKVT], join=True, sem=s2.sub("st"))
            WG = k.tile([128, 8, D], BF16, "WG")
            for kk in range(0, 8, 2):
                self.cast_load(WG[:, kk:kk + 2, :], w_in[:, kk:kk + 2, 3 * D:4 * D], "p (a b) -> p a b", WG,
                               first=(kk == 0), a=2)
            WAB = k.tile([128, 8, 32], BF16, "WAB")
            with nc.allow_non_contiguous_dma(reason="32-col slice"):
                self.cast_load(WAB.ap(), w_in[:, :, 4 * D:4 * D + 32], "p (a b) -> p a b", WAB, first=True, a=8)
            ALc = k.tile([128, 32], F32, "ALc")
            DTc = k.tile([128, 32], F32, "DTc")
            k.op("dve", lambda e: e.memset(ALc.ap(), 0.0), writes=[ALc])
            k.op("dve", lambda e: e.memset(DTc.ap(), 0.0), writes=[DTc])
            with nc.allow_non_contiguous_dma(reason="tiny broadcast"):
                for d_ in range(2):
                    k.dma("sp", ALc[:, d_ * 16:d_ * 16 + 8], self.gdn_a_log[j, d_].partition_broadcast(128),
                          reads=[self.gdn_a_log], writes=[ALc], join=(d_ > 0))
                    k.dma("sp", DTc[:, d_ * 16:d_ * 16 + 8], self.gdn_dt_bias[j, d_].partition_broadcast(128),
                          reads=[self.gdn_dt_bias], writes=[DTc], join=(d_ > 0))
            k.op("act", lambda e: e.activation(out=ALc.ap(), in_=ALc.ap(), func=AF.Exp), reads=[ALc], writes=[ALc])
            k.op("dve", lambda e: e.tensor_scalar(out=ALc.ap(), in0=ALc.ap(), scalar1=-1.0, scalar2=None, op0=ALU.mult),
                 reads=[ALc], writes=[ALc])
            gt = [k.tile([128, D], BF16, "gt%d" % i) for i in range(2)]
            xa = [k.tile([128, 32], F32, "xa%d" % i) for i in range(2)]
            xb_ = [k.tile([128, 32], F32, "xb_%d" % i) for i in range(2)]
            xc = [k.tile([128, 32], F32, "xc%d" % i) for i in range(2)]
            one_col = self.cst[:, 384:385]
            for ti in range(NTOK // 128):
                g_ = gt[ti % 2]
                for hf in range(2):
                    pg = self.ps[4 + hf]
                    for kk in range(8):
                        k.op("pe", lambda e, kk=kk, pg=pg, hf=hf: e.matmul(
                            pg[:, 0:512], lhsT=H[:, kk, ti * 128:(ti + 1) * 128], rhs=WG[:, kk, hf * 512:(hf + 1) * 512],
                            start=(kk == 0), stop=(kk == 7)), reads=[H, WG], writes=[pg])
                    k.op("act", lambda e, pg=pg, g_=g_, hf=hf: e.activation(out=g_[:, hf * 512:(hf + 1) * 512], in_=pg[:, 0:512],
                                                                           func=AF.Silu), reads=[pg], writes=[g_.sub(hf)])
                k.dma("sp", self.GATE[ti * 128:(ti + 1) * 128, :], g_.ap(), reads=[g_.sub(0), g_.sub(1)], writes=[self.GATE],
                      join=True, sem=g_.sub("st"))
                pa = self.ps[6 + ti % 2]
                a_ = xa[ti % 2]
                b_ = xb_[ti % 2]
                c_ = xc[ti % 2]
                for kk in range(8):
                    k.op("pe", lambda e, kk=kk, pa=pa: e.matmul(
                        pa[:, 0:32], lhsT=H[:, kk, ti * 128:(ti + 1) * 128], rhs=WAB[:, kk, :],
                        start=(kk == 0), stop=(kk == 7)), reads=[H, WAB], writes=[pa])
                k.op("dve", lambda e, pa=pa, a_=a_: e.tensor_tensor(out=a_.ap(), in0=pa[:, 0:32], in1=DTc.ap(), op=ALU.add),
                     reads=[pa, DTc], writes=[a_])
                k.op("dve", lambda e, a_=a_, b_=b_: e.scalar_tensor_tensor(out=b_.ap(), in0=a_.ap(), scalar=-1.0, in1=a_.ap(),
                                                                          op0=ALU.mult, op1=ALU.max), reads=[a_], writes=[b_])
                k.op("act", lambda e, b_=b_: e.activation(out=b_.ap(), in_=b_.ap(), func=AF.Exp, scale=-1.0),
                     reads=[b_], writes=[b_])
                k.op("act", lambda e, b_=b_, c_=c_: e.activation(out=c_.ap(), in_=b_.ap(), func=AF.Ln, bias=one_col, scale=1.0),
                     reads=[b_, self.cst], writes=[c_])
                k.op("dve", lambda e, a_=a_, c_=c_: e.scalar_tensor_tensor(out=c_.ap(), in0=a_.ap(), scalar=0.0, in1=c_.ap(),
                                                                          op0=ALU.max, op1=ALU.add), reads=[a_, c_], writes=[c_])
                k.op("dve", lambda e, c_=c_: e.tensor_tensor(out=c_.ap(), in0=c_.ap(), in1=ALc.ap(), op=ALU.mult),
                     reads=[c_, ALc], writes=[c_])
                k.op("act", lambda e, pa=pa, b_=b_: e.activation(out=b_.ap(), in_=pa[:, 0:32], func=AF.Sigmoid),
                     reads=[pa], writes=[b_])
                cvw = c_.ap().rearrange("p (d a h) -> p d a h", d=2, a=2)
                bvw = b_.ap().rearrange("p (d a h) -> p d a h", d=2, a=2)
                k.op("dve", lambda e, cvw=cvw, bvw=bvw, c_=c_, b_=b_: e.tensor_copy(out=cvw[:, :, 1, :], in_=bvw[:, :, 1, :]),
                     reads=[b_, c_], writes=[c_])
                k.dma("sp", self.GB[ti * 128:(ti + 1) * 128, :], c_.ap(), reads=[c_], writes=[self.GB], join=True,
                      sem=c_.sub("st"))

    def gdn_scan(self, l, j):
        k = self.k
        nc = self.nc
        QV = self.QKVT.ap().rearrange("(a h p) t -> a p h t", a=3, p=128)
        BIG = 30000.0
        with k.stage():
            f4 = lambda nm: k.tile([128, 8, 128], F32, nm)
            ones32 = self.cst[:, 384:512]
            tril = self.cst[:, 128:256]
            triu = self.cst[:, 256:384]
            I8 = f4("I8")
            for h in range(8):
                k.op("pool", lambda e, h=h: e.tensor_copy(out=I8[:, h, :], in_=self.ident), reads=[self.cst], writes=[I8])
            M2s, NM2T, U = [], [], []
            for d in range(2):
                a = k.tile([128, 128], F32, "M2s%d" % d)
                b = k.tile([128, 128], F32, "NM2T%d" % d)
                src_s = triu if d == 0 else tril
                k.op("dve", lambda e, a=a, src_s=src_s: e.tensor_scalar(out=a.ap(), in0=src_s, scalar1=BIG, scalar2=None,
                                                                        op0=ALU.mult), reads=[self.cst], writes=[a])
                src_t = triu if d == 0 else tril
                k.op("dve", lambda e, b=b, src_t=src_t: e.tensor_scalar(out=b.ap(), in0=src_t, scalar1=BIG, scalar2=-BIG,
                                                                        op0=ALU.mult, op1=ALU.add), reads=[self.cst], writes=[b])
                M2s.append(a)
                NM2T.append(b)
                U.append(triu if d == 0 else tril)
            inb = [[f4("in%d_%d" % (a_, i)) for a_ in range(3)] for i in range(2)]
            gbb = [k.tile([128, 32], F32, "gbb%d" % i) for i in range(2)]
            WS = []
            for d_ in range(2):
                w_ = {}
                for nm in ("gc", "egl", "ekd", "bge"):
                    w_[nm] = k.tile([128, 8], F32, "%s%d" % (nm, d_))
                for nm in ("Gd", "Dms", "DmT", "L0", "L1", "M0", "M1", "R", "vb", "kbg", "kd", "u", "wT", "vn", "qg", "QKd"):
                    w_[nm] = f4("%s%d" % (nm, d_))
                WS.append(w_)
            osb = [k.tile([128, D], F32, "osb%d" % i) for i in range(2)]
            S = [f4("S%d" % d) for d in range(2)]
            bctr = {}

            def nbank(key):
                d_, hf_ = key
                base = 2 * (2 * d_ + hf_)
                n_ = bctr.get(key, 0)
                bctr[key] = n_ + 1
                return self.ps[base + n_ % 2]

            def chain(d, ib, gb, ob, t0, hf):
                qT, kT, vT = ib
                hs = list(range(hf * 4, hf * 4 + 4))
                v4 = lambda t: t[:, hf * 4:(hf + 1) * 4, :].rearrange("p h d -> p (h d)")
                G = lambda h: gb[:, d * 16 + h:d * 16 + h + 1]
                Bt = lambda h: gb[:, d * 16 + 8 + h:d * 16 + 8 + h + 1]
                csl = lambda i: slice(i * 128, (i + 1) * 128)
                Sd = S[d]
                w_ = WS[d]
                gc, egl, ekd, bge = w_["gc"], w_["egl"], w_["ekd"], w_["bge"]
                Gd, Dms, DmT, R = w_["Gd"], w_["Dms"], w_["DmT"], w_["R"]
                Lb, Mb = [w_["L0"], w_["L1"]], [w_["M0"], w_["M1"]]
                vb, kbg, kd, u, wT, vn, qg, QKd = (w_[n_] for n_ in ("vb", "kbg", "kd", "u", "wT", "vn", "qg", "QKd"))
                nb = lambda: nbank((d, hf))

                def mm4(lhs_fn, rhs_fn, bank, reads, start=True, stop=True):
                    for i, h in enumerate(hs):
                        k.op("pe", lambda e, i=i, h=h: e.matmul(bank[:, csl(i)], lhsT=lhs_fn(h), rhs=rhs_fn(h), start=start, stop=stop),
                             reads=list(reads), writes=[bank])

                def ev(dst, bank, eng):
                    if eng == "act":
                        k.op("act", lambda e: e.copy(out=v4(dst), in_=bank.ap()), reads=[bank], writes=[dst.sub(hf)])
                    else:
                        k.op("dve", lambda e: e.tensor_copy(out=v4(dst), in_=bank.ap()), reads=[bank], writes=[dst.sub(hf)])

                for h in hs:
                    k.op("act", lambda e, h=h: e.activation(out=Gd[:, h, :], in_=U[d], func=AF.Copy, scale=G(h)),
                         reads=[self.cst, gb], writes=[Gd.sub(hf)])
                yield
                bB = nb()
                mm4(lambda h: ones32, lambda h: Gd[:, h, :], bB, [self.cst, Gd.sub(hf)])
                yield
                for i, h in enumerate(hs):
                    k.op("dve", lambda e, h=h, i=i: e.scalar_tensor_tensor(
                        out=Dms[:, h, :], in0=bB[:, csl(i)], scalar=gc[:, h:h + 1], in1=M2s[d].ap(),
                        op0=ALU.subtract, op1=ALU.max), reads=[bB, gc, M2s[d]], writes=[Dms.sub(hf)])
                    k.op("dve", lambda e, h=h, i=i: e.scalar_tensor_tensor(
                        out=DmT[:, h, :], in0=bB[:, csl(i)], scalar=gc[:, h:h + 1], in1=NM2T[d].ap(),
                        op0=ALU.subtract, op1=ALU.min), reads=[bB, gc, NM2T[d]], writes=[DmT.sub(hf)])
                yield
                k.op("act", lambda e: e.activation(out=v4(qg), in_=bB.ap(), func=AF.Exp), reads=[bB], writes=[qg.sub(hf)])
                k.op("act", lambda e: e.activation(out=v4(Dms), in_=v4(Dms), func=AF.Exp, scale=-1.0), reads=[Dms.sub(hf)],
                     writes=[Dms.sub(hf)])
                k.op("act", lambda e: e.activation(out=v4(DmT), in_=v4(DmT), func=AF.Exp), reads=[DmT.sub(hf)], writes=[DmT.sub(hf)])
                bK = nb()
                mm4(lambda h: kT[:, h, :], lambda h: kT[:, h, :], bK, [kT])
                yield
                k.op("dve", lambda e: e.tensor_tensor(out=v4(qg), in0=v4(qg), in1=v4(qT), op=ALU.mult),
                     reads=[qg.sub(hf), qT], writes=[qg.sub(hf)])
                L, M = Lb[0], Mb[0]
                for i, h in enumerate(hs):
                    k.op("dve", lambda e, h=h, i=i: e.scalar_tensor_tensor(
                        out=L[:, h, :], in0=bK[:, csl(i)], scalar=Bt(h), in1=Dms[:, h, :], op0=ALU.mult, op1=ALU.mult),
                        reads=[bK, gb, Dms.sub(hf)], writes=[L.sub(hf)])
                yield
                bM = nb()
                for i, h in enumerate(hs):
                    k.op("pe", lambda e, h=h, i=i: e.transpose(bM[:, csl(i)], L[:, h, :], self.ident),
                         reads=[L.sub(hf), self.cst], writes=[bM])
                yield
                ev(M, bM, "act")
                k.op("dve", lambda e: e.tensor_tensor(out=v4(R), in0=v4(I8), in1=bM.ap(), op=ALU.subtract),
                     reads=[bM, I8], writes=[R.sub(hf)])
                yield
                for lev in range(1, 7):
                    Lp, Mp = Lb[(lev - 1) % 2], Mb[(lev - 1) % 2]
                    Ln_, Mn_ = Lb[lev % 2], Mb[lev % 2]
                    bL = nb()
                    mm4(lambda h: Mp[:, h, :], lambda h: Lp[:, h, :], bL, [Mp.sub(hf), Lp.sub(hf)])
                    if lev < 6:
                        bN = nb()
                        mm4(lambda h: Lp[:, h, :], lambda h: Mp[:, h, :], bN, [Mp.sub(hf), Lp.sub(hf)])
                    yield
                    ev(Ln_, bL, "act")
                    if lev < 6:
                        ev(Mn_, bN, "dve")
                    yield
                    bR = nb()
                    mm4(lambda h: Ln_[:, h, :], lambda h: R[:, h, :], bR, [Ln_.sub(hf), R.sub(hf)])
                    yield
                    k.op("dve", lambda e, bR=bR: e.tensor_tensor(out=v4(R), in0=v4(R), in1=bR.ap(), op=ALU.add),
                         reads=[bR, R.sub(hf)], writes=[R.sub(hf)])
                    yield
                bT = nb()
                for i, h in enumerate(hs):
                    k.op("pe", lambda e, h=h, i=i: e.transpose(bT[:, csl(i)], kT[:, h, :], self.ident), reads=[kT, self.cst], writes=[bT])
                bV = nb()
                for i, h in enumerate(hs):
                    k.op("pe", lambda e, h=h, i=i: e.transpose(bV[:, csl(i)], vT[:, h, :], self.ident), reads=[vT, self.cst], writes=[bV])
                yield
                for i, h in enumerate(hs):
                    k.op("act", lambda e, h=h, i=i: e.activation(out=kbg[:, h, :], in_=bT[:, csl(i)], func=AF.Copy,
                                                                scale=bge[:, h:h + 1]), reads=[bT, bge], writes=[kbg.sub(hf)])
                    k.op("dve", lambda e, h=h, i=i: e.tensor_scalar(out=kd[:, h, :], in0=bT[:, csl(i)], scalar1=ekd[:, h:h + 1],
                                                                  scalar2=None, op0=ALU.mult), reads=[bT, ekd], writes=[kd.sub(hf)])
                yield
                for i, h in enumerate(hs):
                    k.op("act", lambda e, h=h, i=i: e.activation(out=vb[:, h, :], in_=bV[:, csl(i)], func=AF.Copy, scale=Bt(h)),
                         reads=[bV, gb], writes=[vb.sub(hf)])
                yield
                bU = nb()
                mm4(lambda h: R[:, h, :], lambda h: vb[:, h, :], bU, [R.sub(hf), vb.sub(hf)])
                bW = nb()
                mm4(lambda h: kbg[:, h, :], lambda h: R[:, h, :], bW, [R.sub(hf), kbg.sub(hf)])
                yield
                ev(u, bU, "act")
                ev(wT, bW, "dve")
                bQ = nb()
                mm4(lambda h: kT[:, h, :], lambda h: qT[:, h, :], bQ, [kT, qT])
                yield
                k.op("dve", lambda e: e.tensor_tensor(out=v4(QKd), in0=bQ.ap(), in1=v4(DmT), op=ALU.mult),
                     reads=[bQ, DmT.sub(hf)], writes=[QKd.sub(hf)])
                bS = nb()
                mm4(lambda h: wT[:, h, :], lambda h: Sd[:, h, :], bS, [wT.sub(hf), Sd.sub(hf)])
                yield
                k.op("dve", lambda e: e.tensor_tensor(out=v4(vn), in0=v4(u), in1=bS.ap(), op=ALU.subtract),
                     reads=[bS, u.sub(hf)], writes=[vn.sub(hf)])
                yield
                bO = nb()
                for i, h in enumerate(hs):
                    k.op("pe", lambda e, h=h, i=i: e.matmul(bO[:, csl(i)], lhsT=qg[:, h, :], rhs=Sd[:, h, :], start=True, stop=False),
                         reads=[qg.sub(hf), Sd.sub(hf)], writes=[bO])
                    k.op("pe", lambda e, h=h, i=i: e.matmul(bO[:, csl(i)], lhsT=QKd[:, h, :], rhs=vn[:, h, :], start=False, stop=True),
                         reads=[QKd.sub(hf), vn.sub(hf)], writes=[bO])
                bN2 = nb()
                mm4(lambda h: kd[:, h, :], lambda h: vn[:, h, :], bN2, [kd.sub(hf), vn.sub(hf)])
                yield
                k.op("act", lambda e: e.copy(out=ob[:, hf * 512:(hf + 1) * 512], in_=bO.ap()), reads=[bO], writes=[ob.sub(hf)])
                k.dma("sp", self.OD[d, t0:t0 + 128, hf * 512:(hf + 1) * 512], ob[:, hf * 512:(hf + 1) * 512],
                      reads=[ob.sub(hf)], writes=[self.OD], join=True, sem=ob.sub("st%d" % hf))
                for i, h in enumerate(hs):
                    k.op("dve", lambda e, h=h, i=i: e.scalar_tensor_tensor(
                        out=Sd[:, h, :], in0=Sd[:, h, :], scalar=egl[:, h:h + 1], in1=bN2[:, csl(i)], op0=ALU.mult, op1=ALU.add),
                        reads=[bN2, egl, Sd.sub(hf)], writes=[Sd.sub(hf)])
                yield

            it = 0
            order = {0: [32, 33] + list(range(32)), 1: [33, 32] + list(range(31, -1, -1))}
            for step in range(34):
                gens = []
                for d in range(2):
                    c = order[d][step]
                    t0 = c * 128
                    ib = inb[d]
                    gb = gbb[d]
                    ob = osb[d]
                    w_ = WS[d]
                    gc, egl, ekd, bge = w_["gc"], w_["egl"], w_["ekd"], w_["bge"]
                    for a_ in range(3):
                        k.dma("sp", ib[a_].ap(), QV[a_, :, :, t0:t0 + 128], reads=[self.QKVT], writes=[ib[a_]])
                    k.dma("sp", gb.ap(), self.GB[t0:t0 + 128, :], reads=[self.GB], writes=[gb])
                    if step == 0:
                        for hf in range(2):
                            k.op("pool", lambda e, d=d, hf=hf: e.memset(S[d][:, hf * 4:(hf + 1) * 4, :], 0.0), writes=[S[d].sub(hf)])
                    Gall = gb[:, d * 16:d * 16 + 8]
                    Ball = gb[:, d * 16 + 8:d * 16 + 16]
                    p0 = nbank((d, 0))
                    k.op("pe", lambda e, p0=p0, d=d, Gall=Gall: e.matmul(p0[:, 0:8], lhsT=U[d], rhs=Gall, start=True, stop=True),
                         reads=[self.cst, gb], writes=[p0])
                    k.op("pe", lambda e, p0=p0, Gall=Gall: e.matmul(p0[:, 8:16], lhsT=ones32, rhs=Gall, start=True, stop=True),
                         reads=[self.cst, gb], writes=[p0])
                    k.op("dve", lambda e, p0=p0, gc=gc: e.tensor_copy(out=gc.ap(), in_=p0[:, 0:8]), reads=[p0], writes=[gc])
                    k.op("dve", lambda e, p0=p0, gc=gc, ekd=ekd: e.tensor_tensor(out=ekd.ap(), in0=p0[:, 8:16], in1=gc.ap(), op=ALU.subtract),
                         reads=[p0, gc], writes=[ekd])
                    k.op("act", lambda e, p0=p0, egl=egl: e.activation(out=egl.ap(), in_=p0[:, 8:16], func=AF.Exp), reads=[p0], writes=[egl])
                    k.op("act", lambda e, ekd=ekd: e.activation(out=ekd.ap(), in_=ekd.ap(), func=AF.Exp), reads=[ekd], writes=[ekd])
                    k.op("act", lambda e, gc=gc, bge=bge: e.activation(out=bge.ap(), in_=gc.ap(), func=AF.Exp), reads=[gc], writes=[bge])
                    k.op("dve", lambda e, Ball=Ball, bge=bge: e.tensor_tensor(out=bge.ap(), in0=bge.ap(), in1=Ball, op=ALU.mult),
                         reads=[bge, gb], writes=[bge])
                    gens += [chain(d, ib, gb, ob, t0, 0), chain(d, ib, gb, ob, t0, 1)]
                while gens:
                    for g_ in list(gens):
                        try:
                            next(g_)
                        except StopIteration:
                            gens.remove(g_)

    def gdn_finish(self, l, j):
        k = self.k
        nc = self.nc
        OTv = self.OT.ap().rearrange("(c p) t -> p c t", p=128)
        with k.stage():
            NG = k.tile([128, D], F32, "NG")
            with nc.allow_non_contiguous_dma(reason="tiny broadcast"):
                for h in range(8):
                    k.dma("sp", NG[:, h * 128:(h + 1) * 128], self.gdn_norm_g[j].partition_broadcast(128),
                          reads=[self.gdn_norm_g], writes=[NG], join=(h > 0))
            o0 = [k.tile([128, D], F32, "o0_%d" % i) for i in range(2)]
            o1 = [k.tile([128, D], F32, "o1_%d" % i) for i in range(2)]
            gtb = [k.tile([128, D], BF16, "gtb%d" % i) for i in range(2)]
            sq = k.tile([128, D], F32, "sq")
            ss = [k.tile([128, 8], F32, "ss%d" % i) for i in range(2)]
            yT = [k.tile([128, 8, 512], BF16, "yT%d" % i) for i in range(2)]
            ntile = NTOK // 128
            for ti in range(ntile):
                a, b, g_ = o0[ti % 2], o1[ti % 2], gtb[ti % 2]
                s_ = ss[ti % 2]
                grp, gi = ti // 4, ti % 4
                y_ = yT[grp % 2]
                for hf in range(2):
                    sl = slice(hf * 512, (hf + 1) * 512)
                    k.dma("sp", a[:, sl], self.OD[0, ti * 128:(ti + 1) * 128, sl], reads=[self.OD], writes=[a], join=(hf > 0))
                    k.dma("sp", b[:, sl], self.OD[1, ti * 128:(ti + 1) * 128, sl], reads=[self.OD], writes=[b], join=(hf > 0))
                k.dma("sp", g_.ap(), self.GATE[ti * 128:(ti + 1) * 128, :], reads=[self.GATE], writes=[g_])
                k.op("dve", lambda e, a=a, b=b: e.tensor_tensor(out=a.ap(), in0=a.ap(), in1=b.ap(), op=ALU.add), reads=[a, b], writes=[a])
                k.op("act", lambda e, a=a: e.activation(out=sq.ap(), in_=a.ap(), func=AF.Square), reads=[a], writes=[sq])
                k.op("dve", lambda e, s_=s_: e.tensor_reduce(out=s_.ap(), in_=sq.ap().rearrange("p (h d) -> p h d", h=8),
                                                             op=ALU.add, axis=mybir.AxisListType.X), reads=[sq], writes=[s_])
                k.op("act", lambda e, s_=s_: e.activation(out=s_.ap(), in_=s_.ap(), func=AF.Sqrt, scale=1.0 / 128, bias=self.eps_t.ap()),
                     reads=[s_, self.eps_t], writes=[s_])
                k.op("dve", lambda e, s_=s_: e.reciprocal(out=s_.ap(), in_=s_.ap()), reads=[s_], writes=[s_])
                for h in range(8):
                    k.op("dve", lambda e, h=h, a=a, s_=s_: e.scalar_tensor_tensor(
                        out=a[:, h * 128:(h + 1) * 128], in0=a[:, h * 128:(h + 1) * 128], scalar=s_[:, h:h + 1],
                        in1=NG[:, h * 128:(h + 1) * 128], op0=ALU.mult, op1=ALU.mult), reads=[a, s_, NG], writes=[a])
                k.op("dve", lambda e, a=a, g_=g_: e.tensor_tensor(out=a.ap(), in0=a.ap(), in1=g_.ap(), op=ALU.mult), reads=[a, g_], writes=[a])
                for hf in range(2):
                    bank = self.ps[(ti % 2) * 2 + hf]
                    for c4 in range(4):
                        c = hf * 4 + c4
                        k.op("pe", lambda e, c=c, c4=c4, bank=bank, a=a: e.transpose(bank[:, c4 * 128:(c4 + 1) * 128], a[:, c * 128:(c + 1) * 128],
                                                                                    self.ident), reads=[a, self.cst], writes=[bank])
                    o_ = y_[:, hf * 4:(hf + 1) * 4, gi * 128:(gi + 1) * 128]
                    if hf == 0:
                        k.op("act", lambda e, o_=o_, bank=bank, y_=y_: e.copy(out=o_, in_=bank.ap().rearrange("p (c t) -> p c t", t=128)),
                             reads=[bank], writes=[y_])
                    else:
                        k.op("dve", lambda e, o_=o_, bank=bank, y_=y_: e.tensor_copy(out=o_, in_=bank.ap().rearrange("p (c t) -> p c t", t=128)),
                             reads=[bank], writes=[y_])
                if gi == 3 or ti == ntile - 1:
                    nn = (gi + 1) * 128
                    k.dma("sp", OTv[:, :, grp * 512:grp * 512 + nn], y_[:, :, 0:nn], reads=[y_], writes=[self.OT], join=True,
                          sem=y_.sub("st"))


class _Shift:
    def __init__(self, t, sh):
        self.t = t
        self.d = t.d
        self.sh = sh

    def __getitem__(self, kk):
        a, b, c = kk
        c = slice(c.start + self.sh, c.stop + self.sh)
        return self.t[a, b, c]


class _SubView:
    def __init__(self, t, key):
        self.t = t
        self.d = t.sub(key)

    def __getitem__(self, kk):
        return self.t[kk]


KB._norm_orig = KB._norm


def _norm2(ds):
    out = []
    for d in ds:
        if d is None:
            continue
        if isinstance(d, (Tl, _SubView, _Shift)):
            out.append(d.d)
        else:
            out.append(d)
    return out


KB._norm = staticmethod(_norm2)


def build(layers=(0, 1, 2, 3), stages=None):
    nc = bass.Bass("TRN2", target_bir_lowering=False)
    P = Prog(nc, layers)
    P.stage_init()
    P.stage_in_transpose()
    for l in layers:
        if stages is None or "mix" in stages:
            if l % 2 == 0:
                P.stage_gdn(l)
            else:
                P.stage_na(l)
        if stages is None or "ffn" in stages:
            P.stage_ffn(l, moe=(l % 2 == 1))
    P.stage_out_transpose()
    P.k.barrier()
    return nc, P


def make_consts():
    c = np.zeros((128, 512 + 1024), np.float32)
    for e in range(8):
        c[e, 512 + e * 128:512 + (e + 1) * 128] = 1.0
    c[:, 0:128] = np.eye(128, dtype=np.float32)
    c[:, 128:256] = np.tril(np.ones((128, 128), np.float32))
    c[:, 256:384] = np.triu(np.ones((128, 128), np.float32))
    c[:, 384:512] = 1.0
    return c


def make_na_bias(rpb):
    rpb = np.asarray(rpb, np.float32)
    nl = rpb.shape[0]
    out = np.full((nl, 9, 128, 16, 320), -30000.0, np.float32)
    p = np.arange(128)
    q = np.arange(64)
    kc = p % 64
    wstart = np.clip(q - 8, 0, 48)
    valid_c = (kc[:, None] >= wstart[None, :]) & (kc[:, None] < wstart[None, :] + 16)
    dc_idx = np.clip(kc[:, None] - q[None, :] + 15, 0, 30)
    for rt in range(9):
        if rt < 4:
            r, rs_ = rt, 0
        elif rt == 4:
            r, rs_ = 8, 4
        elif rt == 5:
            r, rs_ = 9, 5
        else:
            r, rs_ = 56 + rt - 1, 56
        base = rs_ - rs_ % 2
        nt = 4 if rs_ % 2 == 0 else 5
        for slot in range(nt):
            grow = base + 2 * slot + p // 64
            jw = grow - rs_
            valid = valid_c & ((jw >= 0) & (jw < 8))[:, None]
            dr_idx = np.clip(grow - r + 7, 0, 14)
            vals = rpb[:, :, dr_idx[:, None], dc_idx]
            vals = np.transpose(vals, (0, 2, 1, 3))
            blk = out[:, rt, :, :, slot * 64:(slot + 1) * 64]
            out[:, rt, :, :, slot * 64:(slot + 1) * 64] = np.where(valid[None, :, None, :], vals, blk)
    return out.reshape(nl, 9, 128, 16 * 320)


def make_in_maps(P, inp, shared):
    in_maps = []
    for b in range(NCORES):
        m = {n: v for n, v in shared.items() if n in P.exts}
        m["x"] = np.ascontiguousarray(inp["x"][b])
        m["ctx"] = np.ascontiguousarray(inp["ctx"][b])
        m["cvec"] = np.ascontiguousarray(np.stack([inp["c"][b], inp["c_ctx"]], 0))
        in_maps.append(m)
    return in_maps


def make_shared(inp):
    shared = {n: np.ascontiguousarray(inp[n], dtype=np.float32) for n in (
        "ada_w", "ada_b", "norm1_g", "norm2_g", "gdn_w_in", "gdn_conv_w", "gdn_a_log", "gdn_dt_bias",
        "gdn_norm_g", "gdn_w_out", "na_w_in", "na_q_norm", "na_k_norm", "na_w_out", "ffn_w13", "ffn_w2",
        "moe_router", "moe_w13", "moe_w2")}
    shared["consts"] = make_consts()
    shared["na_bias"] = make_na_bias(inp["na_rpb"])
    return shared


def kernel(**inp):
    nc, P = build()
    shared = make_shared(inp)
    in_maps = make_in_maps(P, inp, shared)
    res = run_bass_kernel_spmd(nc, in_maps, core_ids=list(range(NCORES)))
    return np.stack([r["y"] for r in res.results], 0)
```

```python
import numpy as np
from contextlib import ExitStack
import concourse.bass as bass
import concourse.mybir as mybir
from concourse.bass_utils import run_bass_kernel_spmd

F32 = mybir.dt.float32
BF16 = mybir.dt.bfloat16
AF = mybir.ActivationFunctionType
ALU = mybir.AluOpType

D = 1024
T = 4096
NCTX = 256
NTOK = T + NCTX
DEPTH = 4
NCORES = 8
EPS = 1e-6
FF_DENSE = 2816
FF_EXPERT = 3584
NEXP = 8


class Dep:
    __slots__ = ("w", "r", "dsem", "excl")

    def __init__(self):
        self.w = {}
        self.r = {}
        self.dsem = None
        self.excl = False


class Tl:
    def __init__(self, t, is_dram=False):
        self.t = t
        self.d = Dep()
        self.subs = {}
        self.is_dram = is_dram

    def __getitem__(self, k):
        if self.is_dram:
            return self.t.ap()[k]
        return self.t[k]

    def ap(self):
        return self.t.ap() if self.is_dram else self.t[:]

    def sub(self, key):
        if key not in self.subs:
            self.subs[key] = Dep()
        return self.subs[key]


class KB:
    def __init__(self, nc):
        self.nc = nc
        self.E = {"pe": nc.tensor, "act": nc.scalar, "dve": nc.vector, "pool": nc.gpsimd, "sp": nc.sync}
        self.es = ExitStack()
        self.sems = []
        self.csem = {}
        for e in self.E:
            self.csem[e] = self._newsem("c_" + e)
        self.cnt = {e: 0 for e in self.E}
        self.known = {e: {} for e in self.E}
        self.dfree = [self._newsem("d%d" % i) for i in range(90)]
        self.dval = {}
        self.dused = []
        self.stage_deps = []
        self.stage_es = None
        self.uid = 0
        self.ninstr = 0

    def _newsem(self, name):
        s = self.es.enter_context(self.nc.semaphore(name))
        self.sems.append(s)
        return len(self.sems) - 1

    def tile(self, shape, dtype, name=None, persistent=False):
        self.uid += 1
        name = (name or "t") + "_%d" % self.uid
        es = self.es if persistent else self.stage_es
        t = es.enter_context(self.nc.sbuf_tensor(name, list(shape), dtype))
        return Tl(t)

    def dram(self, name, shape, dtype, kind="Internal"):
        t = self.nc.dram_tensor(name, list(shape), dtype, kind=kind)
        return Tl(t, is_dram=True)

    def _wait(self, e, tok):
        if tok is None:
            return
        idx, val = tok
        if e == "pe" and idx == self.csem["pe"]:
            return
        if self.known[e].get(idx, 0) >= val:
            return
        self.E[e].wait_ge(self.sems[idx], val)
        self.known[e][idx] = val

    def _deps_wait(self, e, reads, writes, join=False):
        for d in reads:
            for tok in d.w.items():
                self._wait(e, tok)
            if d.excl:
                for tok in d.r.items():
                    if tok[0] != self.csem.get(e):
                        self._wait(e, tok)
        for d in writes:
            if not join:
                for tok in d.w.items():
                    self._wait(e, tok)
            for tok in d.r.items():
                self._wait(e, tok)

    @staticmethod
    def _norm(ds):
        out = []
        for d in ds:
            if d is None:
                continue
            out.append(d.d if isinstance(d, Tl) else d)
        return out

    def op(self, e, fn, reads=(), writes=()):
        reads = self._norm(reads)
        writes = self._norm(writes)
        self._deps_wait(e, reads, writes)
        ins = fn(self.E[e])
        self.cnt[e] += 1
        self.ninstr += 1
        ins.then_inc(self.sems[self.csem[e]], 1)
        tok = (self.csem[e], self.cnt[e])
        for d in reads:
            if d.r.get(tok[0], 0) < tok[1]:
                d.r[tok[0]] = tok[1]
        for d in writes:
            d.w = {tok[0]: tok[1]}
            d.r = {}
        return ins

    def dma(self, q, out, in_, reads=(), writes=(), join=False, sem=None, **kw):
        reads = self._norm(reads)
        writes = self._norm(writes)
        assert len(writes) == 1
        wd = writes[0]
        self._deps_wait(q, reads, writes, join=join)
        sd = wd if sem is None else self._norm([sem])[0]
        if sd.dsem is None:
            sd.dsem = self.dfree.pop()
            self.dused.append(sd)
        idx = sd.dsem
        self.dval[idx] = self.dval.get(idx, 0) + 16
        ins = self.E[q].dma_start(out=out, in_=in_, **kw)
        ins.then_inc(self.sems[idx], 16)
        self.ninstr += 1
        tok = (idx, self.dval[idx])
        for d in reads:
            if d.r.get(idx, 0) < tok[1]:
                d.r[idx] = tok[1]
        if join:
            wd.w[idx] = tok[1]
        else:
            wd.w = {idx: tok[1]}
            wd.r = {}
        return ins

    def barrier(self):
        toks = [(self.csem[e], self.cnt[e]) for e in self.E if self.cnt[e] > 0]
        toks += [(idx, v) for idx, v in self.dval.items()]
        for e in self.E:
            for tok in toks:
                self._wait(e, tok)
        for d in self.dused:
            self.dfree.append(d.dsem)
            d.dsem = None
        self.dused = []

    class _Stage:
        def __init__(self, kb):
            self.kb = kb

        def __enter__(self):
            self.prev = self.kb.stage_es
            self.kb.stage_es = ExitStack()
            self.kb.stage_es.__enter__()
            return self.kb

        def __exit__(self, *a):
            self.kb.barrier()
            self.kb.stage_es.__exit__(*a)
            self.kb.stage_es = self.prev
            return False

    def stage(self):
        return KB._Stage(self)


def token_blocks(n0, n1, blk=512):
    out = []
    t = n0
    while t < n1:
        b = min(blk, n1 - t)
        out.append((t, b))
        t += b
    return out


class Prog:
    def __init__(self, nc, layers=(0, 1, 2, 3), debug=False):
        self.nc = nc
        self.k = KB(nc)
        k = self.k
        self.layers = layers
        self.ext_shapes = {
            "x": [T, D], "ctx": [NCTX, D], "cvec": [2, D], "ada_w": [DEPTH, D, 6 * D], "ada_b": [DEPTH, 6 * D],
            "norm1_g": [DEPTH, D], "norm2_g": [DEPTH, D], "gdn_w_in": [2, D, 4 * D + 32],
            "gdn_conv_w": [2, 3, 3 * D], "gdn_a_log": [2, 2, 8], "gdn_dt_bias": [2, 2, 8],
            "gdn_norm_g": [2, 128], "gdn_w_out": [2, D, D], "na_w_in": [2, D, 3 * D], "na_q_norm": [2, 64],
            "na_k_norm": [2, 64], "na_bias": [2, 9, 128, 16 * 320], "na_w_out": [2, D, D],
            "ffn_w13": [2, D, 2 * FF_DENSE], "ffn_w2": [2, FF_DENSE, D], "moe_router": [2, D, NEXP],
            "moe_w13": [2, NEXP, D, 2 * FF_EXPERT], "moe_w2": [2, NEXP, FF_EXPERT, D],
            "consts": [128, 4 * 128 + 1024]}
        self.exts = {}
        self.y = k.dram("y", [T, D], F32, kind="ExternalOutput")
        self.XT = k.dram("XT", [D, NTOK], F32)
        self.OT = k.dram("OT", [D, NTOK], BF16)
        self.cst = k.tile([128, 4 * 128], F32, "cst", persistent=True)
        self.ones_bf = k.tile([128, 128], BF16, "ones_bf", persistent=True)
        self.ident = self.cst[:, 0:128]
        self.MODS = k.tile([128, DEPTH * 6 * 8 * 2], F32, "mods", persistent=True)
        self.GG = k.tile([128, DEPTH * 2 * 8 * 2], F32, "gg", persistent=True)
        self.eps_t = k.tile([128, 1], F32, "eps", persistent=True)
        self.ps = []
        for b in range(8):
            t = k.es.enter_context(nc.psum_tensor("ps%d" % b, [128, 512], F32))
            pt_ = Tl(t)
            pt_.d.excl = True
            self.ps.append(pt_)

    def __getattr__(self, name):
        shapes = self.__dict__.get("ext_shapes", {})
        if name in shapes:
            if name not in self.exts:
                self.exts[name] = self.k.dram(name, shapes[name], F32, kind="ExternalInput")
            return self.exts[name]
        raise AttributeError(name)

    STG = 2048

    def alloc_stg(self):
        self.stg = [self.k.tile([128, self.STG], F32, "stg%d" % i) for i in range(2)]
        self.stg_i = 0

    def cast_load(self, dst, src, pat, wdep, first=True, **dims):
        k = self.k
        st = self.stg[self.stg_i % 2]
        self.stg_i += 1
        n = 1
        for d_ in dst.shape[1:]:
            n *= d_
        assert n <= self.STG
        view = st[:, 0:n]
        if pat is not None:
            view = view.rearrange(pat, **dims)
        k.dma("sp", view, src, reads=[self.consts], writes=[st])
        deps_w = [wdep]
        k.op("pool", lambda e: e.tensor_copy(out=dst, in_=view), reads=[st], writes=deps_w) if first else \
            self._join_op("pool", lambda e: e.tensor_copy(out=dst, in_=view), [st], wdep)

    def _join_op(self, eng, fn, reads, wdep):
        k = self.k
        wd = k._norm([wdep])[0]
        reads_n = k._norm(reads)
        k._deps_wait(eng, reads_n, [wd], join=True)
        ins = fn(k.E[eng])
        k.cnt[eng] += 1
        k.ninstr += 1
        ins.then_inc(k.sems[k.csem[eng]], 1)
        tok = (k.csem[eng], k.cnt[eng])
        for d in reads_n:
            if d.r.get(tok[0], 0) < tok[1]:
                d.r[tok[0]] = tok[1]
        wd.w[tok[0]] = tok[1]

    def mod(self, l, m, c, t):
        o = ((l * 6 + m) * 8 + c) * 2 + t
        return self.MODS[:, o:o + 1]

    def gg(self, l, n, c, t):
        o = ((l * 2 + n) * 8 + c) * 2 + t
        return self.GG[:, o:o + 1]

    def stage_init(self):
        k = self.k
        nc = self.nc
        with k.stage():
            k.dma("sp", self.cst.ap(), self.consts[:, 0:512], reads=[self.consts], writes=[self.cst])
            k.op("dve", lambda e: e.tensor_copy(out=self.ones_bf.ap(), in_=self.cst[:, 384:512]),
                 reads=[self.cst], writes=[self.ones_bf])
            k.op("dve", lambda e: e.memset(self.eps_t.ap(), EPS), writes=[self.eps_t])
            craw = k.tile([128, 2, 8], F32, "craw")
            sc = k.tile([128, 8, 2], F32, "sc")
            with nc.allow_non_contiguous_dma(reason="tiny"):
                k.dma("sp", craw.ap(), self.cvec.ap().rearrange("t (c p) -> p t c", p=128),
                      reads=[self.cvec], writes=[craw])
            k.op("act", lambda e: e.activation(out=sc.ap().rearrange("p c t -> p t c"), in_=craw.ap(), func=AF.Silu),
                 reads=[craw], writes=[sc])
            ab = k.tile([128, DEPTH, 48], F32, "adab")
            g1 = k.tile([128, 2, DEPTH, 8], F32, "ng")
            with nc.allow_non_contiguous_dma(reason="tiny"):
                k.dma("sp", ab.ap(), self.ada_b.ap().rearrange("l (o p) -> p l o", p=128),
                      reads=[self.ada_b], writes=[ab])
                k.dma("sp", g1[:, 0, :, :], self.norm1_g.ap().rearrange("l (c p) -> p l c", p=128),
                      reads=[self.norm1_g], writes=[g1.sub(0)])
                k.dma("sp", g1[:, 1, :, :], self.norm2_g.ap().rearrange("l (c p) -> p l c", p=128),
                      reads=[self.norm2_g], writes=[g1.sub(1)])
            NW = 1536
            wb = [k.tile([128, 8, NW], F32, "adaw%d" % i) for i in range(2)]
            it = 0
            for l in range(DEPTH):
                pst = self.ps[l % 2]
                for q in range(6 * D // NW):
                    w = wb[it % 2]
                    it += 1
                    k.dma("sp", w.ap(),
                          self.ada_w[l].rearrange("(kk p) n -> p kk n", p=128)[:, :, q * NW:(q + 1) * NW],
                          reads=[self.ada_w], writes=[w])
                    for oc in range(NW // 128):
                        occ = q * (NW // 128) + oc
                        for kk in range(8):
                            k.op("pe", lambda e, kk=kk, oc=oc, occ=occ, w=w, pst=pst: e.matmul(
                                pst[:, occ * 2:occ * 2 + 2], lhsT=w[:, kk, oc * 128:(oc + 1) * 128],
                                rhs=sc[:, kk, :], start=(kk == 0), stop=(kk == 7)),
                                reads=[w, sc], writes=[pst])
                mv = self.MODS[:, l * 96:(l + 1) * 96].rearrange("p (o t) -> p o t", t=2)
                for t in range(2):
                    k.op("dve", lambda e, t=t, mv=mv, pst=pst, l=l: e.tensor_tensor(
                        out=mv[:, :, t], in0=pst[:, 0:96].rearrange("p (o t) -> p o t", t=2)[:, :, t],
                        in1=ab[:, l, :], op=ALU.add), reads=[pst, ab], writes=[self.MODS])
                for n in range(2):
                    m = 1 if n == 0 else 4
                    for t in range(2):
                        o = (l * 6 + m) * 16
                        src = self.MODS[:, o:o + 16].rearrange("p (c t) -> p c t", t=2)[:, :, t]
                        og = (l * 2 + n) * 16
                        dst = self.GG[:, og:og + 16].rearrange("p (c t) -> p c t", t=2)[:, :, t]
                        k.op("dve", lambda e, src=src, dst=dst, l=l, n=n: e.scalar_tensor_tensor(
                            out=dst, in0=src, scalar=1.0, in1=g1[:, n, l, :], op0=ALU.add, op1=ALU.mult),
                            reads=[self.MODS, g1.sub(0), g1.sub(1)], writes=[self.GG])

    def stage_in_transpose(self):
        k = self.k
        XTv = self.XT.ap().rearrange("(c p) t -> p c t", p=128)
        with k.stage():
            xin = [k.tile([128, D], F32, "xin%d" % i) for i in range(2)]
            xo = [k.tile([128, 8, 128], F32, "xo%d" % i) for i in range(2)]
            for ti in range(NTOK // 128):
                src = self.x[ti * 128:(ti + 1) * 128, :] if ti < T // 128 else \
                    self.ctx[(ti - T // 128) * 128:(ti - T // 128 + 1) * 128, :]
                xi = xin[ti % 2]
                o = xo[ti % 2]
                k.dma("sp", xi.ap(), src, reads=[self.x], writes=[xi])
                for half in range(2):
                    pst = self.ps[(ti % 2) * 2 + half]
                    for c4 in range(4):
                        c = half * 4 + c4
                        k.op("pe", lambda e, c=c, c4=c4, pst=pst, xi=xi: e.transpose(
                            pst[:, c4 * 128:(c4 + 1) * 128], xi[:, c * 128:(c + 1) * 128], self.ident),
                            reads=[xi, self.cst], writes=[pst])
                    eng = "act" if half == 0 else "dve"
                    if eng == "act":
                        k.op("act", lambda e, pst=pst, o=o, half=half: e.copy(
                            out=o[:, half * 4:(half + 1) * 4, :], in_=pst.ap().rearrange("p (c t) -> p c t", t=128)),
                            reads=[pst], writes=[o.sub(half)])
                    else:
                        k.op("dve", lambda e, pst=pst, o=o, half=half: e.tensor_copy(
                            out=o[:, half * 4:(half + 1) * 4, :], in_=pst.ap().rearrange("p (c t) -> p c t", t=128)),
                            reads=[pst], writes=[o.sub(half)])
                k.dma("sp", XTv[:, :, ti * 128:(ti + 1) * 128], o.ap(),
                      reads=[o.sub(0), o.sub(1)], writes=[self.XT], join=True, sem=o.sub("st"))

    def stage_out_transpose(self, debug_ctx=False):
        k = self.k
        XTv = self.XT.ap().rearrange("(c p) t -> p c t", p=128)
        if debug_ctx:
            self.yc = k.dram("yc", [NCTX, D], F32, kind="ExternalOutput")
        with k.stage():
            xin = [k.tile([128, 8, 128], F32, "oin%d" % i) for i in range(2)]
            xo = [k.tile([128, D], F32, "oo%d" % i) for i in range(2)]
            for ti in range((NTOK if debug_ctx else T) // 128):
                xi = xin[ti % 2]
                o = xo[ti % 2]
                k.dma("sp", xi.ap(), XTv[:, :, ti * 128:(ti + 1) * 128], reads=[self.XT], writes=[xi])
                for half in range(2):
                    pst = self.ps[(ti % 2) * 2 + half]
                    for c4 in range(4):
                        c = half * 4 + c4
                        k.op("pe", lambda e, c=c, c4=c4, pst=pst, xi=xi: e.transpose(
                            pst[:, c4 * 128:(c4 + 1) * 128], xi[:, c, :], self.ident),
                            reads=[xi, self.cst], writes=[pst])
                    if half == 0:
                        k.op("act", lambda e, pst=pst, o=o, half=half: e.copy(
                            out=o[:, half * 512:(half + 1) * 512], in_=pst.ap()),
                            reads=[pst], writes=[o.sub(half)])
                    else:
                        k.op("dve", lambda e, pst=pst, o=o, half=half: e.tensor_copy(
                            out=o[:, half * 512:(half + 1) * 512], in_=pst.ap()),
                            reads=[pst], writes=[o.sub(half)])
                dsto = self.y[ti * 128:(ti + 1) * 128, :] if ti < T // 128 else \
                    self.yc[(ti - T // 128) * 128:(ti - T // 128 + 1) * 128, :]
                k.dma("sp", dsto, o.ap(),
                      reads=[o.sub(0), o.sub(1)], writes=[self.y], join=True, sem=o.sub("st"))

    def norm_block(self, X, h, off, n, l, nidx, tsel, sq, rs, ps_ss, h32=None):
        k = self.k
        m_shift = 0 if nidx == 0 else 3
        k.op("act", lambda e: e.activation(out=sq[:, :, 0:n], in_=X[:, :, off:off + n], func=AF.Square),
             reads=[X], writes=[sq])
        for c in range(8):
            k.op("pe", lambda e, c=c: e.matmul(ps_ss[:, 0:n], lhsT=self.ones_bf.ap(), rhs=sq[:, c, 0:n],
                                               start=(c == 0), stop=(c == 7)),
                 reads=[sq, self.ones_bf], writes=[ps_ss])
        k.op("act", lambda e: e.activation(out=rs[:, 0:n], in_=ps_ss[:, 0:n], func=AF.Sqrt,
                                           scale=1.0 / D, bias=self.eps_t.ap()),
             reads=[ps_ss, self.eps_t], writes=[rs])
        k.op("dve", lambda e: e.reciprocal(out=rs[:, 0:n], in_=rs[:, 0:n]), reads=[rs], writes=[rs])
        for c in range(8):
            if h32 is not None:
                k.op("dve", lambda e, c=c: e.scalar_tensor_tensor(
                    out=h32[:, c, 0:n], in0=X[:, c, off:off + n], scalar=self.gg(l, nidx, c, tsel),
                    in1=rs[:, 0:n], op0=ALU.mult, op1=ALU.mult), reads=[X, rs, self.GG], writes=[h32])
                k.op("act", lambda e, c=c: e.activation(
                    out=h32[:, c, 0:n], in_=h32[:, c, 0:n], func=AF.Identity,
                    bias=self.mod(l, m_shift, c, tsel), scale=1.0), reads=[h32, self.MODS], writes=[h32])
                k.op("pool", lambda e, c=c: e.tensor_copy(out=h[:, c, off:off + n], in_=h32[:, c, 0:n]),
                     reads=[h32], writes=[h])
            else:
                k.op("dve", lambda e, c=c: e.scalar_tensor_tensor(
                    out=h[:, c, off:off + n], in0=X[:, c, off:off + n], scalar=self.gg(l, nidx, c, tsel),
                    in1=rs[:, 0:n], op0=ALU.mult, op1=ALU.mult), reads=[X, rs, self.GG], writes=[h])
                k.op("act", lambda e, c=c: e.activation(
                    out=h[:, c, off:off + n], in_=h[:, c, off:off + n], func=AF.Identity,
                    bias=self.mod(l, m_shift, c, tsel), scale=1.0), reads=[h, self.MODS], writes=[h])

    def stage_ffn(self, l, moe):
        k = self.k
        nc = self.nc
        j = l // 2
        last = (l == DEPTH - 1)
        ntok = T if last else NTOK
        FF = FF_EXPERT if moe else FF_DENSE
        nfc = FF // 128
        FG = 4
        fgroups = [(f0, min(FG, nfc - f0)) for f0 in range(0, nfc, FG)]
        nexp = NEXP if moe else 1
        halves = [(0, ntok // 2), (ntok // 2, ntok)]
        XTv = self.XT.ap().rearrange("(c p) t -> p c t", p=128)
        for (h0, h1) in halves:
            nh = h1 - h0
            with k.stage():
                X = k.tile([128, 8, nh], F32, "X")
                H = k.tile([128, 8, nh], BF16, "H")
                GW = k.tile([8, nh], F32, "GW") if moe else None
                sel = k.tile([8, NEXP * 128], F32, "sel") if moe else None
                ph1 = k.stage()
                ph1.__enter__()
                sq = k.tile([128, 8, 512], BF16, "sq")
                rs = k.tile([128, 512], F32, "rs")
                blocks = []
                for (t0, n) in token_blocks(h0, h1):
                    if t0 < T < t0 + n:
                        blocks.append((t0, T - t0))
                        blocks.append((T, t0 + n - T))
                    else:
                        blocks.append((t0, n))
                for bi, (t0, n) in enumerate(blocks):
                    k.dma("sp", X[:, :, t0 - h0:t0 - h0 + n], XTv[:, :, t0:t0 + n],
                          reads=[self.XT], writes=[X.sub(bi)])
                if moe:
                    h32 = k.tile([128, 8, 512], F32, "h32")
                    wr = k.tile([128, 8, NEXP], F32, "wr")
                    with nc.allow_non_contiguous_dma(reason="small router weight"):
                        k.dma("sp", wr.ap(), self.moe_router[j].rearrange("(kk p) e -> p kk e", p=128),
                              reads=[self.moe_router], writes=[wr])
                    k.dma("sp", sel.ap(), self.consts[0:8, 512:512 + NEXP * 128], reads=[self.consts], writes=[sel])
                    lg = k.tile([128, 8], F32, "lg")
                    r1 = k.tile([128, 8], F32, "r1")
                    r2 = k.tile([128, 8], F32, "r2")
                    m1 = k.tile([128, 1], F32, "m1")
                    m2 = k.tile([128, 1], F32, "m2")
                    gwt = k.tile([128, 8], F32, "gwt")
                for bi, (t0, n) in enumerate(blocks):
                    tsel = 0 if t0 < T else 1
                    off = t0 - h0
                    self.norm_block(_SubView(X, bi), H, off, n, l, 1, tsel, sq, rs, self.ps[0],
                                    h32=(h32 if moe else None))
                    if moe:
                        for s in range(n // 128):
                            pl = self.ps[1]
                            for kk in range(8):
                                k.op("pe", lambda e, kk=kk, s=s, pl=pl: e.matmul(
                                    pl[:, 0:8], lhsT=h32[:, kk, s * 128:(s + 1) * 128], rhs=wr[:, kk, :],
                                    start=(kk == 0), stop=(kk == 7)), reads=[h32, wr], writes=[pl])
                            k.op("dve", lambda e, pl=pl: e.tensor_copy(out=lg.ap(), in_=pl[:, 0:8]),
                                 reads=[pl], writes=[lg])
                            k.op("dve", lambda e: e.reduce_max(out=m1.ap(), in_=lg.ap(), axis=mybir.AxisListType.X),
                                 reads=[lg], writes=[m1])
                            k.op("dve", lambda e: e.tensor_scalar(out=r1.ap(), in0=lg.ap(), scalar1=m1.ap(),
                                                                  scalar2=None, op0=ALU.is_ge),
                                 reads=[lg, m1], writes=[r1])
                            k.op("dve", lambda e: e.scalar_tensor_tensor(out=r2.ap(), in0=r1.ap(), scalar=-1e30,
                                                                         in1=lg.ap(), op0=ALU.mult, op1=ALU.add),
                                 reads=[r1, lg], writes=[r2])
                            k.op("dve", lambda e: e.reduce_max(out=m2.ap(), in_=r2.ap(), axis=mybir.AxisListType.X),
                                 reads=[r2], writes=[m2])
                            k.op("dve", lambda e: e.tensor_scalar(out=r1.ap(), in0=lg.ap(), scalar1=m2.ap(),
                                                                  scalar2=None, op0=ALU.is_ge),
                                 reads=[lg, m2], writes=[r1])
                            k.op("dve", lambda e: e.tensor_scalar(out=r2.ap(), in0=lg.ap(), scalar1=m1.ap(),
                                                                  scalar2=None, op0=ALU.subtract),
                                 reads=[lg, m1], writes=[r2])
                            k.op("act", lambda e: e.activation(out=r2.ap(), in_=r2.ap(), func=AF.Exp),
                                 reads=[r2], writes=[r2])
                            k.op("dve", lambda e: e.tensor_tensor(out=gwt.ap(), in0=r1.ap(), in1=r2.ap(), op=ALU.mult),
                                 reads=[r1, r2], writes=[gwt])
                            k.op("dve", lambda e: e.reduce_sum(out=m2.ap(), in_=gwt.ap(), axis=mybir.AxisListType.X),
                                 reads=[gwt], writes=[m2])
                            k.op("dve", lambda e: e.reciprocal(out=m2.ap(), in_=m2.ap()), reads=[m2], writes=[m2])
                            k.op("dve", lambda e: e.tensor_scalar(out=gwt.ap(), in0=gwt.ap(), scalar1=m2.ap(),
                                                                  scalar2=None, op0=ALU.mult),
                                 reads=[gwt, m2], writes=[gwt])
                            pt = self.ps[2]
                            k.op("pe", lambda e, pt=pt: e.transpose(pt[0:8, 0:128], gwt.ap(), self.ident),
                                 reads=[gwt, self.cst], writes=[pt])
                            k.op("act", lambda e, pt=pt, s=s, off=off: e.copy(
                                out=GW[:, off + s * 128:off + (s + 1) * 128], in_=pt[0:8, 0:128]),
                                reads=[pt], writes=[GW])
                ph1.__exit__(None, None, None)
                ph2 = k.stage()
                ph2.__enter__()
                self.alloc_stg()
                w13b = [k.tile([128, 8, 2, FG * 128], BF16, "w13b%d" % i) for i in range(2)]
                w2b = [k.tile([128, FG, D], BF16, "w2b%d" % i) for i in range(2)]
                actb = [k.tile([128, FG, 512], BF16, "actb%d" % i) for i in range(2)]
                sg = [k.tile([128, 512], F32, "sg%d" % i) for i in range(2)]
                it = 0
                ai = 0
                pi = 0
                pend = None

                def emit_w2(bi, t0, n, off, act, w2, nf):
                    tsel = 0 if t0 < T else 1
                    for dc in range(8):
                        po = self.ps[4 + dc % 2]
                        for f in range(nf):
                            k.op("pe", lambda e, f=f, dc=dc, po=po: e.matmul(
                                po[:, 0:n], lhsT=w2[:, f, dc * 128:(dc + 1) * 128], rhs=act[:, f, 0:n],
                                start=(f == 0), stop=(f == nf - 1)), reads=[w2, act], writes=[po])
                        k.op("dve", lambda e, dc=dc, po=po: e.scalar_tensor_tensor(
                            out=X[:, dc, off:off + n], in0=po[:, 0:n], scalar=self.mod(l, 5, dc, tsel),
                            in1=X[:, dc, off:off + n], op0=ALU.mult, op1=ALU.add),
                            reads=[po, self.MODS, X.sub(bi)], writes=[X.sub(bi)])

                for ex in range(nexp):
                    if moe:
                        w13src = self.moe_w13[j, ex]
                        w2src = self.moe_w2[j, ex]
                    else:
                        w13src = self.ffn_w13[j]
                        w2src = self.ffn_w2[j]
                    for (f0, nf) in fgroups:
                        w13 = w13b[it % 2]
                        w2 = w2b[it % 2]
                        it += 1
                        w13v = w13src.rearrange("(kk p) n -> p kk n", p=128)
                        for gu in range(2):
                            for kk in range(0, 8, 4):
                                self.cast_load(w13[:, kk:kk + 4, gu, 0:nf * 128],
                                               w13v[:, kk:kk + 4, gu * FF + f0 * 128: gu * FF + (f0 + nf) * 128],
                                               "p (a b) -> p a b", w13, first=(gu == 0 and kk == 0), a=4)
                        w2v = w2src[f0 * 128:(f0 + nf) * 128, :].rearrange("(f p) n -> p f n", p=128)
                        for f2 in range(0, nf, 2):
                            self.cast_load(w2[:, f2:f2 + 2, :], w2v[:, f2:f2 + 2, :], "p (a b) -> p a b", w2,
                                           first=(f2 == 0), a=2)
                        for bi, (t0, n) in enumerate(blocks):
                            off = t0 - h0
                            act = actb[ai % 2]
                            ai += 1
                            pgw = None
                            if moe:
                                pgw = self.ps[6 + ai % 2]
                                k.op("pe", lambda e, ex=ex, pgw=pgw, n=n, off=off: e.matmul(
                                    pgw[:, 0:n], lhsT=sel[:, ex * 128:(ex + 1) * 128], rhs=GW[:, off:off + n],
                                    start=True, stop=True), reads=[sel, GW], writes=[pgw])
                            for f in range(nf):
                                pg = self.ps[(pi % 2) * 2]
                                pu = self.ps[(pi % 2) * 2 + 1]
                                s_ = sg[pi % 2]
                                pi += 1
                                for kk in range(8):
                                    k.op("pe", lambda e, kk=kk, f=f, pg=pg, w13=w13, n=n, off=off: e.matmul(
                                        pg[:, 0:n], lhsT=w13[:, kk, 0, f * 128:(f + 1) * 128],
                                        rhs=H[:, kk, off:off + n], start=(kk == 0), stop=(kk == 7)),
                                        reads=[w13, H], writes=[pg])
                                for kk in range(8):
                                    k.op("pe", lambda e, kk=kk, f=f, pu=pu, w13=w13, n=n, off=off: e.matmul(
                                        pu[:, 0:n], lhsT=w13[:, kk, 1, f * 128:(f + 1) * 128],
                                        rhs=H[:, kk, off:off + n], start=(kk == 0), stop=(kk == 7)),
                                        reads=[w13, H], writes=[pu])
                                k.op("act", lambda e, pg=pg, s_=s_, n=n: e.activation(out=s_[:, 0:n], in_=pg[:, 0:n],
                                                                                     func=AF.Silu),
                                     reads=[pg], writes=[s_])
                                if moe:
                                    k.op("dve", lambda e, pu=pu, s_=s_, n=n: e.tensor_tensor(
                                        out=s_[:, 0:n], in0=s_[:, 0:n], in1=pu[:, 0:n], op=ALU.mult),
                                        reads=[s_, pu], writes=[s_])
                                    k.op("dve", lambda e, f=f, act=act, s_=s_, pgw=pgw, n=n: e.tensor_tensor(
                                        out=act[:, f, 0:n], in0=s_[:, 0:n], in1=pgw[:, 0:n], op=ALU.mult),
                                        reads=[s_, pgw], writes=[act])
                                else:
                                    k.op("dve", lambda e, f=f, act=act, pu=pu, s_=s_, n=n: e.tensor_tensor(
                                        out=act[:, f, 0:n], in0=s_[:, 0:n], in1=pu[:, 0:n], op=ALU.mult),
                                        reads=[s_, pu], writes=[act])
                            if pend is not None:
                                emit_w2(*pend)
                            pend = (bi, t0, n, off, act, w2, nf)
                if pend is not None:
                    emit_w2(*pend)
                ph2.__exit__(None, None, None)
                for bi, (t0, n) in enumerate(blocks):
                    k.dma("sp", XTv[:, :, t0:t0 + n], X[:, :, t0 - h0:t0 - h0 + n],
                          reads=[X.sub(bi)], writes=[self.XT], join=True, sem=X.sub(bi))


    def load_norm_all(self, l, nidx, ntok, H):
        k = self.k
        XTv = self.XT.ap().rearrange("(c p) t -> p c t", p=128)
        xb = [k.tile([128, 8, 512], F32, "xb%d" % i) for i in range(2)]
        sq = k.tile([128, 8, 512], BF16, "sq")
        rs = k.tile([128, 512], F32, "rs")
        for bi, (t0, n) in enumerate(token_blocks(0, ntok)):
            X = xb[bi % 2]
            k.dma("sp", X[:, :, 0:n], XTv[:, :, t0:t0 + n], reads=[self.XT], writes=[X])
            tsel = 0 if t0 < T else 1
            self.norm_block(_Shift(X, -t0), H, t0, n, l, nidx, tsel, sq, rs, self.ps[7])

    def stage_outproj(self, l, wsrc, OT, ntok):
        k = self.k
        XTv = self.XT.ap().rearrange("(c p) t -> p c t", p=128)
        OTv = OT.ap().rearrange("(c p) t -> p c t", p=128)
        with k.stage():
            W = k.tile([128, 8, D], BF16, "wo")
            self.alloc_stg()
            for kk in range(0, 8, 2):
                self.cast_load(W[:, kk:kk + 2, :], wsrc.rearrange("(kk p) n -> p kk n", p=128)[:, kk:kk + 2, :],
                               "p (a b) -> p a b", W, first=(kk == 0), a=2)
            xb = [k.tile([128, 8, 512], F32, "xb%d" % i) for i in range(2)]
            ob = [k.tile([128, 8, 512], BF16, "ob%d" % i) for i in range(2)]
            for bi, (t0, n) in enumerate(token_blocks(0, ntok)):
                X = xb[bi % 2]
                O = ob[bi % 2]
                tsel = 0 if t0 < T else 1
                k.dma("sp", X[:, :, 0:n], XTv[:, :, t0:t0 + n], reads=[self.XT], writes=[X])
                k.dma("sp", O[:, :, 0:n], OTv[:, :, t0:t0 + n], reads=[OT], writes=[O])
                for dc in range(8):
                    po = self.ps[dc % 4]
                    for kk in range(8):
                        k.op("pe", lambda e, kk=kk, dc=dc, po=po, O=O: e.matmul(
                            po[:, 0:n], lhsT=W[:, kk, dc * 128:(dc + 1) * 128], rhs=O[:, kk, 0:n],
                            start=(kk == 0), stop=(kk == 7)), reads=[W, O], writes=[po])
                    k.op("dve", lambda e, dc=dc, po=po, X=X, tsel=tsel: e.scalar_tensor_tensor(
                        out=X[:, dc, 0:n], in0=po[:, 0:n], scalar=self.mod(l, 2, dc, tsel),
                        in1=X[:, dc, 0:n], op0=ALU.mult, op1=ALU.add), reads=[po, self.MODS, X], writes=[X])
                k.dma("sp", XTv[:, :, t0:t0 + n], X[:, :, 0:n], reads=[X], writes=[self.XT], join=True, sem=X.sub("st"))

    def stage_na(self, l):
        k = self.k
        nc = self.nc
        j = l // 2
        last = (l == DEPTH - 1)
        OT = self.OT
        with k.stage():
            H = k.tile([128, 8, NTOK], BF16, "H")
            with k.stage():
                self.load_norm_all(l, 0, NTOK, H)
            gq = k.tile([128, 1], F32, "gq")
            gk = k.tile([128, 1], F32, "gk")
            with nc.allow_non_contiguous_dma(reason="tiny"):
                for hh in range(2):
                    k.dma("sp", gq[hh * 64:(hh + 1) * 64, :], self.na_q_norm[j].rearrange("(p o) -> p o", o=1),
                          reads=[self.na_q_norm], writes=[gq], join=(hh > 0))
                    k.dma("sp", gk[hh * 64:(hh + 1) * 64, :], self.na_k_norm[j].rearrange("(p o) -> p o", o=1),
                          reads=[self.na_k_norm], writes=[gk], join=(hh > 0))
            k.op("dve", lambda e: e.tensor_scalar(out=gq.ap(), in0=gq.ap(), scalar1=0.125, scalar2=None, op0=ALU.mult),
                 reads=[gq], writes=[gq])
            bd = k.tile([128, 128], BF16, "bd")
            k.op("dve", lambda e: e.memset(bd.ap(), 0.0), writes=[bd])
            for hh in range(2):
                k.op("dve", lambda e, hh=hh: e.memset(bd[hh * 64:(hh + 1) * 64, hh * 64:(hh + 1) * 64], 1.0 / 64),
                     writes=[bd])
            self.alloc_stg()
            wq = [k.tile([128, 8, 3, 128], BF16, "wqkv%d" % i) for i in range(2)]
            QT = k.tile([128, 2, NTOK], BF16, "QT")
            k.op("pool", lambda e: e.memset(QT.ap(), 0.0), writes=[QT])
            KT = k.tile([128, NTOK], BF16, "KT")
            V = k.tile([128, NTOK // 128, 2, 64], BF16, "V")
            OTs = k.tile([128, NTOK], BF16, "OTs")
            BI = k.tile([128, 9, 2, 320], F32, "BI")
            qf = [k.tile([128, 512], F32, "qf%d" % i) for i in range(4)]
            qs = [k.tile([128, 512], BF16, "qs%d" % i) for i in range(4)]
            qr = [k.tile([128, 512], F32, "qr%d" % i) for i in range(4)]
            ET = [k.tile([128, 512], BF16, "ET%d" % i) for i in range(4)]
            rd = [k.tile([128, 256], F32, "rd%d" % i) for i in range(4)]
            w_in = self.na_w_in[j].rearrange("(kk p) n -> p kk n", p=128)
            OTv = OT.ap().rearrange("(c p) t -> p c t", p=128)
            cnt = 0
            for hp in range(getattr(self, "na_hp_limit", 8)):
                w = wq[hp % 2]
                for part in range(3):
                    self.cast_load(w[:, :, part, :], w_in[:, :, part * D + hp * 128: part * D + (hp + 1) * 128],
                                   "p (a b) -> p a b", w, first=(part == 0), a=8)
                if getattr(self, "na_cut", 9) > 2:
                  k.dma("sp", BI.ap().rearrange("p r h q -> p r (h q)"),
                      self.na_bias[j].rearrange("r p (h q) -> p r h q", h=16)[:, :, 2 * hp:2 * hp + 2, :].rearrange("p r h q -> p r (h q)"),
                      reads=[self.na_bias], writes=[BI])
                def qk_chain(part, dst, gcol, t0, n, ci, w=w):
                    pp = self.ps[ci % 4]
                    pm = self.ps[4 + ci % 4]
                    f_ = qf[ci % 4]
                    s_ = qs[ci % 4]
                    r_ = qr[ci % 4]
                    for kk in range(8):
                        k.op("pe", lambda e, kk=kk: e.matmul(
                            pp[:, 0:n], lhsT=w[:, kk, part, :], rhs=H[:, kk, t0:t0 + n],
                            start=(kk == 0), stop=(kk == 7)), reads=[w, H], writes=[pp])
                    yield
                    k.op("act", lambda e: e.activation(out=s_[:, 0:n], in_=pp[:, 0:n], func=AF.Square),
                         reads=[pp], writes=[s_])
                    k.op("dve", lambda e: e.tensor_copy(out=f_[:, 0:n], in_=pp[:, 0:n]),
                         reads=[pp], writes=[f_])
                    yield
                    k.op("pe", lambda e: e.matmul(pm[:, 0:n], lhsT=bd.ap(), rhs=s_[:, 0:n],
                                                  start=True, stop=True), reads=[bd, s_], writes=[pm])
                    k.op("act", lambda e: e.activation(out=r_[:, 0:n], in_=pm[:, 0:n], func=AF.Sqrt,
                                                       scale=1.0, bias=self.eps_t.ap()),
                         reads=[pm, self.eps_t], writes=[r_])
                    yield
                    k.op("dve", lambda e: e.reciprocal(out=r_[:, 0:n], in_=r_[:, 0:n]), reads=[r_], writes=[r_])
                    if part == 0:
                        for hl in range(2):
                            hs = slice(hl * 64, (hl + 1) * 64)
                            k.op("dve", lambda e, hs=hs, hl=hl: e.scalar_tensor_tensor(
                                out=QT[hs, hl, t0:t0 + n], in0=f_[hs, 0:n], scalar=gq[hs, :], in1=r_[hs, 0:n],
                                op0=ALU.mult, op1=ALU.mult), reads=[f_, r_, gq], writes=[QT])
                    else:
                        k.op("dve", lambda e: e.scalar_tensor_tensor(
                            out=dst[:, t0:t0 + n], in0=f_[:, 0:n], scalar=gcol.ap(), in1=r_[:, 0:n],
                            op0=ALU.mult, op1=ALU.mult), reads=[f_, r_, gcol], writes=[dst])
                    yield

                todo = [(part, dst, gcol, t0, n) for part, (dst, gcol) in enumerate(((QT, gq), (KT, gk)))
                        for (t0, n) in token_blocks(0, NTOK)]
                active = []
                while todo or active:
                    while todo and len(active) < 3:
                        active.append(qk_chain(*todo.pop(0), cnt))
                        cnt += 1
                    for g_ in list(active):
                        try:
                            next(g_)
                        except StopIteration:
                            active.remove(g_)
                for ti in range(NTOK // 128 if getattr(self, "na_cut", 9) > 1 else 0):
                    pv = self.ps[4 + ti % 2]
                    for kk in range(8):
                        k.op("pe", lambda e, kk=kk, pv=pv, w=w, ti=ti: e.matmul(
                            pv[:, 0:128], lhsT=H[:, kk, ti * 128:(ti + 1) * 128], rhs=w[:, kk, 2, :],
                            start=(kk == 0), stop=(kk == 7)), reads=[w, H], writes=[pv])
                    eng = "act" if ti % 2 == 0 else "dve"
                    if eng == "act":
                        k.op("act", lambda e, pv=pv, ti=ti: e.copy(out=V[:, ti, :, :].rearrange("p h d -> p (h d)"),
                                                                  in_=pv[:, 0:128]), reads=[pv], writes=[V])
                    else:
                        k.op("dve", lambda e, pv=pv, ti=ti: e.tensor_copy(out=V[:, ti, :, :].rearrange("p h d -> p (h d)"),
                                                                         in_=pv[:, 0:128]), reads=[pv], writes=[V])
                if getattr(self, "na_cut", 9) <= 2:
                    continue
                groups = []
                for r in range(64):
                    rs_ = min(max(r - 4, 0), 56)
                    tb = rs_ // 2
                    nt = 4 if rs_ % 2 == 0 else 5
                    if r < 4:
                        rt = r
                    elif r > 60:
                        rt = r - 56 + 1
                    else:
                        rt = 4 + (rs_ % 2)
                    groups.append((r * 64, 64, [tb + i for i in range(nt)] + [32, 33], rt, nt))
                if not last and getattr(self, "na_cut", 9) > 3:
                    groups.append((T, 256, [32, 33], None, 0))
                def att_chain(q0, nq, tiles, rt, nt, hl, ci):
                    pS = self.ps[ci % 4]
                    pO = self.ps[4 + ci % 4]
                    et = ET[ci % 4]
                    rd_ = rd[ci % 4]
                    hs = slice(hl * 64, (hl + 1) * 64)
                    W_ = len(tiles) * nq
                    for si, tl in enumerate(tiles):
                        k.op("pe", lambda e, si=si, tl=tl: e.matmul(
                            pS[:, si * nq:(si + 1) * nq], lhsT=KT[:, tl * 128:(tl + 1) * 128],
                            rhs=QT[:, hl, q0:q0 + nq], start=True, stop=True), reads=[KT, QT], writes=[pS])
                    if rt is not None:
                        k.op("dve", lambda e: e.tensor_tensor(
                            out=pS[:, 0:nt * 64], in0=pS[:, 0:nt * 64], in1=BI[:, rt, hl, 0:nt * 64], op=ALU.add),
                            reads=[pS, BI], writes=[pS])
                    k.op("act", lambda e: e.activation(out=et[:, 0:W_], in_=pS[:, 0:W_], func=AF.Exp),
                         reads=[pS], writes=[et])
                    yield
                    for si, tl in enumerate(tiles):
                        k.op("pe", lambda e, si=si, tl=tl: e.matmul(
                            pO[hs, 0:nq], lhsT=V[:, tl, hl, :], rhs=et[:, si * nq:(si + 1) * nq],
                            start=(si == 0), stop=(si == len(tiles) - 1)), reads=[V, et], writes=[pO])
                    for si, tl in enumerate(tiles):
                        k.op("pe", lambda e, si=si: e.matmul(
                            pO[hs, 256:256 + nq], lhsT=self.ones_bf[:, 0:64], rhs=et[:, si * nq:(si + 1) * nq],
                            start=(si == 0), stop=(si == len(tiles) - 1)), reads=[self.ones_bf, et], writes=[pO])
                    yield
                    k.op("dve", lambda e: e.reciprocal(out=rd_[hs, 0:nq], in_=pO[hs, 256:256 + nq]),
                         reads=[pO], writes=[rd_])
                    k.op("dve", lambda e: e.tensor_tensor(
                        out=OTs[hs, q0:q0 + nq], in0=pO[hs, 0:nq], in1=rd_[hs, 0:nq], op=ALU.mult),
                        reads=[pO, rd_], writes=[OTs.sub(hl)])
                    yield

                todo = [(q0, nq, tiles, rt, nt, hl) for (q0, nq, tiles, rt, nt) in groups for hl in range(2)]
                active = []
                ci = 0
                while todo or active:
                    while todo and len(active) < 3:
                        active.append(att_chain(*todo.pop(0), ci))
                        ci += 1
                    for g_ in list(active):
                        try:
                            next(g_)
                        except StopIteration:
                            active.remove(g_)
                ntk = T if last else NTOK
                for c0 in range(0, ntk, 1024):
                    c1 = min(ntk, c0 + 1024)
                    k.dma("sp", OTv[:, hp, c0:c1], OTs[:, c0:c1], reads=[OTs.sub(0), OTs.sub(1)], writes=[OT],
                          join=True, sem=OTs.sub("st"))
        self.stage_outproj(l, self.na_w_out[j], OT, T if last else NTOK)


    def stage_gdn(self, l):
        j = l // 2
        if not hasattr(self, "QKVT"):
            k = self.k
            self.QKVT = k.dram("QKVT", [3 * D, NTOK], F32)
            self.GATE = k.dram("GATE", [NTOK, D], BF16)
            self.GB = k.dram("GB", [NTOK, 32], F32)
            self.OD = k.dram("OD", [2, NTOK, D], F32)
        self.gdn_proj(l, j)
        self.gdn_scan(l, j)
        self.gdn_finish(l, j)
        self.stage_outproj(l, self.gdn_w_out[j], self.OT, NTOK)

    def gdn_proj(self, l, j):
        k = self.k
        nc = self.nc
        w_in = self.gdn_w_in[j].rearrange("(kk p) n -> p kk n", p=128)
        QV = self.QKVT.ap().rearrange("(c p) t -> p c t", p=128)
        PADN = NTOK + 4
        with k.stage():
            H = k.tile([128, 8, NTOK], BF16, "H")
            with k.stage():
                self.load_norm_all(l, 0, NTOK, H)
            self.alloc_stg()
            cw = k.tile([128, 24, 3], F32, "cw")
            with nc.allow_non_contiguous_dma(reason="small conv weights"):
                for s_ in range(3):
                    k.dma("sp", cw[:, :, s_], self.gdn_conv_w[j, s_].rearrange("(c p) -> p c", p=128),
                          reads=[self.gdn_conv_w], writes=[cw], join=(s_ > 0))
            wb = [k.tile([128, 8, 128], BF16, "wfc%d" % i) for i in range(2)]
            PT = k.tile([128, PADN], F32, "PT")
            k.op("pool", lambda e: e.memset(PT.ap(), 0.0), writes=[PT])
            cv = [k.tile([128, 512], F32, "cv%d" % i) for i in range(4)]
            sv = [k.tile([128, 512], F32, "sv%d" % i) for i in range(4)]
            sqb = [k.tile([128, 512], BF16, "sqb%d" % i) for i in range(4)]
            rr = [k.tile([128, 512], F32, "rr%d" % i) for i in range(4)]
            cnt = 0
            for fc in range(24):
                w = wb[fc % 2]
                self.cast_load(w.ap(), w_in[:, :, fc * 128:(fc + 1) * 128], "p (a b) -> p a b", w, first=True, a=8)
                for (t0, n) in token_blocks(0, NTOK):
                    pp = self.ps[cnt % 2]
                    cnt += 1
                    for kk in range(8):
                        k.op("pe", lambda e, kk=kk, pp=pp, w=w: e.matmul(
                            pp[:, 0:n], lhsT=w[:, kk, :], rhs=H[:, kk, t0:t0 + n], start=(kk == 0), stop=(kk == 7)),
                            reads=[w, H], writes=[pp])
                    pc = 1 + t0 if t0 < T else 3 + t0
                    k.op("act", lambda e, pp=pp, pc=pc: e.copy(out=PT[:, pc:pc + n], in_=pp[:, 0:n]),
                         reads=[pp], writes=[PT])
                def conv_chain(t0, n, ci, fc=fc):
                    pc = t0 if t0 < T else 2 + t0
                    c_ = cv[ci % 4]
                    s2 = sv[ci % 4]
                    q_ = sqb[ci % 4]
                    r_ = rr[ci % 4]
                    pm = self.ps[2 + ci % 4]
                    k.op("dve", lambda e: e.tensor_scalar(
                        out=c_[:, 0:n], in0=PT[:, pc:pc + n], scalar1=cw[:, fc, 0:1], scalar2=None, op0=ALU.mult),
                        reads=[PT, cw], writes=[c_])
                    for s_ in (1, 2):
                        k.op("dve", lambda e, s_=s_: e.scalar_tensor_tensor(
                            out=c_[:, 0:n], in0=PT[:, pc + s_:pc + s_ + n], scalar=cw[:, fc, s_:s_ + 1], in1=c_[:, 0:n],
                            op0=ALU.mult, op1=ALU.add), reads=[PT, cw, c_], writes=[c_])
                    k.op("act", lambda e: e.activation(out=s2[:, 0:n], in_=c_[:, 0:n], func=AF.Silu),
                         reads=[c_], writes=[s2])
                    if fc < 16:
                        k.op("act", lambda e: e.activation(out=q_[:, 0:n], in_=s2[:, 0:n], func=AF.Square),
                             reads=[s2], writes=[q_])
                        yield
                        k.op("pe", lambda e: e.matmul(pm[:, 0:n], lhsT=self.ones_bf.ap(), rhs=q_[:, 0:n],
                                                      start=True, stop=True),
                             reads=[self.ones_bf, q_], writes=[pm])
                        k.op("act", lambda e: e.activation(out=r_[:, 0:n], in_=pm[:, 0:n], func=AF.Sqrt,
                                                           scale=1.0, bias=self.eps_t.ap()),
                             reads=[pm, self.eps_t], writes=[r_])
                        yield
                        k.op("dve", lambda e: e.reciprocal(out=r_[:, 0:n], in_=r_[:, 0:n]), reads=[r_], writes=[r_])
                        sc_ = (128.0 ** -0.5) if fc < 8 else 1.0
                        k.op("dve", lambda e: e.scalar_tensor_tensor(
                            out=s2[:, 0:n], in0=s2[:, 0:n], scalar=sc_, in1=r_[:, 0:n], op0=ALU.mult, op1=ALU.mult),
                            reads=[s2, r_], writes=[s2])
                    k.dma("sp", QV[:, fc, t0:t0 + n], s2[:, 0:n], reads=[s2], writes=[self.QKVT], join=True, sem=s2.sub("st"))
                    yield

                todo = list(token_blocks(0, NTOK))
                active = []
                while todo or active:
                    while todo and len(active) < 3:
                        active.append(conv_chain(*todo.pop(0), cnt))
                        cnt += 1
                    for g_ in list(active):
                        try:
                            next(g_)
                        except StopIteration:
                            active.remove(g_)
            WG = k.tile([128, 8, D], BF16, "WG")
            for kk in range(0, 8, 2):
                self.cast_load(WG[:, kk:kk + 2, :], w_in[:, kk:kk + 2, 3 * D:4 * D], "p (a b) -> p a b", WG,
                               first=(kk == 0), a=2)
            WAB = k.tile([128, 8, 32], BF16, "WAB")
            with nc.allow_non_contiguous_dma(reason="32-col slice"):
                self.cast_load(WAB.ap(), w_in[:, :, 4 * D:4 * D + 32], "p (a b) -> p a b", WAB, first=True, a=8)
            ALc = k.tile([128, 32], F32, "ALc")
            DTc = k.tile([128, 32], F32, "DTc")
            k.op("dve", lambda e: e.memset(ALc.ap(), 0.0), writes=[ALc])
            k.op("dve", lambda e: e.memset(DTc.ap(), 0.0), writes=[DTc])
            with nc.allow_non_contiguous_dma(reason="tiny broadcast"):
                for d_ in range(2):
                    k.dma("sp", ALc[:, d_ * 16:d_ * 16 + 8], self.gdn_a_log[j, d_].partition_broadcast(128),
                          reads=[self.gdn_a_log], writes=[ALc], join=(d_ > 0))
                    k.dma("sp", DTc[:, d_ * 16:d_ * 16 + 8], self.gdn_dt_bias[j, d_].partition_broadcast(128),
                          reads=[self.gdn_dt_bias], writes=[DTc], join=(d_ > 0))
            k.op("act", lambda e: e.activation(out=ALc.ap(), in_=ALc.ap(), func=AF.Exp), reads=[ALc], writes=[ALc])
            k.op("dve", lambda e: e.tensor_scalar(out=ALc.ap(), in0=ALc.ap(), scalar1=-1.0, scalar2=None, op0=ALU.mult),
                 reads=[ALc], writes=[ALc])
            gt = [k.tile([128, D], BF16, "gt%d" % i) for i in range(2)]
            xa = [k.tile([128, 32], F32, "xa%d" % i) for i in range(2)]
            xb_ = [k.tile([128, 32], F32, "xb_%d" % i) for i in range(2)]
            xc = [k.tile([128, 32], F32, "xc%d" % i) for i in range(2)]
            one_col = self.cst[:, 384:385]
            for ti in range(NTOK // 128):
                g_ = gt[ti % 2]
                for hf in range(2):
                    pg = self.ps[4 + hf]
                    for kk in range(8):
                        k.op("pe", lambda e, kk=kk, pg=pg, hf=hf: e.matmul(
                            pg[:, 0:512], lhsT=H[:, kk, ti * 128:(ti + 1) * 128], rhs=WG[:, kk, hf * 512:(hf + 1) * 512],
                            start=(kk == 0), stop=(kk == 7)), reads=[H, WG], writes=[pg])
                    k.op("act", lambda e, pg=pg, g_=g_, hf=hf: e.activation(out=g_[:, hf * 512:(hf + 1) * 512], in_=pg[:, 0:512],
                                                                           func=AF.Silu), reads=[pg], writes=[g_.sub(hf)])
                k.dma("sp", self.GATE[ti * 128:(ti + 1) * 128, :], g_.ap(), reads=[g_.sub(0), g_.sub(1)], writes=[self.GATE],
                      join=True, sem=g_.sub("st"))
                pa = self.ps[6 + ti % 2]
                a_ = xa[ti % 2]
                b_ = xb_[ti % 2]
                c_ = xc[ti % 2]
                for kk in range(8):
                    k.op("pe", lambda e, kk=kk, pa=pa: e.matmul(
                        pa[:, 0:32], lhsT=H[:, kk, ti * 128:(ti + 1) * 128], rhs=WAB[:, kk, :],
                        start=(kk == 0), stop=(kk == 7)), reads=[H, WAB], writes=[pa])
                k.op("dve", lambda e, pa=pa, a_=a_: e.tensor_tensor(out=a_.ap(), in0=pa[:, 0:32], in1=DTc.ap(), op=ALU.add),
                     reads=[pa, DTc], writes=[a_])
                k.op("dve", lambda e, a_=a_, b_=b_: e.scalar_tensor_tensor(out=b_.ap(), in0=a_.ap(), scalar=-1.0, in1=a_.ap(),
                                                                          op0=ALU.mult, op1=ALU.max), reads=[a_], writes=[b_])
                k.op("act", lambda e, b_=b_: e.activation(out=b_.ap(), in_=b_.ap(), func=AF.Exp, scale=-1.0),
                     reads=[b_], writes=[b_])
                k.op("act", lambda e, b_=b_, c_=c_: e.activation(out=c_.ap(), in_=b_.ap(), func=AF.Ln, bias=one_col, scale=1.0),
                     reads=[b_, self.cst], writes=[c_])
                k.op("dve", lambda e, a_=a_, c_=c_: e.scalar_tensor_tensor(out=c_.ap(), in0=a_.ap(), scalar=0.0, in1=c_.ap(),
                                                                          op0=ALU.max, op1=ALU.add), reads=[a_, c_], writes=[c_])
                k.op("dve", lambda e, c_=c_: e.tensor_tensor(out=c_.ap(), in0=c_.ap(), in1=ALc.ap(), op=ALU.mult),
                     reads=[c_, ALc], writes=[c_])
                k.op("act", lambda e, pa=pa, b_=b_: e.activation(out=b_.ap(), in_=pa[:, 0:32], func=AF.Sigmoid),
                     reads=[pa], writes=[b_])
                cvw = c_.ap().rearrange("p (d a h) -> p d a h", d=2, a=2)
                bvw = b_.ap().rearrange("p (d a h) -> p d a h", d=2, a=2)
                k.op("dve", lambda e, cvw=cvw, bvw=bvw, c_=c_, b_=b_: e.tensor_copy(out=cvw[:, :, 1, :], in_=bvw[:, :, 1, :]),
                     reads=[b_, c_], writes=[c_])
                k.dma("sp", self.GB[ti * 128:(ti + 1) * 128, :], c_.ap(), reads=[c_], writes=[self.GB], join=True,
                      sem=c_.sub("st"))

    def gdn_scan(self, l, j):
        k = self.k
        nc = self.nc
        QV = self.QKVT.ap().rearrange("(a h p) t -> a p h t", a=3, p=128)
        BIG = 30000.0
        with k.stage():
            f4 = lambda nm: k.tile([128, 8, 128], F32, nm)
            ones32 = self.cst[:, 384:512]
            tril = self.cst[:, 128:256]
            triu = self.cst[:, 256:384]
            I8 = f4("I8")
            for h in range(8):
                k.op("pool", lambda e, h=h: e.tensor_copy(out=I8[:, h, :], in_=self.ident), reads=[self.cst], writes=[I8])
            M2s, NM2T, U = [], [], []
            for d in range(2):
                a = k.tile([128, 128], F32, "M2s%d" % d)
                b = k.tile([128, 128], F32, "NM2T%d" % d)
                src_s = triu if d == 0 else tril
                k.op("dve", lambda e, a=a, src_s=src_s: e.tensor_scalar(out=a.ap(), in0=src_s, scalar1=BIG, scalar2=None,
                                                                        op0=ALU.mult), reads=[self.cst], writes=[a])
                src_t = triu if d == 0 else tril
                k.op("dve", lambda e, b=b, src_t=src_t: e.tensor_scalar(out=b.ap(), in0=src_t, scalar1=BIG, scalar2=-BIG,
                                                                        op0=ALU.mult, op1=ALU.add), reads=[self.cst], writes=[b])
                M2s.append(a)
                NM2T.append(b)
                U.append(triu if d == 0 else tril)
            inb = [[f4("in%d_%d" % (a_, i)) for a_ in range(3)] for i in range(2)]
            gbb = [k.tile([128, 32], F32, "gbb%d" % i) for i in range(2)]
            WS = []
            for d_ in range(2):
                w_ = {}
                for nm in ("gc", "egl", "ekd", "bge"):
                    w_[nm] = k.tile([128, 8], F32, "%s%d" % (nm, d_))
                for nm in ("Gd", "Dms", "DmT", "L0", "L1", "M0", "M1", "R", "vb", "kbg", "kd", "u", "wT", "vn", "qg", "QKd"):
                    w_[nm] = f4("%s%d" % (nm, d_))
                WS.append(w_)
            osb = [k.tile([128, D], F32, "osb%d" % i) for i in range(2)]
            S = [f4("S%d" % d) for d in range(2)]
            bctr = {}

            def nbank(key):
                d_, hf_ = key
                base = 2 * (2 * d_ + hf_)
                n_ = bctr.get(key, 0)
                bctr[key] = n_ + 1
                return self.ps[base + n_ % 2]

            def chain(d, ib, gb, ob, t0, hf):
                qT, kT, vT = ib
                hs = list(range(hf * 4, hf * 4 + 4))
                v4 = lambda t: t[:, hf * 4:(hf + 1) * 4, :].rearrange("p h d -> p (h d)")
                G = lambda h: gb[:, d * 16 + h:d * 16 + h + 1]
                Bt = lambda h: gb[:, d * 16 + 8 + h:d * 16 + 8 + h + 1]
                csl = lambda i: slice(i * 128, (i + 1) * 128)
                Sd = S[d]
                w_ = WS[d]
                gc, egl, ekd, bge = w_["gc"], w_["egl"], w_["ekd"], w_["bge"]
                Gd, Dms, DmT, R = w_["Gd"], w_["Dms"], w_["DmT"], w_["R"]
                Lb, Mb = [w_["L0"], w_["L1"]], [w_["M0"], w_["M1"]]
                vb, kbg, kd, u, wT, vn, qg, QKd = (w_[n_] for n_ in ("vb", "kbg", "kd", "u", "wT", "vn", "qg", "QKd"))
                nb = lambda: nbank((d, hf))

                def mm4(lhs_fn, rhs_fn, bank, reads, start=True, stop=True):
                    for i, h in enumerate(hs):
                        k.op("pe", lambda e, i=i, h=h: e.matmul(bank[:, csl(i)], lhsT=lhs_fn(h), rhs=rhs_fn(h), start=start, stop=stop),
                             reads=list(reads), writes=[bank])

                def ev(dst, bank, eng):
                    if eng == "act":
                        k.op("act", lambda e: e.copy(out=v4(dst), in_=bank.ap()), reads=[bank], writes=[dst.sub(hf)])
                    else:
                        k.op("dve", lambda e: e.tensor_copy(out=v4(dst), in_=bank.ap()), reads=[bank], writes=[dst.sub(hf)])

                for h in hs:
                    k.op("act", lambda e, h=h: e.activation(out=Gd[:, h, :], in_=U[d], func=AF.Copy, scale=G(h)),
                         reads=[self.cst, gb], writes=[Gd.sub(hf)])
                yield
                bB = nb()
                mm4(lambda h: ones32, lambda h: Gd[:, h, :], bB, [self.cst, Gd.sub(hf)])
                yield
                for i, h in enumerate(hs):
                    k.op("dve", lambda e, h=h, i=i: e.scalar_tensor_tensor(
                        out=Dms[:, h, :], in0=bB[:, csl(i)], scalar=gc[:, h:h + 1], in1=M2s[d].ap(),
                        op0=ALU.subtract, op1=ALU.max), reads=[bB, gc, M2s[d]], writes=[Dms.sub(hf)])
                    k.op("dve", lambda e, h=h, i=i: e.scalar_tensor_tensor(
                        out=DmT[:, h, :], in0=bB[:, csl(i)], scalar=gc[:, h:h + 1], in1=NM2T[d].ap(),
                        op0=ALU.subtract, op1=ALU.min), reads=[bB, gc, NM2T[d]], writes=[DmT.sub(hf)])
                yield
                k.op("act", lambda e: e.activation(out=v4(qg), in_=bB.ap(), func=AF.Exp), reads=[bB], writes=[qg.sub(hf)])
                k.op("act", lambda e: e.activation(out=v4(Dms), in_=v4(Dms), func=AF.Exp, scale=-1.0), reads=[Dms.sub(hf)],
                     writes=[Dms.sub(hf)])
                k.op("act", lambda e: e.activation(out=v4(DmT), in_=v4(DmT), func=AF.Exp), reads=[DmT.sub(hf)], writes=[DmT.sub(hf)])
                bK = nb()
                mm4(lambda h: kT[:, h, :], lambda h: kT[:, h, :], bK, [kT])
                yield
                k.op("dve", lambda e: e.tensor_tensor(out=v4(qg), in0=v4(qg), in1=v4(qT), op=ALU.mult),
                     reads=[qg.sub(hf), qT], writes=[qg.sub(hf)])
                L, M = Lb[0], Mb[0]
                for i, h in enumerate(hs):
                    k.op("dve", lambda e, h=h, i=i: e.scalar_tensor_tensor(
                        out=L[:, h, :], in0=bK[:, csl(i)], scalar=Bt(h), in1=Dms[:, h, :], op0=ALU.mult, op1=ALU.mult),
                        reads=[bK, gb, Dms.sub(hf)], writes=[L.sub(hf)])
                yield
                bM = nb()
                for i, h in enumerate(hs):
                    k.op("pe", lambda e, h=h, i=i: e.transpose(bM[:, csl(i)], L[:, h, :], self.ident),
                         reads=[L.sub(hf), self.cst], writes=[bM])
                yield
                ev(M, bM, "act")
                k.op("dve", lambda e: e.tensor_tensor(out=v4(R), in0=v4(I8), in1=bM.ap(), op=ALU.subtract),
                     reads=[bM, I8], writes=[R.sub(hf)])
                yield
                for lev in range(1, 7):
                    Lp, Mp = Lb[(lev - 1) % 2], Mb[(lev - 1) % 2]
                    Ln_, Mn_ = Lb[lev % 2], Mb[lev % 2]
                    bL = nb()
                    mm4(lambda h: Mp[:, h, :], lambda h: Lp[:, h, :], bL, [Mp.sub(hf), Lp.sub(hf)])
                    if lev < 6:
                        bN = nb()
                        mm4(lambda h: Lp[:, h, :], lambda h: Mp[:, h, :], bN, [Mp.sub(hf), Lp.sub(hf)])
                    yield
                    ev(Ln_, bL, "act")
                    if lev < 6:
                        ev(Mn_, bN, "dve")
                    yield
                    bR = nb()
                    mm4(lambda h: Ln_[:, h, :], lambda h: R[:, h, :], bR, [Ln_.sub(hf), R.sub(hf)])
                    yield
                    k.op("dve", lambda e, bR=bR: e.tensor_tensor(out=v4(R), in0=v4(R), in1=bR.ap(), op=ALU.add),
                         reads=[bR, R.sub(hf)], writes=[R.sub(hf)])
                    yield
                bT = nb()
                for i, h in enumerate(hs):
                    k.op("pe", lambda e, h=h, i=i: e.transpose(bT[:, csl(i)], kT[:, h, :], self.ident), reads=[kT, self.cst], writes=[bT])
                bV = nb()
                for i, h in enumerate(hs):
                    k.op("pe", lambda e, h=h, i=i: e.transpose(bV[:, csl(i)], vT[:, h, :], self.ident), reads=[vT, self.cst], writes=[bV])
                yield
                for i, h in enumerate(hs):
                    k.op("act", lambda e, h=h, i=i: e.activation(out=kbg[:, h, :], in_=bT[:, csl(i)], func=AF.Copy,
                                                                scale=bge[:, h:h + 1]), reads=[bT, bge], writes=[kbg.sub(hf)])
                    k.op("dve", lambda e, h=h, i=i: e.tensor_scalar(out=kd[:, h, :], in0=bT[:, csl(i)], scalar1=ekd[:, h:h + 1],
                                                                  scalar2=None, op0=ALU.mult), reads=[bT, ekd], writes=[kd.sub(hf)])
                yield
                for i, h in enumerate(hs):
                    k.op("act", lambda e, h=h, i=i: e.activation(out=vb[:, h, :], in_=bV[:, csl(i)], func=AF.Copy, scale=Bt(h)),
                         reads=[bV, gb], writes=[vb.sub(hf)])
                yield
                bU = nb()
                mm4(lambda h: R[:, h, :], lambda h: vb[:, h, :], bU, [R.sub(hf), vb.sub(hf)])
                bW = nb()
                mm4(lambda h: kbg[:, h, :], lambda h: R[:, h, :], bW, [R.sub(hf), kbg.sub(hf)])
                yield
                ev(u, bU, "act")
                ev(wT, bW, "dve")
                bQ = nb()
                mm4(lambda h: kT[:, h, :], lambda h: qT[:, h, :], bQ, [kT, qT])
                yield
                k.op("dve", lambda e: e.tensor_tensor(out=v4(QKd), in0=bQ.ap(), in1=v4(DmT), op=ALU.mult),
                     reads=[bQ, DmT.sub(hf)], writes=[QKd.sub(hf)])
                bS = nb()
                mm4(lambda h: wT[:, h, :], lambda h: Sd[:, h, :], bS, [wT.sub(hf), Sd.sub(hf)])
                yield
                k.op("dve", lambda e: e.tensor_tensor(out=v4(vn), in0=v4(u), in1=bS.ap(), op=ALU.subtract),
                     reads=[bS, u.sub(hf)], writes=[vn.sub(hf)])
                yield
                bO = nb()
                for i, h in enumerate(hs):
                    k.op("pe", lambda e, h=h, i=i: e.matmul(bO[:, csl(i)], lhsT=qg[:, h, :], rhs=Sd[:, h, :], start=True, stop=False),
                         reads=[qg.sub(hf), Sd.sub(hf)], writes=[bO])
                    k.op("pe", lambda e, h=h, i=i: e.matmul(bO[:, csl(i)], lhsT=QKd[:, h, :], rhs=vn[:, h, :], start=False, stop=True),
                         reads=[QKd.sub(hf), vn.sub(hf)], writes=[bO])
                bN2 = nb()
                mm4(lambda h: kd[:, h, :], lambda h: vn[:, h, :], bN2, [kd.sub(hf), vn.sub(hf)])
                yield
                k.op("act", lambda e: e.copy(out=ob[:, hf * 512:(hf + 1) * 512], in_=bO.ap()), reads=[bO], writes=[ob.sub(hf)])
                k.dma("sp", self.OD[d, t0:t0 + 128, hf * 512:(hf + 1) * 512], ob[:, hf * 512:(hf + 1) * 512],
                      reads=[ob.sub(hf)], writes=[self.OD], join=True, sem=ob.sub("st%d" % hf))
                for i, h in enumerate(hs):
                    k.op("dve", lambda e, h=h, i=i: e.scalar_tensor_tensor(
                        out=Sd[:, h, :], in0=Sd[:, h, :], scalar=egl[:, h:h + 1], in1=bN2[:, csl(i)], op0=ALU.mult, op1=ALU.add),
                        reads=[bN2, egl, Sd.sub(hf)], writes=[Sd.sub(hf)])
                yield

            it = 0
            order = {0: [32, 33] + list(range(32)), 1: [33, 32] + list(range(31, -1, -1))}
            for step in range(34):
                gens = []
                for d in range(2):
                    c = order[d][step]
                    t0 = c * 128
                    ib = inb[d]
                    gb = gbb[d]
                    ob = osb[d]
                    w_ = WS[d]
                    gc, egl, ekd, bge = w_["gc"], w_["egl"], w_["ekd"], w_["bge"]
                    for a_ in range(3):
                        k.dma("sp", ib[a_].ap(), QV[a_, :, :, t0:t0 + 128], reads=[self.QKVT], writes=[ib[a_]])
                    k.dma("sp", gb.ap(), self.GB[t0:t0 + 128, :], reads=[self.GB], writes=[gb])
                    if step == 0:
                        for hf in range(2):
                            k.op("pool", lambda e, d=d, hf=hf: e.memset(S[d][:, hf * 4:(hf + 1) * 4, :], 0.0), writes=[S[d].sub(hf)])
                    Gall = gb[:, d * 16:d * 16 + 8]
                    Ball = gb[:, d * 16 + 8:d * 16 + 16]
                    p0 = nbank((d, 0))
                    k.op("pe", lambda e, p0=p0, d=d, Gall=Gall: e.matmul(p0[:, 0:8], lhsT=U[d], rhs=Gall, start=True, stop=True),
                         reads=[self.cst, gb], writes=[p0])
                    k.op("pe", lambda e, p0=p0, Gall=Gall: e.matmul(p0[:, 8:16], lhsT=ones32, rhs=Gall, start=True, stop=True),
                         reads=[self.cst, gb], writes=[p0])
                    k.op("dve", lambda e, p0=p0, gc=gc: e.tensor_copy(out=gc.ap(), in_=p0[:, 0:8]), reads=[p0], writes=[gc])
                    k.op("dve", lambda e, p0=p0, gc=gc, ekd=ekd: e.tensor_tensor(out=ekd.ap(), in0=p0[:, 8:16], in1=gc.ap(), op=ALU.subtract),
                         reads=[p0, gc], writes=[ekd])
                    k.op("act", lambda e, p0=p0, egl=egl: e.activation(out=egl.ap(), in_=p0[:, 8:16], func=AF.Exp), reads=[p0], writes=[egl])
                    k.op("act", lambda e, ekd=ekd: e.activation(out=ekd.ap(), in_=ekd.ap(), func=AF.Exp), reads=[ekd], writes=[ekd])
                    k.op("act", lambda e, gc=gc, bge=bge: e.activation(out=bge.ap(), in_=gc.ap(), func=AF.Exp), reads=[gc], writes=[bge])
                    k.op("dve", lambda e, Ball=Ball, bge=bge: e.tensor_tensor(out=bge.ap(), in0=bge.ap(), in1=Ball, op=ALU.mult),
                         reads=[bge, gb], writes=[bge])
                    gens += [chain(d, ib, gb, ob, t0, 0), chain(d, ib, gb, ob, t0, 1)]
                while gens:
                    for g_ in list(gens):
                        try:
                            next(g_)
                        except StopIteration:
                            gens.remove(g_)

    def gdn_finish(self, l, j):
        k = self.k
        nc = self.nc
        OTv = self.OT.ap().rearrange("(c p) t -> p c t", p=128)
        with k.stage():
            NG = k.tile([128, D], F32, "NG")
            with nc.allow_non_contiguous_dma(reason="tiny broadcast"):
                for h in range(8):
                    k.dma("sp", NG[:, h * 128:(h + 1) * 128], self.gdn_norm_g[j].partition_broadcast(128),
                          reads=[self.gdn_norm_g], writes=[NG], join=(h > 0))
            o0 = [k.tile([128, D], F32, "o0_%d" % i) for i in range(2)]
            o1 = [k.tile([128, D], F32, "o1_%d" % i) for i in range(2)]
            gtb = [k.tile([128, D], BF16, "gtb%d" % i) for i in range(2)]
            sq = k.tile([128, D], F32, "sq")
            ss = [k.tile([128, 8], F32, "ss%d" % i) for i in range(2)]
            yT = [k.tile([128, 8, 512], BF16, "yT%d" % i) for i in range(2)]
            ntile = NTOK // 128
            for ti in range(ntile):
                a, b, g_ = o0[ti % 2], o1[ti % 2], gtb[ti % 2]
                s_ = ss[ti % 2]
                grp, gi = ti // 4, ti % 4
                y_ = yT[grp % 2]
                for hf in range(2):
                    sl = slice(hf * 512, (hf + 1) * 512)
                    k.dma("sp", a[:, sl], self.OD[0, ti * 128:(ti + 1) * 128, sl], reads=[self.OD], writes=[a], join=(hf > 0))
                    k.dma("sp", b[:, sl], self.OD[1, ti * 128:(ti + 1) * 128, sl], reads=[self.OD], writes=[b], join=(hf > 0))
                k.dma("sp", g_.ap(), self.GATE[ti * 128:(ti + 1) * 128, :], reads=[self.GATE], writes=[g_])
                k.op("dve", lambda e, a=a, b=b: e.tensor_tensor(out=a.ap(), in0=a.ap(), in1=b.ap(), op=ALU.add), reads=[a, b], writes=[a])
                k.op("act", lambda e, a=a: e.activation(out=sq.ap(), in_=a.ap(), func=AF.Square), reads=[a], writes=[sq])
                k.op("dve", lambda e, s_=s_: e.tensor_reduce(out=s_.ap(), in_=sq.ap().rearrange("p (h d) -> p h d", h=8),
                                                             op=ALU.add, axis=mybir.AxisListType.X), reads=[sq], writes=[s_])
                k.op("act", lambda e, s_=s_: e.activation(out=s_.ap(), in_=s_.ap(), func=AF.Sqrt, scale=1.0 / 128, bias=self.eps_t.ap()),
                     reads=[s_, self.eps_t], writes=[s_])
                k.op("dve", lambda e, s_=s_: e.reciprocal(out=s_.ap(), in_=s_.ap()), reads=[s_], writes=[s_])
                for h in range(8):
                    k.op("dve", lambda e, h=h, a=a, s_=s_: e.scalar_tensor_tensor(
                        out=a[:, h * 128:(h + 1) * 128], in0=a[:, h * 128:(h + 1) * 128], scalar=s_[:, h:h + 1],
                        in1=NG[:, h * 128:(h + 1) * 128], op0=ALU.mult, op1=ALU.mult), reads=[a, s_, NG], writes=[a])
                k.op("dve", lambda e, a=a, g_=g_: e.tensor_tensor(out=a.ap(), in0=a.ap(), in1=g_.ap(), op=ALU.mult), reads=[a, g_], writes=[a])
                for hf in range(2):
                    bank = self.ps[(ti % 2) * 2 + hf]
                    for c4 in range(4):
                        c = hf * 4 + c4
                        k.op("pe", lambda e, c=c, c4=c4, bank=bank, a=a: e.transpose(bank[:, c4 * 128:(c4 + 1) * 128], a[:, c * 128:(c + 1) * 128],
                                                                                    self.ident), reads=[a, self.cst], writes=[bank])
                    o_ = y_[:, hf * 4:(hf + 1) * 4, gi * 128:(gi + 1) * 128]
                    if hf == 0:
                        k.op("act", lambda e, o_=o_, bank=bank, y_=y_: e.copy(out=o_, in_=bank.ap().rearrange("p (c t) -> p c t", t=128)),
                             reads=[bank], writes=[y_])
                    else:
                        k.op("dve", lambda e, o_=o_, bank=bank, y_=y_: e.tensor_copy(out=o_, in_=bank.ap().rearrange("p (c t) -> p c t", t=128)),
                             reads=[bank], writes=[y_])
                if gi == 3 or ti == ntile - 1:
                    nn = (gi + 1) * 128
                    k.dma("sp", OTv[:, :, grp * 512:grp * 512 + nn], y_[:, :, 0:nn], reads=[y_], writes=[self.OT], join=True,
                          sem=y_.sub("st"))


class _Shift:
    def __init__(self, t, sh):
        self.t = t
        self.d = t.d
        self.sh = sh

    def __getitem__(self, kk):
        a, b, c = kk
        c = slice(c.start + self.sh, c.stop + self.sh)
        return self.t[a, b, c]


class _SubView:
    def __init__(self, t, key):
        self.t = t
        self.d = t.sub(key)

    def __getitem__(self, kk):
        return self.t[kk]


KB._norm_orig = KB._norm


def _norm2(ds):
    out = []
    for d in ds:
        if d is None:
            continue
        if isinstance(d, (Tl, _SubView, _Shift)):
            out.append(d.d)
        else:
            out.append(d)
    return out


KB._norm = staticmethod(_norm2)


def build(layers=(0, 1, 2, 3), stages=None):
    nc = bass.Bass("TRN2", target_bir_lowering=False)
    P = Prog(nc, layers)
    P.stage_init()
    P.stage_in_transpose()
    for l in layers:
        if stages is None or "mix" in stages:
            if l % 2 == 0:
                P.stage_gdn(l)
            else:
                P.stage_na(l)
        if stages is None or "ffn" in stages:
            P.stage_ffn(l, moe=(l % 2 == 1))
    P.stage_out_transpose()
    P.k.barrier()
    return nc, P


def make_consts():
    c = np.zeros((128, 512 + 1024), np.float32)
    for e in range(8):
        c[e, 512 + e * 128:512 + (e + 1) * 128] = 1.0
    c[:, 0:128] = np.eye(128, dtype=np.float32)
    c[:, 128:256] = np.tril(np.ones((128, 128), np.float32))
    c[:, 256:384] = np.triu(np.ones((128, 128), np.float32))
    c[:, 384:512] = 1.0
    return c


def make_na_bias(rpb):
    rpb = np.asarray(rpb, np.float32)
    nl = rpb.shape[0]
    out = np.full((nl, 9, 128, 16, 320), -30000.0, np.float32)
    p = np.arange(128)
    q = np.arange(64)
    kc = p % 64
    wstart = np.clip(q - 8, 0, 48)
    valid_c = (kc[:, None] >= wstart[None, :]) & (kc[:, None] < wstart[None, :] + 16)
    dc_idx = np.clip(kc[:, None] - q[None, :] + 15, 0, 30)
    for rt in range(9):
        if rt < 4:
            r, rs_ = rt, 0
        elif rt == 4:
            r, rs_ = 8, 4
        elif rt == 5:
            r, rs_ = 9, 5
        else:
            r, rs_ = 56 + rt - 1, 56
        base = rs_ - rs_ % 2
        nt = 4 if rs_ % 2 == 0 else 5
        for slot in range(nt):
            grow = base + 2 * slot + p // 64
            jw = grow - rs_
            valid = valid_c & ((jw >= 0) & (jw < 8))[:, None]
            dr_idx = np.clip(grow - r + 7, 0, 14)
            vals = rpb[:, :, dr_idx[:, None], dc_idx]
            vals = np.transpose(vals, (0, 2, 1, 3))
            blk = out[:, rt, :, :, slot * 64:(slot + 1) * 64]
            out[:, rt, :, :, slot * 64:(slot + 1) * 64] = np.where(valid[None, :, None, :], vals, blk)
    return out.reshape(nl, 9, 128, 16 * 320)


def make_in_maps(P, inp, shared):
    in_maps = []
    for b in range(NCORES):
        m = {n: v for n, v in shared.items() if n in P.exts}
        m["x"] = np.ascontiguousarray(inp["x"][b])
        m["ctx"] = np.ascontiguousarray(inp["ctx"][b])
        m["cvec"] = np.ascontiguousarray(np.stack([inp["c"][b], inp["c_ctx"]], 0))
        in_maps.append(m)
    return in_maps


def make_shared(inp):
    shared = {n: np.ascontiguousarray(inp[n], dtype=np.float32) for n in (
        "ada_w", "ada_b", "norm1_g", "norm2_g", "gdn_w_in", "gdn_conv_w", "gdn_a_log", "gdn_dt_bias",
        "gdn_norm_g", "gdn_w_out", "na_w_in", "na_q_norm", "na_k_norm", "na_w_out", "ffn_w13", "ffn_w2",
        "moe_router", "moe_w13", "moe_w2")}
    shared["consts"] = make_consts()
    shared["na_bias"] = make_na_bias(inp["na_rpb"])
    return shared


def kernel(**inp):
    nc, P = build()
    shared = make_shared(inp)
    in_maps = make_in_maps(P, inp, shared)
    res = run_bass_kernel_spmd(nc, in_maps, core_ids=list(range(NCORES)))
    return np.stack([r["y"] for r in res.results], 0)
```
